# Optimizing a Trainium2 kernel written in Bass

```python
import math
import jax, jax.numpy as jnp
from jax import lax
import numpy as np

D_MODEL = 1024
BATCH = 8
SEQ = 4096
DEPTH = 2

CHUNK = 64
D_MIX = D_MODEL
NORM_EPS = 1e-5
GLA_HEADS = 4
GLA_WIDTH = 3 * D_MIX // 8
GLA_DV = GLA_WIDTH // GLA_HEADS
GLA_DK = GLA_DV // 2
GLA_GATE_RANK = 16
GLA_GATE_TEMP = 16.0
RET_HEADS = 4
RET_WIDTH = 3 * D_MIX // 8
RET_DV = RET_WIDTH // RET_HEADS
RET_DK = RET_DV // 2
ROPE_BASE = 10000.0
S5_WIDTH = D_MIX - GLA_WIDTH - RET_WIDTH
S5_GROUP_DIM = 16
S5_GROUPS = S5_WIDTH // S5_GROUP_DIM
S5_STATE = 64
N_EXPERTS = 32
TOP_K = 4
D_FF = D_MODEL
SWIGLU_LIMIT = 7.0
SWIGLU_ALPHA = 1.702
EXPERT_BLOCK = 128
IN_SIZES = (GLA_HEADS * GLA_DK, GLA_HEADS * GLA_DK, GLA_WIDTH, GLA_WIDTH, GLA_GATE_RANK,
            RET_HEADS * RET_DK, RET_HEADS * RET_DK, RET_WIDTH, RET_WIDTH, S5_WIDTH)
IN_COLS = sum(IN_SIZES)

kernel_name = 'hybrid_gla_retnet_s5_moe_adaln'


def rms_norm(x, w):
    xf = x.astype(jnp.float32)
    y = xf * lax.rsqrt(jnp.mean(xf * xf, axis=-1, keepdims=True) + NORM_EPS)
    return (y * w.astype(jnp.float32)).astype(x.dtype)


def head_rms_norm(o, w):
    y = o * lax.rsqrt(jnp.mean(o * o, axis=-1, keepdims=True) + NORM_EPS)
    return y * w.astype(jnp.float32).reshape(o.shape[-2], o.shape[-1])


def head_group_norm(o, w):
    mu = jnp.mean(o, axis=-1, keepdims=True)
    oc = o - mu
    y = oc * lax.rsqrt(jnp.mean(oc * oc, axis=-1, keepdims=True) + NORM_EPS)
    return y * w.astype(jnp.float32).reshape(o.shape[-2], o.shape[-1])


def rotary(t, cos, sin):
    t1, t2 = jnp.split(t, 2, axis=-1)
    return jnp.concatenate([t1 * cos - t2 * sin, t1 * sin + t2 * cos], axis=-1)


def chunked_gated_linear_attention(q, k, v, log_a):
    b, l, h, dk = q.shape
    dv = v.shape[-1]
    n = l // CHUNK

    def chunks(t):
        return t.reshape(b, n, CHUNK, h, t.shape[-1]).transpose(0, 3, 1, 2, 4)

    q, k, v, log_a = chunks(q), chunks(k), chunks(v), chunks(log_a)
    cum = jnp.cumsum(log_a, axis=3)
    q_dec = q * jnp.exp(cum)
    k_inv = k * jnp.exp(-cum)
    causal = jnp.tril(jnp.ones((CHUNK, CHUNK), dtype=bool))
    scores = jnp.where(causal, jnp.einsum('bhnid,bhnjd->bhnij', q_dec, k_inv), 0.0)
    o_intra = jnp.einsum('bhnij,bhnjv->bhniv', scores, v)
    total = cum[:, :, :, -1]
    k_end = k * jnp.exp(total[:, :, :, None, :] - cum)
    d_state = jnp.einsum('bhncd,bhncv->nbhdv', k_end, v)
    decay = jnp.moveaxis(jnp.exp(total), 2, 0)

    def step(state, inp):
        ds, dec = inp
        return dec[..., None] * state + ds, state

    _, s_prev = lax.scan(step, jnp.zeros((b, h, dk, dv), q.dtype), (d_state, decay))
    o_inter = jnp.einsum('bhncd,nbhdv->bhncv', q_dec, s_prev)
    o = o_intra + o_inter
    return o.transpose(0, 2, 3, 1, 4).reshape(b, l, h, dv)


def s5_mixer(u, a_re, a_im, log_dt, b_re, b_im, c_re, c_im, d_skip, w_glu, b_glu):
    f32 = jnp.float32
    b, l, _ = u.shape
    uf = u.reshape(b, l, S5_GROUPS, S5_GROUP_DIM)
    lam = lax.complex(a_re.astype(f32), a_im.astype(f32))
    dt = jnp.exp(log_dt.astype(f32))[:, None]
    lam_bar = jnp.exp(lam * dt)
    b_mat = lax.complex(b_re.astype(f32), b_im.astype(f32))
    c_mat = lax.complex(c_re.astype(f32), c_im.astype(f32))
    b_bar = ((lam_bar - 1.0) / lam)[..., None] * b_mat
    bu = jnp.einsum('blgh,gph->blgp', uf.astype(jnp.complex64), b_bar)
    a_elems = jnp.broadcast_to(lam_bar, bu.shape)

    def combine(e1, e2):
        a1, x1 = e1
        a2, x2 = e2
        return a1 * a2, a2 * x1 + x2

    _, states = lax.associative_scan(combine, (a_elems, bu), axis=1)
    y = jnp.einsum('blgp,ghp->blgh', states, c_mat).real
    y = y + d_skip.astype(f32).reshape(S5_GROUPS, S5_GROUP_DIM) * uf
    y = jax.nn.gelu(y.reshape(b, l, S5_WIDTH))
    return y * jax.nn.sigmoid(y @ w_glu.astype(f32) + b_glu.astype(f32))


def token_mixer(h, w_in, gla_w_a2, gla_b_a, gla_norm_w, ret_norm_w, s5_a_re, s5_a_im,
                s5_log_dt, s5_b_re, s5_b_im, s5_c_re, s5_c_im, s5_d, s5_w_glu, s5_b_glu, w_out):
    f32 = jnp.float32
    b, l, _ = h.shape
    proj = (h @ w_in).astype(f32)
    points = np.cumsum(IN_SIZES)[:-1].tolist()
    gq, gk, gv, gg, ga, rq, rk, rv, rg, su = jnp.split(proj, points, axis=-1)

    log_a = jax.nn.log_sigmoid(ga @ gla_w_a2.astype(f32) + gla_b_a.astype(f32)) / GLA_GATE_TEMP
    o_gla = chunked_gated_linear_attention(
        gq.reshape(b, l, GLA_HEADS, GLA_DK) * GLA_DK ** -0.5,
        gk.reshape(b, l, GLA_HEADS, GLA_DK),
        gv.reshape(b, l, GLA_HEADS, GLA_DV),
        log_a.reshape(b, l, GLA_HEADS, GLA_DK))
    o_gla = head_rms_norm(o_gla, gla_norm_w).reshape(b, l, GLA_WIDTH) * jax.nn.silu(gg)

    pos = jnp.arange(l, dtype=f32)
    inv_freq = ROPE_BASE ** (-jnp.arange(0, RET_DK, 2, dtype=f32) / RET_DK)
    ang = pos[:, None] * inv_freq[None, :]
    cos, sin = jnp.cos(ang)[None, :, None, :], jnp.sin(ang)[None, :, None, :]
    q_r = rotary(rq.reshape(b, l, RET_HEADS, RET_DK), cos, sin)
    k_r = rotary(rk.reshape(b, l, RET_HEADS, RET_DK), cos, sin) * RET_DK ** -0.5
    log_gamma = jnp.log1p(-jnp.exp2(-5.0 - jnp.arange(RET_HEADS, dtype=f32)))
    log_decay = jnp.broadcast_to(log_gamma[None, None, :, None], (b, l, RET_HEADS, RET_DK))
    o_ret = chunked_gated_linear_attention(q_r, k_r, rv.reshape(b, l, RET_HEADS, RET_DV), log_decay)
    o_ret = head_group_norm(o_ret, ret_norm_w).reshape(b, l, RET_WIDTH) * jax.nn.silu(rg)

    o_s5 = s5_mixer(su, s5_a_re, s5_a_im, s5_log_dt, s5_b_re, s5_b_im, s5_c_re, s5_c_im,
                    s5_d, s5_w_glu, s5_b_glu)

    mixed = jnp.concatenate([o_gla, o_ret, o_s5], axis=-1).astype(h.dtype)
    return mixed @ w_out


def moe_ffn(h, router_w, router_b, w_up, b_up, w_down, b_down):
    b, l, d = h.shape
    t = b * l
    hf = h.reshape(t, d)
    logits = hf.astype(jnp.float32) @ router_w.astype(jnp.float32) + router_b.astype(jnp.float32)
    top_logits, top_idx = lax.top_k(logits, TOP_K)
    gates = jax.nn.softmax(top_logits, axis=-1)
    n_slots = t * TOP_K
    flat_e = top_idx.reshape(-1)
    order = jnp.argsort(flat_e)
    sorted_e = flat_e[order]
    counts = jnp.bincount(flat_e, length=N_EXPERTS)
    starts = jnp.cumsum(counts) - counts
    padded = (counts + EXPERT_BLOCK - 1) // EXPERT_BLOCK * EXPERT_BLOCK
    pad_ends = jnp.cumsum(padded)
    pad_starts = pad_ends - padded
    dest_sorted = pad_starts[sorted_e] + jnp.arange(n_slots) - starts[sorted_e]
    dest = jnp.zeros_like(dest_sorted).at[order].set(dest_sorted)
    n_blocks = n_slots // EXPERT_BLOCK + N_EXPERTS
    rows = jnp.zeros((n_blocks * EXPERT_BLOCK, d), h.dtype).at[dest].set(jnp.repeat(hf, TOP_K, axis=0))
    block_e = jnp.minimum(jnp.searchsorted(pad_ends, jnp.arange(n_blocks) * EXPERT_BLOCK, side='right'),
                          N_EXPERTS - 1)

    def expert_block(args):
        xb, e = args
        up = xb @ w_up[e] + b_up[e]
        x_glu, x_lin = jnp.split(up, 2, axis=-1)
        x_glu = jnp.minimum(x_glu, SWIGLU_LIMIT)
        x_lin = jnp.clip(x_lin, -SWIGLU_LIMIT, SWIGLU_LIMIT)
        act = x_glu * jax.nn.sigmoid(SWIGLU_ALPHA * x_glu) * (x_lin + 1.0)
        return act @ w_down[e] + b_down[e]

    out_rows = lax.map(expert_block, (rows.reshape(n_blocks, EXPERT_BLOCK, d), block_e)).reshape(-1, d)
    y = jnp.einsum('tkd,tk->td', out_rows[dest].reshape(t, TOP_K, d), gates.astype(h.dtype))
    return y.reshape(b, l, d)


def setup_inputs(seed: int = 0) -> dict:
    key = jax.random.key(seed)
    ks = iter(jax.random.split(key, 40))
    nrm = lambda shape, s: jax.random.normal(next(ks), shape, jnp.float32) * s
    gain = lambda shape: 1.0 + nrm(shape, 0.02)
    L = DEPTH
    d = D_MODEL
    a_im0 = jnp.pi * jnp.arange(S5_STATE, dtype=jnp.float32)
    return {
        'x': nrm((BATCH, SEQ, d), 1.0),
        'c': nrm((BATCH, d), 1.0),
        'norm1_w': gain((L, d)),
        'norm2_w': gain((L, d)),
        'w_mod': nrm((L, d, 6 * d), 0.5 * d ** -0.5),
        'b_mod': nrm((L, 6 * d), 0.02),
        'w_in': nrm((L, d, IN_COLS), d ** -0.5),
        'gla_w_a2': nrm((L, GLA_GATE_RANK, GLA_HEADS * GLA_DK), GLA_GATE_RANK ** -0.5),
        'gla_b_a': nrm((L, GLA_HEADS * GLA_DK), 0.1),
        'gla_norm_w': gain((L, GLA_WIDTH)),
        'ret_norm_w': gain((L, RET_WIDTH)),
        's5_a_re': -0.5 + nrm((L, S5_GROUPS, S5_STATE), 0.01),
        's5_a_im': a_im0 + nrm((L, S5_GROUPS, S5_STATE), 0.01),
        's5_log_dt': jax.random.uniform(next(ks), (L, S5_GROUPS), jnp.float32,
                                        math.log(1e-3), math.log(1e-1)),
        's5_b_re': nrm((L, S5_GROUPS, S5_STATE, S5_GROUP_DIM), (2.0 * S5_GROUP_DIM) ** -0.5),
        's5_b_im': nrm((L, S5_GROUPS, S5_STATE, S5_GROUP_DIM), (2.0 * S5_GROUP_DIM) ** -0.5),
        's5_c_re': nrm((L, S5_GROUPS, S5_GROUP_DIM, S5_STATE), S5_STATE ** -0.5),
        's5_c_im': nrm((L, S5_GROUPS, S5_GROUP_DIM, S5_STATE), S5_STATE ** -0.5),
        's5_d': nrm((L, S5_WIDTH), 1.0),
        's5_w_glu': nrm((L, S5_WIDTH, S5_WIDTH), S5_WIDTH ** -0.5),
        's5_b_glu': nrm((L, S5_WIDTH), 0.02),
        'w_out': nrm((L, D_MIX, d), D_MIX ** -0.5),
        'router_w': nrm((L, d, N_EXPERTS), d ** -0.5),
        'router_b': nrm((L, N_EXPERTS), 0.01),
        'w_up': nrm((L, N_EXPERTS, d, 2 * D_FF), d ** -0.5),
        'b_up': nrm((L, N_EXPERTS, 2 * D_FF), 0.02),
        'w_down': nrm((L, N_EXPERTS, D_FF, d), D_FF ** -0.5),
        'b_down': nrm((L, N_EXPERTS, d), 0.02),
        'final_norm_w': gain((d,)),
    }


def reference(x, c, norm1_w, norm2_w, w_mod, b_mod, w_in, gla_w_a2, gla_b_a, gla_norm_w,
              ret_norm_w, s5_a_re, s5_a_im, s5_log_dt, s5_b_re, s5_b_im, s5_c_re, s5_c_im,
              s5_d, s5_w_glu, s5_b_glu, w_out, router_w, router_b, w_up, b_up, w_down,
              b_down, final_norm_w):
    cond = jax.nn.silu(c)
    for i in range(DEPTH):
        mod = cond @ w_mod[i] + b_mod[i]
        sh1, sc1, g1, sh2, sc2, g2 = jnp.split(mod[:, None, :], 6, axis=-1)
        hdn = rms_norm(x, norm1_w[i]) * (1.0 + sc1) + sh1
        x = x + g1 * token_mixer(hdn, w_in[i], gla_w_a2[i], gla_b_a[i], gla_norm_w[i],
                                 ret_norm_w[i], s5_a_re[i], s5_a_im[i], s5_log_dt[i],
                                 s5_b_re[i], s5_b_im[i], s5_c_re[i], s5_c_im[i], s5_d[i],
                                 s5_w_glu[i], s5_b_glu[i], w_out[i])
        hdn = rms_norm(x, norm2_w[i]) * (1.0 + sc2) + sh2
        x = x + g2 * moe_ffn(hdn, router_w[i], router_b[i], w_up[i], b_up[i], w_down[i], b_down[i])
    return rms_norm(x, final_norm_w)
```

```python
import math
import numpy as np
from contextlib import ExitStack
import concourse.bass as bass
import concourse.mybir as mybir
from concourse.bass_utils import run_bass_kernel_spmd

F32 = mybir.dt.float32
BF16 = mybir.dt.bfloat16
I32 = mybir.dt.int32
U8 = mybir.dt.uint8
AF = mybir.ActivationFunctionType
ALU = mybir.AluOpType
AX = mybir.AxisListType

EPOCH = 16000
NDSEM = 8


class Prog:
    ENGS = ("pe", "act", "dve", "pool", "sp")

    def __init__(self, nc):
        self.nc = nc
        self.streams = {e: [] for e in self.ENGS}
        self.count = {e: 0 for e in self.ENGS}
        self.sems = {}
        self.dcount = {}
        self.dn = {e: 0 for e in self.ENGS}
        self.last_w = {}
        self.readers = {}
        self.known = {e: {} for e in self.ENGS}
        self._stack = None
        self.out_toks = []

    def _sem(self, key):
        d = self.sems
        if key not in d:
            d[key] = self._stack.enter_context(
                self.nc.semaphore("s_" + "_".join(str(x) for x in key)))
        return d[key]

    def _need(self, eng, tok):
        if tok is None:
            return None
        semkey, val = tok
        if semkey[0] == eng and eng == "pe" and semkey[1] != "d":
            return None
        k = self.known[eng]
        if k.get(semkey, 0) >= val:
            return None
        k[semkey] = val
        return (semkey, val)

    def _deps(self, eng, reads, writes):
        need = []
        for r in reads:
            t = self._need(eng, self.last_w.get(r))
            if t:
                need.append(t)
        for w in writes:
            t = self._need(eng, self.last_w.get(w))
            if t:
                need.append(t)
            for rt in self.readers.get(w, {}).items():
                t = self._need(eng, rt)
                if t:
                    need.append(t)
        best = {}
        for sk, v in need:
            best[sk] = max(best.get(sk, 0), v)
        return list(best.items())

    def _commit(self, tok, reads, writes):
        for r in reads:
            d = self.readers.setdefault(r, {})
            d[tok[0]] = max(d.get(tok[0], 0), tok[1])
        for w in writes:
            self.last_w[w] = tok
            self.readers[w] = {}

    def op(self, eng, fn, reads=(), writes=()):
        psr = [r for r in reads if isinstance(r, tuple) and r[0] == "ps"]
        if psr:
            reads = [r for r in reads if not (isinstance(r, tuple) and r[0] == "ps")]
            writes = list(writes) + [r for r in psr if r not in writes]
        deps = self._deps(eng, reads, writes)
        self.count[eng] += 1
        n = self.count[eng]
        ep = (n - 1) // EPOCH
        semkey = (eng, ep)
        tok = (semkey, n - ep * EPOCH)
        self._commit(tok, reads, writes)

        def emit(e, deps=deps, semkey=semkey, fn=fn):
            for sk, v in deps:
                e.wait_ge(self._sem(sk), v)
            fn(e).then_inc(self._sem(semkey), 1)
        self.streams[eng].append(emit)
        return tok

    def dma(self, q, out, in_, reads=(), writes=(), **kw):
        r = self.dn[q] % NDSEM
        self.dn[q] += 1
        semkey = (q, "d", r)
        prev = self.dcount.get(semkey, 0)
        deps = self._deps(q, reads, writes)
        if prev > 0:
            t = self._need(q, (semkey, 16 * prev))
            if t:
                deps.append(t)
        self.dcount[semkey] = prev + 1
        tok = (semkey, 16 * (prev + 1))
        self._commit(tok, reads, writes)

        def emit(e, deps=deps, semkey=semkey):
            for sk, v in deps:
                e.wait_ge(self._sem(sk), v)
            e.dma_start(out=out, in_=in_, **kw).then_inc(self._sem(semkey), 16)
        self.streams[q].append(emit)
        return tok

    def idma(self, fn, reads=(), writes=()):
        q = "pool"
        r = self.dn[q] % NDSEM
        self.dn[q] += 1
        semkey = (q, "d", r)
        prev = self.dcount.get(semkey, 0)
        deps = self._deps(q, reads, writes)
        if prev > 0:
            t = self._need(q, (semkey, 16 * prev))
            if t:
                deps.append(t)
        self.dcount[semkey] = prev + 1
        tok = (semkey, 16 * (prev + 1))
        self._commit(tok, reads, writes)

        def emit(e, deps=deps, semkey=semkey):
            for sk, v in deps:
                e.wait_ge(self._sem(sk), v)
            fn(e).then_inc(self._sem(semkey), 16)
        self.streams[q].append(emit)
        return tok

    def barrier(self):
        toks = []
        for e in self.ENGS:
            n = self.count[e]
            if n:
                ep = (n - 1) // EPOCH
                toks.append(((e, ep), n - ep * EPOCH))
        for k, v in self.dcount.items():
            toks.append((k, 16 * v))
        for e in self.ENGS:
            deps = []
            for t in toks:
                if t[0][0] == e and len(t[0]) == 2:
                    continue
                n = self._need(e, t)
                if n:
                    deps.append(n)

            def emit(eng, deps=deps):
                for sk, v in deps:
                    eng.wait_ge(self._sem(sk), v)
            self.streams[e].append(emit)

    def wait_all(self, eng, toks):
        deps = []
        for t in toks:
            n = self._need(eng, t)
            if n:
                deps.append(n)

        def emit(e, deps=deps):
            for sk, v in deps:
                e.wait_ge(self._sem(sk), v)
        self.streams[eng].append(emit)

    def run(self, stack):
        self._stack = stack
        nc = self.nc
        for e in self.ENGS:
            for ep in range((self.count[e] + EPOCH - 1) // EPOCH):
                self._sem((e, ep))
        for k in self.dcount:
            self._sem(k)
        block = stack.enter_context(nc.Block())
        S = self.streams

        @block.tensor
        def _(e):
            for f in S["pe"]:
                f(e)

        @block.scalar
        def _(e):
            for f in S["act"]:
                f(e)

        @block.vector
        def _(e):
            for f in S["dve"]:
                f(e)

        @block.gpsimd
        def _(e):
            for f in S["pool"]:
                f(e)

        @block.sync
        def _(e):
            for f in S["sp"]:
                f(e)


D = 1024
SEQ = 4096
NT = 32
KC = 8
NCOL = 2576
DEPTH = 2
NE = 32
EPS = 1e-5
G1 = (0, 400)
G2 = (400, 784)
G3 = (784, 1168)
R1 = (1168, 1552)
R2 = (1552, 1936)
R3 = (1936, 2320)
S1 = (2320, 2576)
TB = 1024
NTB = TB // 128


def make_consts():
    f = np.float32
    j = np.arange(128)
    c = {}
    maskT = (j[:, None] <= j[None, :]).astype(f)
    c["c_maskT"] = maskT
    c["c_triS"] = (maskT * (-1.0 / 16.0)).astype(f)
    c["c_revS"] = ((j[:, None] > j[None, :]).astype(f) * (-1.0 / 16.0)).astype(f)
    c["c_allS"] = np.full((128, 1), -1.0 / 16.0, f)
    sel = np.zeros((128, 128), f)
    sel[127, :] = 1.0
    c["c_sel127"] = sel
    c["c_ident"] = np.eye(128, dtype=f)
    pos = np.arange(SEQ, dtype=f)
    inv_freq = (10000.0 ** (-np.arange(0, 48, 2, dtype=f) / f(48))).astype(f)
    ang = (pos[:, None] * inv_freq[None, :]).astype(f).astype(np.float64)
    cos = np.cos(ang).astype(f).reshape(NT, 128, 24).transpose(1, 0, 2)
    sin = np.sin(ang).astype(f).reshape(NT, 128, 24).transpose(1, 0, 2)
    c["c_rope"] = np.ascontiguousarray(np.stack([cos, sin], axis=2))
    lg = np.log1p(-np.exp2(-5.0 - np.arange(4, dtype=np.float64)))
    cum = (j[:, None] + 1.0) * lg[None, :]
    tot = 128.0 * lg
    sc = 48.0 ** -0.5
    rep = lambda a: np.repeat(a, 48, axis=1).astype(f)
    c["c_retE"] = np.ascontiguousarray(np.stack(
        [rep(np.exp(cum)), rep(np.exp(-cum) * sc), rep(np.exp(tot[None, :] - cum) * sc)], axis=1))
    c["c_retdec"] = np.repeat(np.exp(tot)[None, :], 48, axis=0).astype(f)
    kc = np.stack([j + 1.0, -(j + 1.0)], axis=1).astype(f)
    c["c_kcol"] = kc
    c["c_stri"] = (j[:, None] < j[None, :]).astype(f)
    idxc = np.zeros((128, 73), f)
    idxc[:, 0:64] = np.arange(64)[None, :]
    idxc[:, 64:72] = np.arange(8)[None, :] * 128 + j[:, None]
    idxc[:, 72] = j
    c["c_idxc"] = idxc
    return c


def prep_weights(inp):
    f = np.float32
    w = {}
    win = inp["w_in"]
    w["w_in_r"] = np.ascontiguousarray(np.concatenate(
        [win[:, :, 0:384], win[:, :, 1152:1168], win[:, :, 384:1152], win[:, :, 1168:]], axis=2))
    w["w_out"] = inp["w_out"]
    w["w_mod"] = inp["w_mod"]
    w["b_mod"] = np.ascontiguousarray(inp["b_mod"].reshape(DEPTH, 1, 6 * D))
    w["nw"] = np.ascontiguousarray(np.stack([inp["norm1_w"], inp["norm2_w"]], axis=1).reshape(DEPTH, 1, 2 * D))
    w["fnw"] = np.ascontiguousarray(inp["final_norm_w"].reshape(1, D))
    w["wa2b"] = np.ascontiguousarray(np.concatenate([inp["gla_w_a2"], inp["gla_b_a"][:, None, :]], axis=1))
    w["gnw"] = np.ascontiguousarray(np.concatenate([inp["gla_norm_w"], inp["ret_norm_w"]], axis=1).reshape(DEPTH, 1, 768))
    ldt = np.repeat(inp["s5_log_dt"][:, :, None], 64, axis=2)
    w["s5rows"] = np.ascontiguousarray(np.concatenate(
        [inp["s5_a_re"].reshape(DEPTH, 1024), inp["s5_a_im"].reshape(DEPTH, 1024), ldt.reshape(DEPTH, 1024)],
        axis=1).reshape(DEPTH, 1, 3072))
    bblk = np.zeros((DEPTH, 256, 2048), f)
    cst = np.zeros((DEPTH, 128, 16, 16), f)
    for g in range(16):
        bblk[:, g * 16:(g + 1) * 16, g * 128:g * 128 + 64] = inp["s5_b_re"][:, g].transpose(0, 2, 1)
        bblk[:, g * 16:(g + 1) * 16, g * 128 + 64:g * 128 + 128] = inp["s5_b_im"][:, g].transpose(0, 2, 1)
        cst[:, 0:64, g, :] = inp["s5_c_re"][:, g].transpose(0, 2, 1)
        cst[:, 64:128, g, :] = inp["s5_c_im"][:, g].transpose(0, 2, 1)
    w["bblk"] = bblk
    w["cst"] = cst
    w["s5d"] = np.ascontiguousarray(inp["s5_d"].reshape(DEPTH, 1, 256))
    w["wglu"] = inp["s5_w_glu"]
    w["bglu"] = np.ascontiguousarray(inp["s5_b_glu"].reshape(DEPTH, 1, 256))
    w["router_w"] = inp["router_w"]
    w["router_b"] = np.ascontiguousarray(inp["router_b"].reshape(DEPTH, 1, NE))
    w["w_up"] = inp["w_up"]
    w["w_down"] = inp["w_down"]
    w["b_upT"] = np.ascontiguousarray(inp["b_up"].reshape(DEPTH, NE, 16, 128).transpose(0, 3, 1, 2))
    w["b_down"] = inp["b_down"]
    w["b_upT2"] = np.ascontiguousarray(inp["b_up"].reshape(DEPTH, NE, 16, 128).transpose(0, 1, 3, 2).reshape(DEPTH, NE * 128, 16))
    return w


def build(n_layers=DEPTH, n_tiles=NT, n_exp=NE, stop=None, stage=99):
    nc = bass.Bass("TRN2", target_bir_lowering=False)
    ins = {}

    def IN(name, shape):
        ins[name] = nc.dram_tensor(name, list(shape), F32, kind="ExternalInput").ap()
        return ins[name]

    x_in = IN("x", [SEQ, D])
    c_in = IN("c", [128, KC])
    w_in_r = IN("w_in_r", [DEPTH, D, NCOL])
    w_out = IN("w_out", [DEPTH, D, D])
    w_mod = IN("w_mod", [DEPTH, D, 6 * D])
    b_mod = IN("b_mod", [DEPTH, 1, 6 * D])
    nw = IN("nw", [DEPTH, 1, 2 * D])
    fnw = IN("fnw", [1, D])
    wa2b = IN("wa2b", [DEPTH, 17, 192])
    gnw = IN("gnw", [DEPTH, 1, 768])
    s5rows = IN("s5rows", [DEPTH, 1, 3072])
    bblk = IN("bblk", [DEPTH, 256, 2048])
    cst = IN("cst", [DEPTH, 128, 16, 16])
    s5d = IN("s5d", [DEPTH, 1, 256])
    wglu = IN("wglu", [DEPTH, 256, 256])
    bglu = IN("bglu", [DEPTH, 1, 256])
    router_w = IN("router_w", [DEPTH, D, NE])
    router_b = IN("router_b", [DEPTH, 1, NE])
    w_up = IN("w_up", [DEPTH, NE, D, 2 * D])
    w_down = IN("w_down", [DEPTH, NE, D, D])
    b_upT = IN("b_upT", [DEPTH, 128, NE, 16])
    b_down = IN("b_down", [DEPTH, NE, D])
    c_maskT = IN("c_maskT", [128, 128])
    c_triS = IN("c_triS", [128, 128])
    c_revS = IN("c_revS", [128, 128])
    c_allS = IN("c_allS", [128, 1])
    c_sel127 = IN("c_sel127", [128, 128])
    c_ident = IN("c_ident", [128, 128])
    c_rope = IN("c_rope", [128, NT, 2, 24])
    c_retE = IN("c_retE", [128, 3, 192])
    c_retdec = IN("c_retdec", [48, 4])
    c_kcol = IN("c_kcol", [128, 2])
    c_stri = IN("c_stri", [128, 128])
    c_idxc = IN("c_idxc", [128, 73])
    b_upT2 = IN("b_upT2", [DEPTH, NE * 128, 16])

    y_out = nc.dram_tensor("y", [SEQ, D], F32, kind="ExternalOutput").ap()
    xsA = nc.dram_tensor("xsA", [SEQ, D], F32).ap()
    xsB = nc.dram_tensor("xsB", [SEQ, D], F32).ap()
    NBLK = n_tiles * 128 * 4 // 512 + NE
    hd_dram = nc.dram_tensor("hd_dram", [SEQ, D], BF16).ap()
    rowsbuf = nc.dram_tensor("rowsbuf", [NBLK * 512, D], BF16).ap()
    orows = nc.dram_tensor("orows", [NBLK * 512, D], F32).ap()

    st = ExitStack()
    with st:
        p = Prog(nc)
        ARENA = 206 * 1024
        arena = st.enter_context(nc.sbuf_tensor("arena", [128, ARENA], U8))
        psum = st.enter_context(nc.psum_tensor("psum", [128, 4096], F32))
        ESZ = {F32: 4, BF16: 2, I32: 4}

        class Alloc:
            def __init__(self, base, limit):
                self.off = base
                self.limit = limit

            def __call__(self, name, shape, dt=F32, parts=128):
                n = int(np.prod(shape))
                nbytes = n * ESZ[dt]
                off = (self.off + 31) // 32 * 32
                self.off = off + nbytes
                assert self.off <= self.limit, (name, self.off, self.limit)
                v = arena[0:parts, off:off + nbytes].bitcast(dt)
                if len(shape) == 2:
                    v = v.rearrange("p (a b) -> p a b", a=shape[0])
                elif len(shape) == 3:
                    v = v.rearrange("p (a b c) -> p a b c", a=shape[0], b=shape[1])
                return v

        bank = lambda b: psum[:, b * 512:(b + 1) * 512]
        bankbf = lambda b: psum[:, b * 512:(b + 1) * 512].bitcast(BF16)
        B = lambda b: ("ps", b)
        rr = [0]

        def nextbank(lo=0, hi=3):
            b = lo + rr[0] % (hi - lo)
            rr[0] += 1
            return b

        def mm(out, lhsT, rhs, start, stop, R, W):
            return p.op("pe", lambda e: e.matmul(out, lhsT=lhsT, rhs=rhs, start=start, stop=stop), reads=R, writes=W)

        def tr(out, in_, idn, R, W):
            return p.op("pe", lambda e: e.transpose(out, in_, idn), reads=R, writes=W)

        def act(out, in_, func, R, W, **kw):
            return p.op("act", lambda e: e.activation(out, in_, func, **kw), reads=R, writes=W)

        def tt(eng, out, in0, in1, op, R, W):
            return p.op(eng, lambda e: e.tensor_tensor(out, in0, in1, op), reads=R, writes=W)

        def ts(eng, out, in0, s1, s2, op0, op1, R, W):
            if s2 is None:
                return p.op(eng, lambda e: e.tensor_scalar(out, in0, s1, None, op0), reads=R, writes=W)
            return p.op(eng, lambda e: e.tensor_scalar(out, in0, s1, s2, op0, op1), reads=R, writes=W)

        def stt(out, in0, scalar, in1, op0, op1, R, W, **kw):
            return p.op("dve", lambda e: e.scalar_tensor_tensor(out, in0, scalar, in1, op0, op1, **kw), reads=R, writes=W)

        def cp(eng, out, in_, R, W):
            if eng == "act":
                return p.op("act", lambda e: e.copy(out, in_), reads=R, writes=W)
            return p.op(eng, lambda e: e.tensor_copy(out, in_), reads=R, writes=W)

        A = Alloc(0, ARENA)
        ident32 = A("ident32", [128]); ident16 = A("ident16", [128], BF16)
        maskT = A("maskT", [128]); triS = A("triS", [128]); revS = A("revS", [128])
        allS = A("allS", [1]); sel127 = A("sel127", [128]); tri16 = A("tri16", [128], BF16)
        retE = A("retE", [3, 192]); retdec = A("retdec", [4]); kcol = A("kcol", [2])
        nh = A("nh", [4]); ones16 = A("ones16", [128], BF16); ones32 = A("ones32", [128])
        stri = A("stri", [128]); idxc = A("idxc", [73])
        condT = A("condT", [KC])
        modr = A("modr", [3 * D])
        gnwbc = A("gnwbc", [768]); dbc = A("dbc", [256])
        PERS_END = A.off

        for dst, src, nm in [(ident32, c_ident, "ident32"), (maskT, c_maskT, "maskT"), (triS, c_triS, "triS"),
                             (revS, c_revS, "revS"), (allS, c_allS, "allS"), (sel127, c_sel127, "sel127"),
                             (retE, c_retE, "retE"), (kcol, c_kcol, "kcol")]:
            p.dma("sp", dst, src, writes=[nm])
        p.dma("sp", retdec[0:48, :], c_retdec, writes=["retdec"])
        p.dma("sp", stri, c_stri, writes=["stri"])
        p.dma("sp", idxc, c_idxc, writes=["idxc"])
        p.dma("pool", ident16, c_ident, writes=["ident16"])
        p.dma("pool", tri16, c_maskT, writes=["tri16"])
        p.op("pool", lambda e: e.memset(nh, -0.5), writes=["nh"])
        p.op("pool", lambda e: e.memset(ones16, 1.0), writes=["ones16"])
        p.op("pool", lambda e: e.memset(ones32, 1.0), writes=["ones32"])
        ctmp = A("ctmp", [KC]); cth = A("cth", [KC])
        p.dma("sp", ctmp, c_in, writes=["ctmp"])
        act(cth, ctmp, AF.Tanh, ["ctmp"], ["cth"], scale=0.5)
        ts("dve", cth, cth, 1.0, 0.5, ALU.add, ALU.mult, ["cth"], ["cth"])
        tt("dve", condT, cth, ctmp, ALU.mult, ["cth", "ctmp"], ["condT"])
        PERS_END = A.off

        def compute_mod(l, which, Aa):
            condbc = Aa("condbc", [KC, 128])
            wbuf = [Aa("wmodbuf0", [KC, 256]), Aa("wmodbuf1", [KC, 256])]
            brow = [Aa("brow0", [256], parts=1), Aa("brow1", [256], parts=1)]
            nwbc = Aa("nwbc", [D])
            for k in range(KC):
                cp("dve", condbc[:, k, :], condT[:, k:k + 1].to_broadcast([128, 128]), ["condT"], [("condbc", k)])
            p.dma("sp", nwbc, nw[l, :, which * D:(which + 1) * D].partition_broadcast(128), writes=["nwbc"])
            for n in range(12):
                c0 = which * 3 * D + n * 256
                wb = wbuf[n % 2]
                wn = ("wmodbuf", n % 2)
                p.dma("sp", brow[n % 2][0:1, :], b_mod[l, :, c0:c0 + 256], writes=[("brow", n % 2)])
                for k in range(KC):
                    p.dma("sp" if k % 2 == 0 else "act", wb[:, k, :], w_mod[l, k * 128:(k + 1) * 128, c0:c0 + 256], writes=[(wn, k)])
                b = nextbank()
                for k in range(KC):
                    mm(bank(b)[:, 0:256], condbc[:, k, :], wb[:, k, :], k == 0, False, [("condbc", k), (wn, k)], [B(b)])
                mm(bank(b)[:, 0:256], ones32[0:1, :], brow[n % 2][0:1, :], False, True, ["ones32", ("brow", n % 2)], [B(b)])
                seg = n // 4
                q4 = n % 4
                sl = slice(q4 * 256, (q4 + 1) * 256)
                if seg == 0:
                    cp("act", modr[:, D + q4 * 256:D + (q4 + 1) * 256], bank(b)[:, 0:256], [B(b)], [("modr", 1)])
                elif seg == 1:
                    stt(modr[:, sl], bank(b)[:, 0:256], 1.0, nwbc[:, sl], ALU.add, ALU.mult, [B(b), "nwbc"], [("modr", 0)])
                else:
                    cp("act", modr[:, 2 * D + q4 * 256:2 * D + (q4 + 1) * 256], bank(b)[:, 0:256], [B(b)], [("modr", 2)])
        MODR = lambda s: [("modr", s)]

        def norm_mod(xt, xt_res, hdn_out, hdn_res, scr, scr_res, ss, rstd, ss_res="ss", rstd_res="rstd"):
            act(scr, xt, AF.Square, [xt_res], [scr_res, ss_res], accum_out=ss)
            ts("pool", rstd, ss, 1.0 / D, EPS, ALU.mult, ALU.add, [ss_res], [rstd_res])
            tt("pool", rstd, rstd, nh[:, 0:1], ALU.pow, [rstd_res, "nh"], [rstd_res])
            stt(scr, xt, rstd, modr[:, 0:D], ALU.mult, ALU.mult, [xt_res, rstd_res] + MODR(0), [scr_res])
            tt("pool", hdn_out, scr, modr[:, D:2 * D], ALU.add, [scr_res] + MODR(1), [hdn_res])

        def mixer_phase(l, src, dst):
            Aa = Alloc(PERS_END, ARENA)
            win16 = Aa("win16", [KC, NCOL], BF16)
            wout16 = Aa("wout16", [KC, D], BF16)
            bblk16 = Aa("bblk16", [2, 2048], BF16)
            cst16 = Aa("cst16", [16, 16], BF16)
            wglu16 = Aa("wglu16", [2, 256], BF16)
            bglu16 = Aa("bglu16", [256], BF16, parts=1)
            wa2b_sb = Aa("wa2b_sb", [192], parts=17)
            Tn_c = Aa("Tn_c", [1024]); Tn_s = Aa("Tn_s", [1024]); Tp_c = Aa("Tp_c", [1024]); Tp_s = Aa("Tp_s", [1024])
            MIX_W_END = Aa.off
            for k in range(KC):
                p.dma("pool", win16[:, k, :], w_in_r[l, k * 128:(k + 1) * 128, :], writes=[("win16", k)])
            for k in range(KC):
                p.dma("pool", wout16[:, k, :], w_out[l, k * 128:(k + 1) * 128, :], writes=[("wout16", k)])
            for k in range(2):
                p.dma("pool", bblk16[:, k, :], bblk[l, k * 128:(k + 1) * 128, :], writes=["bblk16"])
                p.dma("pool", wglu16[:, k, :], wglu[l, k * 128:(k + 1) * 128, :], writes=["wglu16"])
            p.dma("pool", bglu16[0:1, :], bglu[l], writes=["bglu16"])
            p.dma("sp", wa2b_sb[0:17, :], wa2b[l], writes=["wa2b"])
            p.dma("sp", gnwbc, gnw[l].partition_broadcast(128), writes=["gnwbc"])
            p.dma("sp", dbc, s5d[l].partition_broadcast(128), writes=["dbc"])
            ts("pool", gnwbc, gnwbc, 0.5 * math.sqrt(96.0), None, ALU.mult, None, ["gnwbc"], ["gnwbc"])

            At = Alloc(MIX_W_END, ARENA)
            cst32 = At("cst32", [16, 16])
            p.dma("sp", cst32, cst[l], writes=["cst32"])
            cp("dve", cst16[0:64], cst32[0:64], ["cst32"], ["cst16a"])
            ts("dve", cst16[64:128], cst32[64:128], -1.0, None, ALU.mult, None, ["cst32"], ["cst16b"])
            compute_mod(l, 0, At)
            p.barrier()
            At = Alloc(MIX_W_END, ARENA)
            rows = At("s5rows_sb", [3072])
            p.dma("sp", rows, s5rows[l].partition_broadcast(128), writes=["rows"])
            are = rows[:, 0:1024]; aim = rows[:, 1024:2048]; ldt = rows[:, 2048:3072]
            wre = At("wre", [1024]); wim = At("wim", [1024]); t0 = At("t0", [1024]); t1 = At("t1", [1024])
            t2 = At("t2", [1024]); t3 = At("t3", [1024]); ti = At("ti", [1024], I32)
            cr = At("cr", [1024]); ci = At("ci", [1024])
            act(t0, ldt, AF.Exp, ["rows"], ["t0"])
            tt("dve", wre, are, t0, ALU.mult, ["rows", "t0"], ["wre"])
            tt("dve", wim, aim, t0, ALU.mult, ["rows", "t0"], ["wim"])

            def sincos(ang_ap, ang_res, s_out, s_res, c_out, c_res):
                ts("dve", ti, ang_ap, 1.0 / (2 * math.pi), None, ALU.mult, None, [ang_res], ["ti"])
                cp("dve", t3, ti, ["ti"], ["t3"])
                stt(t3, t3, -2 * math.pi, ang_ap, ALU.mult, ALU.add, ["t3", ang_res], ["t3"])
                ts("dve", t3, t3, math.pi, -math.pi, ALU.min, ALU.max, ["t3"], ["t3"])
                act(s_out, t3, AF.Sin, ["t3"], [s_res])
                ts("dve", t3, t3, math.pi / 2, None, ALU.add, None, ["t3"], ["t3"])
                ts("dve", t2, t3, math.pi, 2 * math.pi, ALU.is_gt, ALU.mult, ["t3"], ["t2"])
                tt("dve", t3, t3, t2, ALU.subtract, ["t3", "t2"], ["t3"])
                ts("dve", t3, t3, math.pi, -math.pi, ALU.min, ALU.max, ["t3"], ["t3"])
                act(c_out, t3, AF.Sin, ["t3"], [c_res])

            m1 = At("m1", [1024]); c1 = At("c1", [1024]); s1 = At("s1", [1024])
            act(m1, wre, AF.Exp, ["wre"], ["m1"])
            sincos(wim, "wim", s1, "s1", c1, "c1")
            tt("dve", c1, c1, m1, ALU.mult, ["c1", "m1"], ["c1"])
            ts("dve", c1, c1, -1.0, None, ALU.add, None, ["c1"], ["c1"])
            tt("dve", s1, s1, m1, ALU.mult, ["s1", "m1"], ["s1"])
            tt("dve", t0, are, are, ALU.mult, ["rows"], ["t0"])
            tt("dve", t1, aim, aim, ALU.mult, ["rows"], ["t1"])
            tt("dve", t0, t0, t1, ALU.add, ["t0", "t1"], ["t0"])
            p.op("dve", lambda e: e.reciprocal(t0, t0), reads=["t0"], writes=["t0"])
            tt("dve", cr, c1, are, ALU.mult, ["c1", "rows"], ["cr"])
            tt("dve", t1, s1, aim, ALU.mult, ["s1", "rows"], ["t1"])
            tt("dve", cr, cr, t1, ALU.add, ["cr", "t1"], ["cr"])
            tt("dve", cr, cr, t0, ALU.mult, ["cr", "t0"], ["cr"])
            tt("dve", ci, s1, are, ALU.mult, ["s1", "rows"], ["ci"])
            tt("dve", t1, c1, aim, ALU.mult, ["c1", "rows"], ["t1"])
            tt("dve", ci, ci, t1, ALU.subtract, ["ci", "t1"], ["ci"])
            tt("dve", ci, ci, t0, ALU.mult, ["ci", "t0"], ["ci"])
            ang = m1; sn = s1; cs = c1; mp = At("mp", [1024]); mn = At("mn", [1024])
            act(mp, wre, AF.Exp, ["wre", "kcol"], ["mp"], scale=kcol[:, 0:1])
            act(mn, wre, AF.Exp, ["wre", "kcol"], ["mn"], scale=kcol[:, 1:2])
            ts("dve", ang, wim, kcol[:, 0:1], None, ALU.mult, None, ["wim", "kcol"], ["m1"])
            sincos(ang, "m1", sn, "s1", cs, "c1")
            tt("dve", Tp_c, mp, cs, ALU.mult, ["mp", "c1"], ["Tp_c"])
            tt("dve", Tp_s, mp, sn, ALU.mult, ["mp", "s1"], ["Tp_s"])
            tt("dve", t0, mn, cs, ALU.mult, ["mn", "c1"], ["t0"])
            tt("dve", t1, mn, sn, ALU.mult, ["mn", "s1"], ["t1"])
            tt("dve", Tn_c, t0, cr, ALU.mult, ["t0", "cr"], ["Tn_c"])
            tt("dve", t2, t1, ci, ALU.mult, ["t1", "ci"], ["t2"])
            tt("dve", Tn_c, Tn_c, t2, ALU.add, ["Tn_c", "t2"], ["Tn_c"])
            tt("dve", Tn_s, t0, ci, ALU.mult, ["t0", "ci"], ["Tn_s"])
            tt("dve", t2, t1, cr, ALU.mult, ["t1", "cr"], ["t2"])
            tt("dve", Tn_s, Tn_s, t2, ALU.subtract, ["Tn_s", "t2"], ["Tn_s"])
            p.barrier()

            Ab = Alloc(MIX_W_END, ARENA)
            xt = [Ab("xt0", [D]), Ab("xt1", [D])]
            F1 = Ab("F1", [D])
            hdn16 = Ab("hdn16", [D], BF16)
            hT16 = Ab("hT16", [KC, 128], BF16)
            mT16 = Ab("mT16", [KC, 128], BF16)
            mixed16 = Ab("mixed16", [D], BF16)
            ss = Ab("ss", [1]); rstd = Ab("rstd", [1])
            rope_sb2 = [Ab("rope_sb0", [2, 24]), Ab("rope_sb1", [2, 24])]
            gqk2 = [Ab("gqk0", [400]), Ab("gqk1", [400])]; rqk2 = [Ab("rqk0", [384]), Ab("rqk1", [384])]
            v162 = [[Ab("gv16_0", [384], BF16), Ab("rv16_0", [384], BF16)], [Ab("gv16_1", [384], BF16), Ab("rv16_1", [384], BF16)]]
            sgm2 = [[Ab("gsg0", [384]), Ab("rsg0", [384])], [Ab("gsg1", [384]), Ab("rsg1", [384])]]
            th = Ab("th", [384]); sq = [th, th]
            gaT = Ab("gaT", [128], parts=17)
            e1 = Ab("e1", [192]); sp_ = Ab("sp", [192])
            Eq = Ab("Eq", [192]); Ek = Ab("Ek", [192]); Eend = Ab("Eend", [192])
            dec = Ab("dec", [4], parts=48)
            qd16 = [Ab("qd16g", [192], BF16), Ab("qd16r", [192], BF16)]
            ki16 = [Ab("ki16g", [192], BF16), Ab("ki16r", [192], BF16)]
            ke16 = [Ab("ke16g", [192], BF16), Ab("ke16r", [192], BF16)]
            qkT16 = [Ab("qkT16g", [8, 128], BF16, parts=48), Ab("qkT16r", [8, 128], BF16, parts=48)]
            sc16 = [Ab("sc16g", [4, 128], BF16), Ab("sc16r", [4, 128], BF16)]
            S32 = [Ab("S32g", [4, 96], parts=48), Ab("S32r", [4, 96], parts=48)]
            S16 = [Ab("S16g", [4, 96], BF16, parts=48), Ab("S16r", [4, 96], BF16, parts=48)]
            o_sb = [Ab("o_sbg", [384]), Ab("o_sbr", [384])]
            st4 = [Ab("st4g", [4]), Ab("st4r", [4])]
            st4b = Ab("st4b", [4])
            rot = Ab("rot", [384]); ra = Ab("ra", [192]); rb = Ab("rb", [192])
            u_sb2 = [Ab("u_sb0", [256]), Ab("u_sb1", [256])]; u162 = [Ab("u16_0", [256], BF16), Ab("u16_1", [256], BF16)]
            uT16 = Ab("uT16", [2, 128], BF16)
            print("mixer SBUF end", Ab.off, ARENA)
            c1t2 = [Ab("c1t0", [512]), Ab("c1t1", [512])]; c2t2 = [Ab("c2t0", [512]), Ab("c2t1", [512])]
            z162 = [Ab("z16_0", [1024], BF16), Ab("z16_1", [1024], BF16)]
            s32 = [Ab("s32a", [2048]), Ab("s32b", [2048])]
            sT16 = Ab("sT16", [16, 128], BF16)
            ya = Ab("ya", [256]); yb = Ab("yb", [256]); yc = Ab("yc", [256]); gy16 = Ab("gy16", [256], BF16)
            gyT16 = Ab("gyT16", [2, 128], BF16)

            for i in range(2):
                p.op("pool", lambda e, i=i: e.memset(S32[i][0:48], 0.0), writes=[("S32", i)])
                p.op("pool", lambda e, i=i: e.memset(S16[i][0:48], 0.0), writes=[("S16", i)])
            p.op("pool", lambda e: e.memset(gaT[0:17, :], 1.0), writes=["gaT"])

            def mkrot(banks):
                st_ = [0]

                def nxt():
                    b_ = banks[st_[0] % len(banks)]
                    st_[0] += 1
                    return b_
                return nxt
            rotG = mkrot([0]); rotR = mkrot([2]); rotP = mkrot([1, 0])
            TB3 = 3

            def attn_core(mi, rotm, dec_ap, dec_res, nw_off, mix_off, centered, pp):
                QT = qkT16[mi]; QR = ("qkT16", mi)
                vv = v162[pp][mi]; VR = ("v16", pp, mi)
                bs = rotm()
                for h in range(4):
                    mm(bank(bs)[:, h * 128:(h + 1) * 128], QT[0:48, 4 + h, :], QT[0:48, h, :], True, True, [QR], [B(bs)])
                tt("dve", sc16[mi], bank(bs).rearrange("p (h i) -> p h i", h=4), maskT.unsqueeze(1).to_broadcast([128, 4, 128]),
                   ALU.mult, [B(bs), "maskT"], [("sc16", mi)])
                yield
                bo = rotm()
                for h in range(4):
                    mm(bank(bo)[:, h * 96:(h + 1) * 96], sc16[mi][:, h, :], vv[:, h * 96:(h + 1) * 96], True, False, [("sc16", mi), VR], [B(bo)])
                    mm(bank(bo)[:, h * 96:(h + 1) * 96], QT[0:48, h, :], S16[mi][0:48, h, :], False, True, [QR, ("S16", mi)], [B(bo)])
                O = o_sb[mi]; OR_ = ("o_sb", mi)
                cp("act", O, bank(bo)[:, 0:384], [B(bo)], [OR_])
                yield
                bd = rotm()
                for h in range(4):
                    mm(bank(bd)[0:48, h * 96:(h + 1) * 96], ke16[mi][:, h * 48:(h + 1) * 48], vv[:, h * 96:(h + 1) * 96], True, True, [("ke16", mi), VR], [B(bd)])
                tt("pool", S32[mi][0:48], S32[mi][0:48], dec_ap[0:48].unsqueeze(2).to_broadcast([48, 4, 96]), ALU.mult,
                   [("S32", mi), dec_res], [("S32", mi)])
                tt("dve", S32[mi][0:48], S32[mi][0:48], bank(bd)[0:48, 0:384].rearrange("p (h v) -> p h v", h=4), ALU.add,
                   [("S32", mi), B(bd)], [("S32", mi)])
                cp("act", S16[mi][0:48], S32[mi][0:48], [("S32", mi)], [("S16", mi)])
                yield
                o3 = O.rearrange("p (h v) -> p h v", h=4)
                if centered:
                    p.op("dve", lambda e: e.tensor_reduce(st4b, o3, AX.X, ALU.add), reads=[OR_], writes=["st4b"])
                    ts("pool", st4b, st4b, -1.0 / 96.0, None, ALU.mult, None, ["st4b"], ["st4b"])
                    tt("dve", o3, o3, st4b.unsqueeze(2).to_broadcast([128, 4, 96]), ALU.add, [OR_, "st4b"], [OR_])
                    yield
                SQ = sq[mi]; S4 = st4[mi]
                act(SQ, O, AF.Square, [OR_], ["th"])
                p.op("dve", lambda e: e.tensor_reduce(S4, SQ.rearrange("p (h v) -> p h v", h=4), AX.X, ALU.add), reads=["th"], writes=[("st4", mi)])
                ts("pool", S4, S4, 96.0 * EPS, None, ALU.add, None, [("st4", mi)], [("st4", mi)])
                tt("pool", S4, S4, nh, ALU.pow, [("st4", mi), "nh"], [("st4", mi)])
                yield
                tt("dve", o3, o3, S4.unsqueeze(2).to_broadcast([128, 4, 96]), ALU.mult, [OR_, ("st4", mi)], [OR_])
                tt("pool", mixed16[:, mix_off:mix_off + 384], O, sgm2[pp][mi], ALU.mult, [OR_, ("sg", pp, mi)], [("mixed16", mix_off)])

            def gate_prep(gbank, mi, nw_off, pp):
                act(th, bank(gbank)[:, 0:384], AF.Tanh, [B(gbank)], ["th"], scale=0.5)
                stt(sgm2[pp][mi], th, 1.0, bank(gbank)[:, 0:384], ALU.add, ALU.mult, ["th", B(gbank)], [("sg", pp, mi)])
                tt("pool", sgm2[pp][mi], sgm2[pp][mi], gnwbc[:, nw_off:nw_off + 384], ALU.mult, [("sg", pp, mi), "gnwbc"], [("sg", pp, mi)])

            def transposes_qk(mi):
                bt = TB3
                for h in range(4):
                    tr(bankbf(bt)[0:48, h * 128:(h + 1) * 128], qd16[mi][:, h * 48:(h + 1) * 48], ident16, [("qd16", mi), "ident16"], [B(bt)])
                    tr(bankbf(bt)[0:48, (4 + h) * 128:(5 + h) * 128], ki16[mi][:, h * 48:(h + 1) * 48], ident16, [("ki16", mi), "ident16"], [B(bt)])
                cp("act", qkT16[mi][0:48].rearrange("p a b -> p (a b)"), bankbf(bt)[0:48, :], [B(bt)], [("qkT16", mi)])

            def chain_gla(t):
                pp = t % 2
                gqk = gqk2[pp]; GQ = ("gqk", pp)
                bz = rotG()
                tr(bank(bz)[0:16, 0:128], gqk[:, 384:400], ident32, [GQ, "ident32"], [B(bz)])
                cp("act", gaT[0:16, :], bank(bz)[0:16, 0:128], [B(bz)], ["gaT"])
                yield
                bz2 = rotG()
                mm(bank(bz2)[:, 0:192], gaT[0:17, :], wa2b_sb[0:17, :], True, True, ["gaT", "wa2b"], [B(bz2)])
                act(e1, bank(bz2)[:, 0:192], AF.Exp, [B(bz2)], ["e1"], scale=-1.0)
                act(sp_, e1, AF.Ln, ["e1"], ["sp"], bias=1.0, scale=1.0)
                yield
                bc_ = rotG()
                mm(bank(bc_)[:, 0:192], triS, sp_, True, True, ["triS", "sp"], [B(bc_)])
                mm(bank(bc_)[:, 192:384], revS, sp_, True, True, ["revS", "sp"], [B(bc_)])
                for h in range(4):
                    mm(bank(bc_)[0:48, 384 + h:385 + h], sp_[:, h * 48:(h + 1) * 48], allS, True, True, ["sp", "allS"], [B(bc_)])
                act(Eq, bank(bc_)[:, 0:192], AF.Exp, [B(bc_)], ["Eq"])
                act(Ek, bank(bc_)[:, 0:192], AF.Exp, [B(bc_)], ["Ek"], scale=-1.0)
                act(Eend, bank(bc_)[:, 192:384], AF.Exp, [B(bc_)], ["Eend"])
                act(dec[0:48], bank(bc_)[0:48, 384:388], AF.Exp, [B(bc_)], ["dec"])
                yield
                stt(qd16[0], gqk[:, 0:192], 48.0 ** -0.5, Eq, ALU.mult, ALU.mult, [GQ, "Eq"], [("qd16", 0)])
                tt("pool", ki16[0], gqk[:, 192:384], Ek, ALU.mult, [GQ, "Ek"], [("ki16", 0)])
                tt("pool", ke16[0], gqk[:, 192:384], Eend, ALU.mult, [GQ, "Eend"], [("ke16", 0)])
                yield
                transposes_qk(0)
                yield
                yield from attn_core(0, rotG, dec, "dec", 0, 0, False, pp)

            def chain_ret(t):
                pp = t % 2
                rqk = rqk2[pp]; rope_sb = rope_sb2[pp]
                x4 = rqk.rearrange("p (a c d) -> p a c d", a=8, c=2)
                r4 = rot.rearrange("p (a c d) -> p a c d", a=8, c=2)
                cosb = rope_sb[:, 0, :].unsqueeze(1).to_broadcast([128, 8, 24])
                sinb = rope_sb[:, 1, :].unsqueeze(1).to_broadcast([128, 8, 24])
                ra3 = ra.rearrange("p (a d) -> p a d", a=8)
                rb3 = rb.rearrange("p (a d) -> p a d", a=8)
                tt("dve", ra3, x4[:, :, 0, :], cosb, ALU.mult, [("rqk", pp), ("rope_sb", pp)], ["ra"])
                tt("pool", rb3, x4[:, :, 1, :], sinb, ALU.mult, [("rqk", pp), ("rope_sb", pp)], ["rb"])
                tt("dve", r4[:, :, 0, :], ra3, rb3, ALU.subtract, ["ra", "rb"], ["rot"])
                yield
                tt("dve", ra3, x4[:, :, 0, :], sinb, ALU.mult, [("rqk", pp), ("rope_sb", pp)], ["ra"])
                tt("pool", rb3, x4[:, :, 1, :], cosb, ALU.mult, [("rqk", pp), ("rope_sb", pp)], ["rb"])
                tt("dve", r4[:, :, 1, :], ra3, rb3, ALU.add, ["ra", "rb"], ["rot"])
                yield
                tt("dve", qd16[1], rot[:, 0:192], retE[:, 0, :], ALU.mult, ["rot", "retE"], [("qd16", 1)])
                tt("pool", ki16[1], rot[:, 192:384], retE[:, 1, :], ALU.mult, ["rot", "retE"], [("ki16", 1)])
                tt("pool", ke16[1], rot[:, 192:384], retE[:, 2, :], ALU.mult, ["rot", "retE"], [("ke16", 1)])
                yield
                transposes_qk(1)
                yield
                yield from attn_core(1, rotR, retdec, "retdec", 384, 384, True, pp)

            Tnc = Tn_c.rearrange("p (g s) -> p g s", g=16); Tns = Tn_s.rearrange("p (g s) -> p g s", g=16)
            Tpc = Tp_c.rearrange("p (g s) -> p g s", g=16); Tps = Tp_s.rearrange("p (g s) -> p g s", g=16)
            def cmul(hf, src4, src_res, dst4, dst_res, Tc, Ts, Tres):
                c1v = c1t2[hf].rearrange("p (g s) -> p g s", g=8)
                c2v = c2t2[hf].rearrange("p (g s) -> p g s", g=8)
                C1 = ("c1t", hf); C2 = ("c2t", hf)
                tt("dve", c1v, src4[:, :, 0, :], Tc, ALU.mult, src_res + Tres, [C1])
                tt("dve", c2v, src4[:, :, 1, :], Ts, ALU.mult, src_res + Tres, [C2])
                tt("pool", dst4[:, :, 0, :], c1v, c2v, ALU.subtract, [C1, C2], [dst_res])
                tt("dve", c1v, src4[:, :, 0, :], Ts, ALU.mult, src_res + Tres, [C1])
                tt("dve", c2v, src4[:, :, 1, :], Tc, ALU.mult, src_res + Tres, [C2])
                tt("pool", dst4[:, :, 1, :], c1v, c2v, ALU.add, [C1, C2], [dst_res])

            def s5_half(t, hf):
                scur = s32[t % 2]
                sprev = s32[(t + 1) % 2]
                b0 = 6 if hf == 0 else 4
                BIG = [B(b0), B(b0 + 1)]
                big = psum[:, b0 * 512:(b0 + 2) * 512].rearrange("p (g c s) -> p g c s", g=8, c=2)
                z16 = z162[hf]; ZR = ("z16", hf)
                z4 = z16.rearrange("p (g c s) -> p g c s", g=8, c=2)
                c0 = hf * 1024
                SR = ("s32", t % 2, hf)
                for n in range(2):
                    mm(bank(b0 + n), uT16[:, hf, :], bblk16[:, hf, c0 + n * 512:c0 + (n + 1) * 512], True, True, ["uT16", "bblk16"], [B(b0 + n)])
                cmul(hf, big, BIG, z4, ZR, Tnc[:, hf * 8:(hf + 1) * 8, :], Tns[:, hf * 8:(hf + 1) * 8, :], ["Tn_c", "Tn_s"])
                yield
                for n in range(2):
                    mm(bank(b0 + n), tri16, z16[:, n * 512:(n + 1) * 512], True, t == 0, ["tri16", ZR], [B(b0 + n)])
                    if t > 0:
                        mm(bank(b0 + n), sel127, sprev[:, c0 + n * 512:c0 + (n + 1) * 512], False, True, ["sel127", ("s32", (t + 1) % 2, hf)], [B(b0 + n)])
                cmul(hf, big, BIG, scur[:, c0:c0 + 1024].rearrange("p (g c s) -> p g c s", g=8, c=2), SR,
                     Tpc[:, hf * 8:(hf + 1) * 8, :], Tps[:, hf * 8:(hf + 1) * 8, :], ["Tp_c", "Tp_s"])
                yield
                for g in range(8):
                    tr(bank(b0 + g // 4)[:, (g % 4) * 128:(g % 4 + 1) * 128], scur[:, c0 + g * 128:c0 + (g + 1) * 128], ident32,
                       [SR, "ident32"], [B(b0 + g // 4)])
                sTf = sT16.rearrange("p a b -> p (a b)")
                cp("act", sTf[:, c0:c0 + 512], bank(b0), [B(b0)], [("sT16", hf)])
                cp("dve", sTf[:, c0 + 512:c0 + 1024], bank(b0 + 1), [B(b0 + 1)], [("sT16", hf)])

            def chain_s5(t):
                pp = t % 2
                u_sb = u_sb2[pp]; u16 = u162[pp]
                bt = TB3
                for k in range(2):
                    tr(bankbf(bt)[:, k * 128:(k + 1) * 128], u16[:, k * 128:(k + 1) * 128], ident16, [("u16", pp), "ident16"], [B(bt)])
                cp("act", uT16.rearrange("p a b -> p (a b)"), bankbf(bt)[:, 0:256], [B(bt)], ["uT16"])
                yield
                halves = [s5_half(t, 0), s5_half(t, 1)]
                while halves:
                    for h_ in list(halves):
                        try:
                            next(h_)
                        except StopIteration:
                            halves.remove(h_)
                    yield
                by = 6
                for g in range(16):
                    mm(bank(by)[:, g * 16:(g + 1) * 16], sT16[:, g, :], cst16[:, g, :], True, True,
                       [("sT16", g // 8), "cst16a", "cst16b"], [B(by)])
                tt("pool", ya, u_sb, dbc, ALU.mult, [("u_sb", pp), "dbc"], ["ya"])
                tt("dve", ya, ya, bank(by)[:, 0:256], ALU.add, ["ya", B(by)], ["ya"])
                yield
                tt("pool", yb, ya, ya, ALU.mult, ["ya"], ["yb"])
                ts("pool", yb, yb, 0.044715, 1.0, ALU.mult, ALU.add, ["yb"], ["yb"])
                tt("dve", yb, yb, ya, ALU.mult, ["yb", "ya"], ["yb"])
                act(yc, yb, AF.Tanh, ["yb"], ["yc"], scale=math.sqrt(2.0 / math.pi))
                yield
                ts("pool", yc, yc, 1.0, 0.5, ALU.add, ALU.mult, ["yc"], ["yc"])
                tt("dve", ya, ya, yc, ALU.mult, ["ya", "yc"], ["ya"])
                cp("dve", gy16, ya, ["ya"], ["gy16"])
                yield
                for k in range(2):
                    tr(bankbf(bt)[:, k * 128:(k + 1) * 128], gy16[:, k * 128:(k + 1) * 128], ident16, ["gy16", "ident16"], [B(bt)])
                cp("act", gyT16.rearrange("p a b -> p (a b)"), bankbf(bt)[:, 0:256], [B(bt)], ["gyT16"])
                yield
                bgl = 7
                for k in range(2):
                    mm(bank(bgl)[:, 0:256], gyT16[:, k, :], wglu16[:, k, :], k == 0, False, ["gyT16", "wglu16"], [B(bgl)])
                mm(bank(bgl)[:, 0:256], ones16[0:1, :], bglu16[0:1, :], False, True, ["ones16", "bglu16"], [B(bgl)])
                act(yc, bank(bgl)[:, 0:256], AF.Tanh, [B(bgl)], ["yc"], scale=0.5)
                ts("pool", yc, yc, 1.0, 0.5, ALU.add, ALU.mult, ["yc"], ["yc"])
                tt("dve", mixed16[:, 768:1024], ya, yc, ALU.mult, ["ya", "yc"], [("mixed16", 768)])

            rotF = mkrot([1])

            def front(t):
                pp = t % 2
                X = xt[pp]; XR = ("xt", pp)
                p.dma("sp", X, src[t * 128:(t + 1) * 128, :], writes=[XR])
                p.dma("sp", rope_sb2[pp], c_rope[:, t], writes=[("rope_sb", pp)])
                norm_mod(X, XR, hdn16, "hdn16", F1, "F1", ss, rstd)
                yield
                bt = TB3
                for k in range(KC):
                    tr(bankbf(bt)[:, k * 128:(k + 1) * 128], hdn16[:, k * 128:(k + 1) * 128], ident16, ["hdn16", "ident16"], [B(bt)])
                cp("act", hT16.rearrange("p a b -> p (a b)"), bankbf(bt), [B(bt)], ["hT16"])
                yield

                def proj(cols):
                    b_ = rotF()
                    n = cols[1] - cols[0]
                    for k in range(KC):
                        mm(bank(b_)[:, 0:n], hT16[:, k, :], win16[:, k, cols[0]:cols[1]], k == 0, k == KC - 1, ["hT16", ("win16", k)], [B(b_)])
                    return b_

                b1 = proj(G1)
                cp("act", gqk2[pp], bank(b1)[:, 0:400], [B(b1)], [("gqk", pp)])
                yield
                b1 = proj(R1)
                cp("act", rqk2[pp], bank(b1)[:, 0:384], [B(b1)], [("rqk", pp)])
                yield
                b1 = proj(S1)
                cp("act", u_sb2[pp], bank(b1)[:, 0:256], [B(b1)], [("u_sb", pp)])
                cp("dve", u162[pp], bank(b1)[:, 0:256], [B(b1)], [("u16", pp)])
                yield
                b2 = proj(G2)
                cp("dve", v162[pp][0], bank(b2)[:, 0:384], [B(b2)], [("v16", pp, 0)])
                yield
                b2 = proj(R2)
                cp("dve", v162[pp][1], bank(b2)[:, 0:384], [B(b2)], [("v16", pp, 1)])
                yield
                bg = proj(G3)
                gate_prep(bg, 0, 0, pp)
                yield
                bg = proj(R3)
                gate_prep(bg, 1, 384, pp)

            for _ in front(0):
                pass
            for t in range(n_tiles):
                X = xt[t % 2]
                XR = ("xt", t % 2)
                bt = TB3
                gens = [chain_gla(t), chain_ret(t), chain_s5(t)]
                if t + 1 < n_tiles:
                    gens.append(front(t + 1))
                while gens:
                    for g_ in list(gens):
                        try:
                            next(g_)
                        except StopIteration:
                            gens.remove(g_)

                MX = [("mixed16", 0), ("mixed16", 384), ("mixed16", 768)]
                for k in range(KC):
                    tr(bankbf(bt)[:, k * 128:(k + 1) * 128], mixed16[:, k * 128:(k + 1) * 128], ident16, MX + ["ident16"], [B(bt)])
                cp("act", mT16.rearrange("p a b -> p (a b)"), bankbf(bt), [B(bt)], ["mT16"])
                for half in range(2):
                    b_ = rotP()
                    for k in range(KC):
                        mm(bank(b_), mT16[:, k, :], wout16[:, k, half * 512:(half + 1) * 512], k == 0, k == KC - 1, ["mT16", ("wout16", k)], [B(b_)])
                    tt("dve", F1[:, half * 512:(half + 1) * 512], bank(b_), modr[:, 2 * D + half * 512:2 * D + (half + 1) * 512], ALU.mult,
                       [B(b_), ("modr", 2)], ["F1"])
                tt("pool", F1, F1, X, ALU.add, ["F1", XR], ["F1"])
                tk = p.dma("sp", dst[t * 128:(t + 1) * 128, :], F1, reads=["F1"], writes=[("dst", t)])
                if stop == "mix":
                    p.out_toks.append(tk)
            p.barrier()

        def moe_phase(l, src, dst, final):
            Aa = Alloc(PERS_END, ARENA)
            wup = [Aa("wup0", [KC, 2 * D], BF16), Aa("wup1", [KC, 2 * D], BF16)]
            wdn = Aa("wdn", [KC, D], BF16)
            hT16 = Aa("mhT16", [KC, TB], BF16)
            acc = Aa("acc", [NTB, D])
            actT = Aa("actT", [KC, 512], BF16)
            rw32 = Aa("rw32", [KC, NE])
            rb32 = Aa("rb32", [NE], parts=1)
            bup = Aa("bup", [NE, 16])
            bup1 = Aa("bup1", [NE, 8])
            bdn = Aa("bdn", [D], parts=32)
            gates = Aa("gates", [NTB, NE])
            gT = Aa("gT", [128], parts=32)
            MOE_W_END = Aa.off
            for k in range(KC):
                p.dma("sp", rw32[:, k, :], router_w[l, k * 128:(k + 1) * 128, :], writes=["rw32"])
            p.dma("sp", rb32[0:1, :], router_b[l], writes=["rb32"])
            p.dma("sp", bup, b_upT[l], writes=["bup"])
            p.dma("sp", bdn[0:32, :], b_down[l], writes=["bdn"])
            ts("pool", bup1, bup[:, :, 8:16], 1.0, None, ALU.add, None, ["bup"], ["bup1"])
            At = Alloc(MOE_W_END, ARENA)
            if final:
                fnwbc = At("fnwbc", [D])
                p.dma("sp", fnwbc, fnw.partition_broadcast(128), writes=["fnwbc"])
            At2 = Alloc(At.off, ARENA)
            compute_mod(l, 1, At)
            p.barrier()
            Ab = At2
            xt = [Ab("mxt0", [D]), Ab("mxt1", [D])]
            F1 = Ab("mF1", [D]); F2 = Ab("mF2", [D])
            hT32 = Ab("hT32", [KC, 128])
            ss = Ab("mss", [1]); rstd = Ab("mrstd", [1])
            lg = Ab("lg", [NE]); mx8 = Ab("mx8", [8]); nm0 = Ab("nm0", [1]); ex = Ab("ex", [NE]); ssum = Ab("ssum", [1])
            gsb = Ab("gsb", [512]); sgb = Ab("sgb", [512]); lsb = Ab("lsb", [512])

            def load_expert(e, buf):
                for k in range(KC):
                    p.dma("pool", wup[buf][:, k, :], w_up[l, e, k * 128:(k + 1) * 128, :], writes=[("wup", buf, k)])

            def load_down(e):
                for k in range(KC):
                    p.dma("pool", wdn[:, k, :], w_down[l, e, k * 128:(k + 1) * 128, :], writes=[("wdn", k)])

            ntb = max(1, n_tiles // NTB)
            tiles_per_blk = min(NTB, n_tiles)
            for tb in range(ntb):
                load_expert(0, 0)
                load_down(0)
                for tl in range(tiles_per_blk):
                    t = tb * NTB + tl
                    X = xt[tl % 2]; XR = ("mxt", tl % 2)
                    p.dma("sp", X, src[t * 128:(t + 1) * 128, :], reads=[("dst", t)], writes=[XR])
                    if stage < 1:
                        continue
                    norm_mod(X, XR, F2, "mF2", F1, "mF1", ss, rstd)
                    if stage < 2:
                        continue
                    for hh in range(2):
                        b = nextbank()
                        for k4 in range(4):
                            k = hh * 4 + k4
                            tr(bank(b)[:, k4 * 128:(k4 + 1) * 128], F2[:, k * 128:(k + 1) * 128], ident32, ["mF2", "ident32"], [B(b)])
                        cp("act", hT32[:, hh * 4:(hh + 1) * 4, :].rearrange("p a b -> p (a b)"), bank(b), [B(b)], [("hT32", hh)])
                        for k4 in range(4):
                            k = hh * 4 + k4
                            cp("dve", hT16[:, k, tl * 128:(tl + 1) * 128], bank(b)[:, k4 * 128:(k4 + 1) * 128], [B(b)], [("mhT16", tl)])
                    if stage < 3:
                        continue
                    b = nextbank()
                    for k in range(KC):
                        mm(bank(b)[:, 0:NE], hT32[:, k, :], rw32[:, k, :], k == 0, False, [("hT32", k // 4), "rw32"], [B(b)])
                    mm(bank(b)[:, 0:NE], ones32[0:1, :], rb32[0:1, :], False, True, ["ones32", "rb32"], [B(b)])
                    cp("act", lg, bank(b)[:, 0:NE], [B(b)], ["lg"])
                    if stage < 4:
                        continue
                    p.op("dve", lambda e: e.max(out=mx8, in_=lg), reads=["lg"], writes=["mx8"])
                    ts("dve", nm0, mx8[:, 0:1], -1.0, None, ALU.mult, None, ["mx8"], ["nm0"])
                    act(ex, lg, AF.Exp, ["lg", "nm0"], ["ex"], bias=nm0, scale=1.0)
                    stt(ex, lg, mx8[:, 3:4], ex, ALU.is_ge, ALU.mult, ["lg", "mx8", "ex"], ["ex", "ssum"], accum_out=ssum)
                    p.op("dve", lambda e: e.reciprocal(ssum, ssum), reads=["ssum"], writes=["ssum"])
                    ts("dve", gates[:, tl, :], ex, ssum, None, ALU.mult, None, ["ex", "ssum"], [("gates", tl)])
                    if stage < 5:
                        continue
                    b = nextbank()
                    tr(bank(b)[0:32, 0:128], gates[:, tl, :], ident32, [("gates", tl), "ident32"], [B(b)])
                    cp("act", gT[0:32, :], bank(b)[0:32, 0:128], [B(b)], ["gT"])
                    for half in range(2):
                        b = nextbank()
                        mm(bank(b), gT[0:32, :], bdn[0:32, half * 512:(half + 1) * 512], True, True, ["gT", "bdn"], [B(b)])
                        cp("act", acc[:, tl, half * 512:(half + 1) * 512], bank(b), [B(b)], [("acc", tl, half)])
                nhalf = max(1, tiles_per_blk * 128 // 512)
                for e in range(n_exp):
                    buf = e % 2
                    if e + 1 < n_exp:
                        load_expert(e + 1, 1 - buf)
                    for hh in range(nhalf):
                        tok0 = hh * 512
                        ntok = min(512, tiles_per_blk * 128)
                        for cch in range(KC):
                            bg_ = nextbank(0, 8); bl_ = nextbank(0, 8)
                            for k in range(KC):
                                mm(bank(bg_)[:, 0:ntok], wup[buf][:, k, cch * 128:(cch + 1) * 128], hT16[:, k, tok0:tok0 + ntok], k == 0, k == KC - 1,
                                   [("wup", buf, k)] + [("mhT16", tok0 // 128 + i) for i in range(ntok // 128)], [B(bg_)])
                            for k in range(KC):
                                mm(bank(bl_)[:, 0:ntok], wup[buf][:, k, D + cch * 128:D + (cch + 1) * 128], hT16[:, k, tok0:tok0 + ntok], k == 0, k == KC - 1,
                                   [("wup", buf, k)] + [("mhT16", tok0 // 128 + i) for i in range(ntok // 128)], [B(bl_)])
                            ts("dve", gsb[:, 0:ntok], bank(bg_)[:, 0:ntok], bup[:, e, cch:cch + 1], 7.0, ALU.add, ALU.min, [B(bg_), "bup"], ["gsb"])
                            act(sgb[:, 0:ntok], gsb[:, 0:ntok], AF.Sigmoid, ["gsb"], ["sgb"], scale=1.702)
                            act(lsb[:, 0:ntok], bank(bl_)[:, 0:ntok], AF.Identity, [B(bl_), "bup1"], ["lsb"], bias=bup1[:, e, cch:cch + 1], scale=1.0)
                            ts("pool", lsb[:, 0:ntok], lsb[:, 0:ntok], 8.0, -6.0, ALU.min, ALU.max, ["lsb"], ["lsb"])
                            tt("pool", gsb[:, 0:ntok], gsb[:, 0:ntok], lsb[:, 0:ntok], ALU.mult, ["gsb", "lsb"], ["gsb"])
                            tt("dve", actT[:, cch, 0:ntok], sgb[:, 0:ntok], gsb[:, 0:ntok], ALU.mult, ["sgb", "gsb"], [("actT", cch)])
                        if hh == nhalf - 1 and e + 1 < n_exp:
                            pass
                        for tl4 in range(ntok // 128):
                            tl = hh * 4 + tl4
                            for half in range(2):
                                b = nextbank(0, 8)
                                for k in range(KC):
                                    mm(bank(b), actT[:, k, tl4 * 128:(tl4 + 1) * 128], wdn[:, k, half * 512:(half + 1) * 512], k == 0, k == KC - 1,
                                       [("actT", k), ("wdn", k)], [B(b)])
                                stt(acc[:, tl, half * 512:(half + 1) * 512], bank(b), gates[:, tl, e:e + 1], acc[:, tl, half * 512:(half + 1) * 512],
                                    ALU.mult, ALU.add, [B(b), ("gates", tl), ("acc", tl, half)], [("acc", tl, half)])
                    if e + 1 < n_exp:
                        load_down(e + 1)
                for tl in range(tiles_per_blk):
                    t = tb * NTB + tl
                    X = xt[tl % 2]; XR = ("mxt", tl % 2)
                    p.dma("sp", X, src[t * 128:(t + 1) * 128, :], reads=[("dst", t)], writes=[XR])
                    tt("dve", F1, acc[:, tl, :], modr[:, 2 * D:3 * D], ALU.mult, [("acc", tl, 0), ("acc", tl, 1)] + MODR(2), ["mF1"])
                    tt("pool", F1, F1, X, ALU.add, ["mF1", XR], ["mF1"])
                    if final:
                        act(F2, F1, AF.Square, ["mF1"], ["mF2", "ss"], accum_out=ss)
                        ts("pool", rstd, ss, 1.0 / D, EPS, ALU.mult, ALU.add, ["ss"], ["rstd"])
                        tt("pool", rstd, rstd, nh[:, 0:1], ALU.pow, ["rstd", "nh"], ["rstd"])
                        stt(F2, F1, rstd, fnwbc, ALU.mult, ALU.mult, ["mF1", "rstd", "fnwbc"], ["mF2"])
                        tk = p.dma("sp", dst[t * 128:(t + 1) * 128, :], F2, reads=["mF2"], writes=[("dst2", t)])
                    else:
                        tk = p.dma("sp", dst[t * 128:(t + 1) * 128, :], F1, reads=["mF1"], writes=[("dst2", t)])
                    if final or stop == "moe":
                        p.out_toks.append(tk)
            p.barrier()


        def moe_sparse(l, src, dst, final):
            nt = n_tiles
            Aa = Alloc(PERS_END, ARENA)
            wup = [Aa("wup0", [KC, 2 * D], BF16), Aa("wup1", [KC, 2 * D], BF16)]
            wdn = [Aa("wdn0", [KC, D], BF16), Aa("wdn1", [KC, D], BF16)]
            bupg = [Aa("bupg0", [16]), Aa("bupg1", [16])]
            bupl = [Aa("bupl0", [8]), Aa("bupl1", [8])]
            rw32 = Aa("rw32", [KC, NE])
            rb32 = Aa("rb32", [NE], parts=1)
            bdn = Aa("bdn", [D], parts=32)
            onehot = Aa("onehot", [nt, 4, NE], BF16)
            rank = Aa("rank", [nt, NE])
            g4 = Aa("g4", [nt, 4])
            desti = Aa("desti", [nt, 4], I32)
            widx = Aa("widx", [NBLK, 8], I32)
            bidx = Aa("bidx", [NBLK], I32)
            prevsum = Aa("prevsum", [NE])
            gT = Aa("gT", [128], parts=32)
            MOE_W_END = Aa.off
            for k in range(KC):
                p.dma("sp", rw32[:, k, :], router_w[l, k * 128:(k + 1) * 128, :], writes=["rw32"])
            p.dma("sp", rb32[0:1, :], router_b[l], writes=["rb32"])
            p.dma("sp", bdn[0:32, :], b_down[l], writes=["bdn"])
            At = Alloc(MOE_W_END, ARENA)
            if final:
                fnwbc = At("fnwbc", [D])
                p.dma("sp", fnwbc, fnw.partition_broadcast(128), writes=["fnwbc"])
            At2 = Alloc(At.off, ARENA)
            compute_mod(l, 1, At)
            p.barrier()
            Ab = At2
            M1_START = Ab.off
            W1 = 3
            hd16 = [Ab("hd16_%d" % i, [D], BF16) for i in range(W1)]
            HD_END = Ab.off
            SETS = []
            for i in range(W1):
                SETS.append(dict(
                    i=i, xt=Ab("mxt%d" % i, [D]), F1=Ab("mF1_%d" % i, [D]), F2=Ab("mF2_%d" % i, [D]), hT32=Ab("hT32_%d" % i, [KC, 128]),
                    ss=Ab("mss%d" % i, [1]), rstd=Ab("mrstd%d" % i, [1]), lg=Ab("lg%d" % i, [NE]), mx8=Ab("mx8_%d" % i, [8]),
                    nm0=Ab("nm0_%d" % i, [1]), ex4=Ab("ex4_%d" % i, [4]), ssum=Ab("ssum%d" % i, [1]), mask32=Ab("mask32_%d" % i, [NE])))
            xt = [SETS[0]["xt"], SETS[1]["xt"]]
            M1_END = Ab.off
            Ab = Alloc(M1_START, ARENA)
            gsb = Ab("gsb", [512]); sgb = Ab("sgb", [512]); lsb = Ab("lsb", [512])
            r16 = [Ab("r16a", [4, D], BF16), Ab("r16b", [4, D], BF16)]
            rT16 = [Ab("rT16a", [KC, 512], BF16), Ab("rT16b", [KC, 512], BF16)]
            actT = Ab("actT", [KC, 512], BF16)
            ysb = [Ab("ysb0", [D]), Ab("ysb1", [D])]

            def lockstep(gens):
                gens = list(gens)
                while gens:
                    for g_ in list(gens):
                        try:
                            next(g_)
                        except StopIteration:
                            gens.remove(g_)

            def m1_tile(t, S):
                i = S["i"]
                X = S["xt"]; XR = ("mxt", i)
                F1s = S["F1"]; F2s = S["F2"]; F1R = ("mF1", i); F2R = ("mF2", i)
                hT = S["hT32"]; lg = S["lg"]; mx8 = S["mx8"]; nm0 = S["nm0"]; ex4 = S["ex4"]; ssum = S["ssum"]; mask32 = S["mask32"]
                bk = [2 * i, 2 * i + 1]
                p.dma("sp", X, src[t * 128:(t + 1) * 128, :], writes=[XR])
                norm_mod(X, XR, F2s, F2R, F1s, F1R, S["ss"], S["rstd"], ("mss", i), ("mrstd", i))
                yield
                H = hd16[i]; HR = ("hd16", i)
                cp("pool", H, F2s, [F2R], [HR])
                p.dma("sp", hd_dram[t * 128:(t + 1) * 128, :], H, reads=[HR], writes=[("hd", t)])
                for hh in range(2):
                    b = bk[hh]
                    for k4 in range(4):
                        k = hh * 4 + k4
                        tr(bank(b)[:, k4 * 128:(k4 + 1) * 128], F2s[:, k * 128:(k + 1) * 128], ident32, [F2R, "ident32"], [B(b)])
                    cp("act", hT[:, hh * 4:(hh + 1) * 4, :].rearrange("p a b -> p (a b)"), bank(b), [B(b)], [("hT32", i, hh)])
                    yield
                b = bk[0]
                for k in range(KC):
                    mm(bank(b)[:, 0:NE], hT[:, k, :], rw32[:, k, :], k == 0, False, [("hT32", i, k // 4), "rw32"], [B(b)])
                mm(bank(b)[:, 0:NE], ones32[0:1, :], rb32[0:1, :], False, True, ["ones32", "rb32"], [B(b)])
                cp("act", lg, bank(b)[:, 0:NE], [B(b)], [("lg", i)])
                yield
                p.op("dve", lambda e: e.max(out=mx8, in_=lg), reads=[("lg", i)], writes=[("mx8", i)])
                ts("dve", nm0, mx8[:, 0:1], -1.0, None, ALU.mult, None, [("mx8", i)], [("nm0", i)])
                yield
                act(ex4, mx8[:, 0:4], AF.Exp, [("mx8", i), ("nm0", i)], [("ex4", i), ("ssum", i)], bias=nm0, scale=1.0, accum_out=ssum)
                for k in range(4):
                    ts("dve", onehot[:, t, k, :], lg, mx8[:, k:k + 1], None, ALU.is_equal, None, [("lg", i), ("mx8", i)], [("onehot", t)])
                p.op("dve", lambda e: e.tensor_reduce(mask32, onehot[:, t].rearrange("p k e -> p e k"), AX.X, ALU.add),
                     reads=[("onehot", t)], writes=[("mask32", i)])
                yield
                p.op("dve", lambda e: e.reciprocal(ssum, ssum), reads=[("ssum", i)], writes=[("ssum", i)])
                ts("dve", g4[:, t, :], ex4, ssum, None, ALU.mult, None, [("ex4", i), ("ssum", i)], [("g4", t)])
                yield
                b = bk[1]
                mm(bank(b)[:, 0:NE], stri, mask32, True, t == 0, ["stri", ("mask32", i)], [B(b)])
                if t > 0:
                    mm(bank(b)[:, 0:NE], ones32, prevsum, False, True, ["ones32", "prevsum"], [B(b)])
                cp("act", rank[:, t, :], bank(b)[:, 0:NE], [B(b)], [("rank", t)])
                if t == 0:
                    cp("dve", prevsum, mask32, [("mask32", i)], ["prevsum"])
                else:
                    tt("dve", prevsum, prevsum, mask32, ALU.add, ["prevsum", ("mask32", i)], ["prevsum"])

            ZR = [("wup", 0, k) for k in range(KC)]
            p.op("pool", lambda e: e.memset(wup[0].rearrange("p a b -> p (a b)"), 0.0), writes=ZR)
            nz = (NBLK * 512) // 2048
            ZF = []
            for i in range(nz):
                ZF.append(("rowsz", i))
                p.dma("sp", rowsbuf[i * 2048:(i + 1) * 2048, :].rearrange("(p a) n -> p (a n)", p=128), wup[0].rearrange("p a b -> p (a b)"),
                      reads=ZR, writes=[("rowsz", i)])
            for t0 in range(0, nt, W1):
                lockstep([m1_tile(t, SETS[t - t0]) for t in range(t0, min(nt, t0 + W1))])
            p.barrier()
            ONEHOT = [("onehot", t) for t in range(nt)]
            RANK = [("rank", t) for t in range(nt)]
            G4 = [("g4", t) for t in range(nt)]

            Ac = Alloc(HD_END, ARENA)
            cnt = Ac("cnt", [NE]); nb = Ac("nb", [NE]); nbi = Ac("nbi", [NE], I32); incl = Ac("incl", [NE]); pst = Ac("pst", [NE])
            onesr = Ac("onesr", [NE]); dest = Ac("dest", [nt, NE]); prod = Ac("prod", [nt, 4, NE]); destf = Ac("destf", [nt, 4])
            cmp_ = Ac("cmp", [NBLK, NE]); blke = Ac("blke", [NBLK]); wf = Ac("wf", [NBLK, 8]); bf_ = Ac("bf", [NBLK])
            b = nextbank()
            mm(bank(b)[:, 0:NE], ones32, prevsum, True, True, ["ones32", "prevsum"], [B(b)])
            cp("act", cnt, bank(b)[:, 0:NE], [B(b)], ["cnt"])
            ts("dve", nb, cnt, 511.0, 1.0 / 512.0, ALU.add, ALU.mult, ["cnt"], ["nb"])
            ts("dve", nbi, nb, -0.5 + 1.0 / 1024.0, None, ALU.add, None, ["nb"], ["nbi"])
            cp("dve", nb, nbi, ["nbi"], ["nb"])
            p.op("pool", lambda e: e.memset(onesr, 1.0), writes=["onesr"])
            p.op("dve", lambda e: e.tensor_tensor_scan(incl, onesr, nb, 0.0, ALU.mult, ALU.add), reads=["onesr", "nb"], writes=["incl"])
            tt("dve", pst, incl, nb, ALU.subtract, ["incl", "nb"], ["pst"])
            ts("dve", pst, pst, 512.0, None, ALU.mult, None, ["pst"], ["pst"])
            tt("dve", dest, rank, pst.unsqueeze(1).to_broadcast([128, nt, NE]), ALU.add, RANK + ["pst"], ["dest"])
            tt("dve", prod, onehot, dest.unsqueeze(2).to_broadcast([128, nt, 4, NE]), ALU.mult, ONEHOT + ["dest"], ["prod"])
            p.op("dve", lambda e: e.tensor_reduce(destf, prod, AX.X, ALU.add), reads=["prod"], writes=["destf"])
            cp("dve", desti, destf, ["destf"], ["desti"])
            NB_ = NBLK
            tt("dve", cmp_, incl.unsqueeze(1).to_broadcast([128, NB_, NE]), idxc[:, 0:NB_].unsqueeze(2).to_broadcast([128, NB_, NE]),
               ALU.is_le, ["incl", "idxc"], ["cmp"])
            p.op("dve", lambda e: e.tensor_reduce(blke, cmp_, AX.X, ALU.add), reads=["cmp"], writes=["blke"])
            ts("dve", blke, blke, float(NE - 1), None, ALU.min, None, ["blke"], ["blke"])
            ts("dve", bf_, blke, 128.0, idxc[:, 72:73], ALU.mult, ALU.add, ["blke", "idxc"], ["bf"])
            if l > 0:
                ts("dve", bf_, bf_, float(l * NE * 128), None, ALU.add, None, ["bf"], ["bf"])
            cp("dve", bidx, bf_, ["bf"], ["bidx"])
            ts("dve", blke, blke, 1024.0, float(l * NE * 1024), ALU.mult, ALU.add, ["blke", "bf"], ["blke"])
            tt("dve", wf, blke.unsqueeze(2).to_broadcast([128, NB_, 8]), idxc[:, 64:72].unsqueeze(1).to_broadcast([128, NB_, 8]),
               ALU.add, ["blke", "idxc"], ["wf"])
            cp("dve", widx, wf, ["wf"], ["widx"])

            SC = []
            for t in range(nt):
                H = hd16[t % 2]; HR = ("hd16", t % 2)
                p.dma("sp", H, hd_dram[t * 128:(t + 1) * 128, :], reads=[("hd", t)], writes=[HR])
                for k in range(4):
                    nm = ("rows", t, k)
                    SC.append(nm)
                    p.idma(lambda e, H=H, t=t, k=k: e.indirect_dma_start(
                        out=rowsbuf, out_offset=bass.IndirectOffsetOnAxis(ap=desti[:, t, k:k + 1], axis=0), in_=H, in_offset=None),
                        reads=[HR, "desti"] + ZF, writes=[nm])

            p.barrier()
            wup_d = w_up.rearrange("l e r n -> (l e r) n")
            wdn_d = w_down.rearrange("l e r n -> (l e r) n")
            bup_d = b_upT2.rearrange("l r c -> (l r) c")

            def load_block_w(j, buf):
                for k in range(KC):
                    p.idma(lambda e, j=j, k=k, buf=buf: e.indirect_dma_start(
                        out=wup[buf][:, k, :], out_offset=None, in_=wup_d, in_offset=bass.IndirectOffsetOnAxis(ap=widx[:, j, k:k + 1], axis=0)),
                        reads=["widx"], writes=[("wup", buf, k)])
                for k in range(KC):
                    p.idma(lambda e, j=j, k=k, buf=buf: e.indirect_dma_start(
                        out=wdn[buf][:, k, :], out_offset=None, in_=wdn_d, in_offset=bass.IndirectOffsetOnAxis(ap=widx[:, j, k:k + 1], axis=0)),
                        reads=["widx"], writes=[("wdn", buf, k)])
                p.idma(lambda e, j=j, buf=buf: e.indirect_dma_start(
                    out=bupg[buf], out_offset=None, in_=bup_d, in_offset=bass.IndirectOffsetOnAxis(ap=bidx[:, j:j + 1], axis=0)),
                    reads=["bidx"], writes=[("bupg", buf)])

            OR = []
            load_block_w(0, 0)

            def load_rows(j):
                rb = j % 2
                p.dma("sp", r16[rb], rowsbuf[j * 512:(j + 1) * 512, :].rearrange("(a p) n -> p a n", p=128), reads=SC, writes=[("r16", rb)])

            def transpose_rows(j):
                rb = j % 2
                for k2 in range(4):
                    bt = nextbank(0, 8)
                    for kk in range(2):
                        k = k2 * 2 + kk
                        for a in range(4):
                            tr(bankbf(bt)[:, kk * 512 + a * 128:kk * 512 + (a + 1) * 128], r16[rb][:, a, k * 128:(k + 1) * 128], ident16,
                               [("r16", rb), "ident16"], [B(bt)])
                    cp("act" if k2 % 2 == 0 else "dve", rT16[rb][:, k2 * 2:k2 * 2 + 2, :].rearrange("p a b -> p (a b)"), bankbf(bt), [B(bt)], [("rT16", rb, k2)])

            load_rows(0)
            transpose_rows(0)
            if NBLK > 1:
                load_rows(1)
            for j in range(NBLK):
                buf = j % 2
                rb = j % 2
                RT = rT16[rb]
                if j + 1 < NBLK:
                    load_block_w(j + 1, 1 - buf)
                ts("dve", bupl[buf], bupg[buf][:, 8:16], 1.0, None, ALU.add, None, [("bupg", buf)], [("bupl", buf)])
                for cch in range(KC):
                    bg_ = nextbank(0, 8); bl_ = nextbank(0, 8)
                    for k in range(KC):
                        mm(bank(bg_), wup[buf][:, k, cch * 128:(cch + 1) * 128], RT[:, k, :], k == 0, k == KC - 1, [("wup", buf, k), ("rT16", rb, k // 2)], [B(bg_)])
                    for k in range(KC):
                        mm(bank(bl_), wup[buf][:, k, D + cch * 128:D + (cch + 1) * 128], RT[:, k, :], k == 0, k == KC - 1, [("wup", buf, k), ("rT16", rb, k // 2)], [B(bl_)])
                    ts("dve", gsb, bank(bg_), bupg[buf][:, cch:cch + 1], 7.0, ALU.add, ALU.min, [B(bg_), ("bupg", buf)], ["gsb"])
                    act(sgb, gsb, AF.Sigmoid, ["gsb"], ["sgb"], scale=1.702)
                    act(lsb, bank(bl_), AF.Identity, [B(bl_), ("bupl", buf)], ["lsb"], bias=bupl[buf][:, cch:cch + 1], scale=1.0)
                    ts("dve", lsb, lsb, 8.0, -6.0, ALU.min, ALU.max, ["lsb"], ["lsb"])
                    tt("dve", gsb, gsb, lsb, ALU.mult, ["gsb", "lsb"], ["gsb"])
                    tt("dve", actT[:, cch, :], sgb, gsb, ALU.mult, ["sgb", "gsb"], [("actT", cch)])
                if j + 1 < NBLK:
                    transpose_rows(j + 1)
                if j + 2 < NBLK:
                    load_rows(j + 2)
                for a in range(4):
                    Y = ysb[a % 2]; YR = ("ysb", a % 2)
                    for half in range(2):
                        b = nextbank(0, 8)
                        for k in range(KC):
                            mm(bank(b), actT[:, k, a * 128:(a + 1) * 128], wdn[buf][:, k, half * 512:(half + 1) * 512], k == 0, k == KC - 1,
                               [("actT", k), ("wdn", buf, k)], [B(b)])
                        cp("act" if half == 0 else "dve", Y[:, half * 512:(half + 1) * 512], bank(b), [B(b)], [YR])
                    nm = ("orow", j, a)
                    OR.append(nm)
                    p.dma("sp", orows[j * 512 + a * 128:j * 512 + (a + 1) * 128, :], Y, reads=[YR], writes=[nm])

            p.barrier()
            Ad = Alloc(PERS_END, PERS_END + 64 * 1024)
            W5 = 2
            M5S = [dict(i=i, yk=Ad("yk%d" % i, [4, D]), gd=Ad("gd%d" % i, [NE]), accb=Ad("accb%d" % i, [D]), gT=Ad("gT5_%d" % i, [128], parts=32))
                   for i in range(W5)]

            def m5_tile(t, S5_):
                i = S5_["i"]
                yk = S5_["yk"]; gd = S5_["gd"]; accb = S5_["accb"]; gTs = S5_["gT"]
                S = SETS[i]
                X = S["xt"]; XR = ("mxt", i); F1s = S["F1"]; F2s = S["F2"]; F1R = ("mF1", i); F2R = ("mF2", i)
                ssx = S["ss"]; rs = S["rstd"]; SSR = ("mss", i); RSR = ("mrstd", i)
                bk = [2 * i, 2 * i + 1]
                for k in range(4):
                    p.idma(lambda e, t=t, k=k: e.indirect_dma_start(
                        out=yk[:, k, :], out_offset=None, in_=orows, in_offset=bass.IndirectOffsetOnAxis(ap=desti[:, t, k:k + 1], axis=0)),
                        reads=OR + ["desti"], writes=[("yk", i, k)])
                p.dma("sp", X, src[t * 128:(t + 1) * 128, :], writes=[XR])
                ts("dve", gd, onehot[:, t, 0, :], g4[:, t, 0:1], None, ALU.mult, None, [("onehot", t), ("g4", t)], [("gd", i)])
                for k in range(1, 4):
                    stt(gd, onehot[:, t, k, :], g4[:, t, k:k + 1], gd, ALU.mult, ALU.add, [("onehot", t), ("g4", t), ("gd", i)], [("gd", i)])
                yield
                b = bk[0]
                tr(bank(b)[0:32, 0:128], gd, ident32, [("gd", i), "ident32"], [B(b)])
                cp("act", gTs[0:32, :], bank(b)[0:32, 0:128], [B(b)], [("gT5", i)])
                yield
                for half in range(2):
                    b = bk[half]
                    mm(bank(b), gTs[0:32, :], bdn[0:32, half * 512:(half + 1) * 512], True, True, [("gT5", i), "bdn"], [B(b)])
                    cp("act", accb[:, half * 512:(half + 1) * 512], bank(b), [B(b)], [("accb", i)])
                yield
                for k in range(4):
                    stt(accb, yk[:, k, :], g4[:, t, k:k + 1], accb, ALU.mult, ALU.add, [("yk", i, k), ("g4", t), ("accb", i)], [("accb", i)])
                    if k % 2 == 1:
                        yield
                tt("dve", F1s, accb, modr[:, 2 * D:3 * D], ALU.mult, [("accb", i)] + MODR(2), [F1R])
                tt("pool", F1s, F1s, X, ALU.add, [F1R, XR], [F1R])
                yield
                if final:
                    act(F2s, F1s, AF.Square, [F1R], [F2R, SSR], accum_out=ssx)
                    ts("pool", rs, ssx, 1.0 / D, EPS, ALU.mult, ALU.add, [SSR], [RSR])
                    tt("pool", rs, rs, nh[:, 0:1], ALU.pow, [RSR, "nh"], [RSR])
                    yield
                    stt(F2s, F1s, rs, fnwbc, ALU.mult, ALU.mult, [F1R, RSR, "fnwbc"], [F2R])
                    tk = p.dma("sp", dst[t * 128:(t + 1) * 128, :], F2s, reads=[F2R], writes=[("dst2", t)])
                else:
                    tk = p.dma("sp", dst[t * 128:(t + 1) * 128, :], F1s, reads=[F1R], writes=[("dst2", t)])
                if final or stop == "moe":
                    p.out_toks.append(tk)

            for t0 in range(0, nt, W5):
                lockstep([m5_tile(t, M5S[t - t0]) for t in range(t0, min(nt, t0 + W5))])
            p.barrier()

        src_name = "x"
        cur = x_in
        for l in range(n_layers):
            last = (l == n_layers - 1)
            if stop == "mix" and last:
                mixer_phase(l, cur, y_out)
                break
            mixer_phase(l, cur, xsA)
            if last:
                moe_sparse(l, xsA, y_out, final=(stop is None))
            else:
                moe_sparse(l, xsA, xsB, final=False)
                cur = xsB
        p.wait_all("sp", p.out_toks)
        p.run(st)
    return nc


_CACHE = {}


def kernel(**inputs):
    inp = {k: np.asarray(v) for k, v in inputs.items()}
    consts = make_consts()
    w = prep_weights(inp)
    if "nc" not in _CACHE:
        _CACHE["nc"] = build()
    nc = _CACHE["nc"]
    in_maps = []
    for b in range(8):
        m = dict(w)
        m.update(consts)
        m["x"] = np.ascontiguousarray(inp["x"][b])
        m["c"] = np.ascontiguousarray(inp["c"][b].reshape(KC, 128).T)
        in_maps.append(m)
    res = run_bass_kernel_spmd(nc, in_maps, core_ids=list(range(8)))
    return np.stack([r["y"] for r in res.results], axis=0).astype(np.float32)
```

```python
import math
import numpy as np
from contextlib import ExitStack
import concourse.bass as bass
import concourse.mybir as mybir
from concourse.bass_utils import run_bass_kernel_spmd

F32 = mybir.dt.float32
BF16 = mybir.dt.bfloat16
I32 = mybir.dt.int32
U8 = mybir.dt.uint8
AF = mybir.ActivationFunctionType
ALU = mybir.AluOpType
AX = mybir.AxisListType

EPOCH = 16000
NDSEM = 8


class Prog:
    ENGS = ("pe", "act", "dve", "pool", "sp")

    def __init__(self, nc):
        self.nc = nc
        self.streams = {e: [] for e in self.ENGS}
        self.count = {e: 0 for e in self.ENGS}
        self.sems = {}
        self.dcount = {}
        self.dn = {e: 0 for e in self.ENGS}
        self.last_w = {}
        self.readers = {}
        self.known = {e: {} for e in self.ENGS}
        self._stack = None
        self.out_toks = []

    def _sem(self, key):
        d = self.sems
        if key not in d:
            d[key] = self._stack.enter_context(
                self.nc.semaphore("s_" + "_".join(str(x) for x in key)))
        return d[key]

    def _need(self, eng, tok):
        if tok is None:
            return None
        semkey, val = tok
        if semkey[0] == eng and eng == "pe" and semkey[1] != "d":
            return None
        k = self.known[eng]
        if k.get(semkey, 0) >= val:
            return None
        k[semkey] = val
        return (semkey, val)

    def _deps(self, eng, reads, writes):
        need = []
        for r in reads:
            t = self._need(eng, self.last_w.get(r))
            if t:
                need.append(t)
        for w in writes:
            t = self._need(eng, self.last_w.get(w))
            if t:
                need.append(t)
            for rt in self.readers.get(w, {}).items():
                t = self._need(eng, rt)
                if t:
                    need.append(t)
        best = {}
        for sk, v in need:
            best[sk] = max(best.get(sk, 0), v)
        return list(best.items())

    def _commit(self, tok, reads, writes):
        for r in reads:
            d = self.readers.setdefault(r, {})
            d[tok[0]] = max(d.get(tok[0], 0), tok[1])
        for w in writes:
            self.last_w[w] = tok
            self.readers[w] = {}

    def op(self, eng, fn, reads=(), writes=()):
        psr = [r for r in reads if isinstance(r, tuple) and r[0] == "ps"]
        if psr:
            reads = [r for r in reads if not (isinstance(r, tuple) and r[0] == "ps")]
            writes = list(writes) + [r for r in psr if r not in writes]
        deps = self._deps(eng, reads, writes)
        self.count[eng] += 1
        n = self.count[eng]
        ep = (n - 1) // EPOCH
        semkey = (eng, ep)
        tok = (semkey, n - ep * EPOCH)
        self._commit(tok, reads, writes)

        def emit(e, deps=deps, semkey=semkey, fn=fn):
            for sk, v in deps:
                e.wait_ge(self._sem(sk), v)
            fn(e).then_inc(self._sem(semkey), 1)
        self.streams[eng].append(emit)
        return tok

    def dma(self, q, out, in_, reads=(), writes=(), **kw):
        r = self.dn[q] % NDSEM
        self.dn[q] += 1
        semkey = (q, "d", r)
        prev = self.dcount.get(semkey, 0)
        deps = self._deps(q, reads, writes)
        if prev > 0:
            t = self._need(q, (semkey, 16 * prev))
            if t:
                deps.append(t)
        self.dcount[semkey] = prev + 1
        tok = (semkey, 16 * (prev + 1))
        self._commit(tok, reads, writes)

        def emit(e, deps=deps, semkey=semkey):
            for sk, v in deps:
                e.wait_ge(self._sem(sk), v)
            e.dma_start(out=out, in_=in_, **kw).then_inc(self._sem(semkey), 16)
        self.streams[q].append(emit)
        return tok

    def idma(self, fn, reads=(), writes=()):
        q = "pool"
        r = self.dn[q] % NDSEM
        self.dn[q] += 1
        semkey = (q, "d", r)
        prev = self.dcount.get(semkey, 0)
        deps = self._deps(q, reads, writes)
        if prev > 0:
            t = self._need(q, (semkey, 16 * prev))
            if t:
                deps.append(t)
        self.dcount[semkey] = prev + 1
        tok = (semkey, 16 * (prev + 1))
        self._commit(tok, reads, writes)

        def emit(e, deps=deps, semkey=semkey):
            for sk, v in deps:
                e.wait_ge(self._sem(sk), v)
            fn(e).then_inc(self._sem(semkey), 16)
        self.streams[q].append(emit)
        return tok

    def barrier(self):
        toks = []
        for e in self.ENGS:
            n = self.count[e]
            if n:
                ep = (n - 1) // EPOCH
                toks.append(((e, ep), n - ep * EPOCH))
        for k, v in self.dcount.items():
            toks.append((k, 16 * v))
        for e in self.ENGS:
            deps = []
            for t in toks:
                if t[0][0] == e and len(t[0]) == 2:
                    continue
                n = self._need(e, t)
                if n:
                    deps.append(n)

            def emit(eng, deps=deps):
                for sk, v in deps:
                    eng.wait_ge(self._sem(sk), v)
            self.streams[e].append(emit)

    def wait_all(self, eng, toks):
        deps = []
        for t in toks:
            n = self._need(eng, t)
            if n:
                deps.append(n)

        def emit(e, deps=deps):
            for sk, v in deps:
                e.wait_ge(self._sem(sk), v)
        self.streams[eng].append(emit)

    def run(self, stack):
        self._stack = stack
        nc = self.nc
        for e in self.ENGS:
            for ep in range((self.count[e] + EPOCH - 1) // EPOCH):
                self._sem((e, ep))
        for k in self.dcount:
            self._sem(k)
        block = stack.enter_context(nc.Block())
        S = self.streams

        @block.tensor
        def _(e):
            for f in S["pe"]:
                f(e)

        @block.scalar
        def _(e):
            for f in S["act"]:
                f(e)

        @block.vector
        def _(e):
            for f in S["dve"]:
                f(e)

        @block.gpsimd
        def _(e):
            for f in S["pool"]:
                f(e)

        @block.sync
        def _(e):
            for f in S["sp"]:
                f(e)


D = 1024
SEQ = 4096
NT = 32
KC = 8
NCOL = 2576
DEPTH = 2
NE = 32
EPS = 1e-5
G1 = (0, 400)
G2 = (400, 784)
G3 = (784, 1168)
R1 = (1168, 1552)
R2 = (1552, 1936)
R3 = (1936, 2320)
S1 = (2320, 2576)
TB = 1024
NTB = TB // 128


def make_consts():
    f = np.float32
    j = np.arange(128)
    c = {}
    maskT = (j[:, None] <= j[None, :]).astype(f)
    c["c_maskT"] = maskT
    c["c_triS"] = (maskT * (-1.0 / 16.0)).astype(f)
    c["c_revS"] = ((j[:, None] > j[None, :]).astype(f) * (-1.0 / 16.0)).astype(f)
    c["c_allS"] = np.full((128, 1), -1.0 / 16.0, f)
    sel = np.zeros((128, 128), f)
    sel[127, :] = 1.0
    c["c_sel127"] = sel
    c["c_ident"] = np.eye(128, dtype=f)
    pos = np.arange(SEQ, dtype=f)
    inv_freq = (10000.0 ** (-np.arange(0, 48, 2, dtype=f) / f(48))).astype(f)
    ang = (pos[:, None] * inv_freq[None, :]).astype(f).astype(np.float64)
    cos = np.cos(ang).astype(f).reshape(NT, 128, 24).transpose(1, 0, 2)
    sin = np.sin(ang).astype(f).reshape(NT, 128, 24).transpose(1, 0, 2)
    c["c_rope"] = np.ascontiguousarray(np.stack([cos, sin], axis=2))
    lg = np.log1p(-np.exp2(-5.0 - np.arange(4, dtype=np.float64)))
    cum = (j[:, None] + 1.0) * lg[None, :]
    tot = 128.0 * lg
    sc = 48.0 ** -0.5
    rep = lambda a: np.repeat(a, 48, axis=1).astype(f)
    c["c_retE"] = np.ascontiguousarray(np.stack(
        [rep(np.exp(cum)), rep(np.exp(-cum) * sc), rep(np.exp(tot[None, :] - cum) * sc)], axis=1))
    c["c_retdec"] = np.repeat(np.exp(tot)[None, :], 48, axis=0).astype(f)
    kc = np.stack([j + 1.0, -(j + 1.0)], axis=1).astype(f)
    c["c_kcol"] = kc
    c["c_stri"] = (j[:, None] < j[None, :]).astype(f)
    idxc = np.zeros((128, 73), f)
    idxc[:, 0:64] = np.arange(64)[None, :]
    idxc[:, 64:72] = np.arange(8)[None, :] * 128 + j[:, None]
    idxc[:, 72] = j
    c["c_idxc"] = idxc
    return c


def prep_weights(inp):
    f = np.float32
    w = {}
    win = inp["w_in"]
    w["w_in_r"] = np.ascontiguousarray(np.concatenate(
        [win[:, :, 0:384], win[:, :, 1152:1168], win[:, :, 384:1152], win[:, :, 1168:]], axis=2))
    w["w_out"] = inp["w_out"]
    w["w_mod"] = inp["w_mod"]
    w["b_mod"] = np.ascontiguousarray(inp["b_mod"].reshape(DEPTH, 1, 6 * D))
    w["nw"] = np.ascontiguousarray(np.stack([inp["norm1_w"], inp["norm2_w"]], axis=1).reshape(DEPTH, 1, 2 * D))
    w["fnw"] = np.ascontiguousarray(inp["final_norm_w"].reshape(1, D))
    w["wa2b"] = np.ascontiguousarray(np.concatenate([inp["gla_w_a2"], inp["gla_b_a"][:, None, :]], axis=1))
    w["gnw"] = np.ascontiguousarray(np.concatenate([inp["gla_norm_w"], inp["ret_norm_w"]], axis=1).reshape(DEPTH, 1, 768))
    ldt = np.repeat(inp["s5_log_dt"][:, :, None], 64, axis=2)
    w["s5rows"] = np.ascontiguousarray(np.concatenate(
        [inp["s5_a_re"].reshape(DEPTH, 1024), inp["s5_a_im"].reshape(DEPTH, 1024), ldt.reshape(DEPTH, 1024)],
        axis=1).reshape(DEPTH, 1, 3072))
    bblk = np.zeros((DEPTH, 256, 2048), f)
    cst = np.zeros((DEPTH, 128, 16, 16), f)
    for g in range(16):
        bblk[:, g * 16:(g + 1) * 16, g * 128:g * 128 + 64] = inp["s5_b_re"][:, g].transpose(0, 2, 1)
        bblk[:, g * 16:(g + 1) * 16, g * 128 + 64:g * 128 + 128] = inp["s5_b_im"][:, g].transpose(0, 2, 1)
        cst[:, 0:64, g, :] = inp["s5_c_re"][:, g].transpose(0, 2, 1)
        cst[:, 64:128, g, :] = inp["s5_c_im"][:, g].transpose(0, 2, 1)
    w["bblk"] = bblk
    w["cst"] = cst
    w["s5d"] = np.ascontiguousarray(inp["s5_d"].reshape(DEPTH, 1, 256))
    w["wglu"] = inp["s5_w_glu"]
    w["bglu"] = np.ascontiguousarray(inp["s5_b_glu"].reshape(DEPTH, 1, 256))
    w["router_w"] = inp["router_w"]
    w["router_b"] = np.ascontiguousarray(inp["router_b"].reshape(DEPTH, 1, NE))
    w["w_up"] = inp["w_up"]
    w["w_down"] = inp["w_down"]
    w["b_upT"] = np.ascontiguousarray(inp["b_up"].reshape(DEPTH, NE, 16, 128).transpose(0, 3, 1, 2))
    w["b_down"] = inp["b_down"]
    w["b_upT2"] = np.ascontiguousarray(inp["b_up"].reshape(DEPTH, NE, 16, 128).transpose(0, 1, 3, 2).reshape(DEPTH, NE * 128, 16))
    return w


def build(n_layers=DEPTH, n_tiles=NT, n_exp=NE, stop=None, stage=99):
    nc = bass.Bass("TRN2", target_bir_lowering=False)
    ins = {}

    def IN(name, shape):
        ins[name] = nc.dram_tensor(name, list(shape), F32, kind="ExternalInput").ap()
        return ins[name]

    x_in = IN("x", [SEQ, D])
    c_in = IN("c", [128, KC])
    w_in_r = IN("w_in_r", [DEPTH, D, NCOL])
    w_out = IN("w_out", [DEPTH, D, D])
    w_mod = IN("w_mod", [DEPTH, D, 6 * D])
    b_mod = IN("b_mod", [DEPTH, 1, 6 * D])
    nw = IN("nw", [DEPTH, 1, 2 * D])
    fnw = IN("fnw", [1, D])
    wa2b = IN("wa2b", [DEPTH, 17, 192])
    gnw = IN("gnw", [DEPTH, 1, 768])
    s5rows = IN("s5rows", [DEPTH, 1, 3072])
    bblk = IN("bblk", [DEPTH, 256, 2048])
    cst = IN("cst", [DEPTH, 128, 16, 16])
    s5d = IN("s5d", [DEPTH, 1, 256])
    wglu = IN("wglu", [DEPTH, 256, 256])
    bglu = IN("bglu", [DEPTH, 1, 256])
    router_w = IN("router_w", [DEPTH, D, NE])
    router_b = IN("router_b", [DEPTH, 1, NE])
    w_up = IN("w_up", [DEPTH, NE, D, 2 * D])
    w_down = IN("w_down", [DEPTH, NE, D, D])
    b_upT = IN("b_upT", [DEPTH, 128, NE, 16])
    b_down = IN("b_down", [DEPTH, NE, D])
    c_maskT = IN("c_maskT", [128, 128])
    c_triS = IN("c_triS", [128, 128])
    c_revS = IN("c_revS", [128, 128])
    c_allS = IN("c_allS", [128, 1])
    c_sel127 = IN("c_sel127", [128, 128])
    c_ident = IN("c_ident", [128, 128])
    c_rope = IN("c_rope", [128, NT, 2, 24])
    c_retE = IN("c_retE", [128, 3, 192])
    c_retdec = IN("c_retdec", [48, 4])
    c_kcol = IN("c_kcol", [128, 2])
    c_stri = IN("c_stri", [128, 128])
    c_idxc = IN("c_idxc", [128, 73])
    b_upT2 = IN("b_upT2", [DEPTH, NE * 128, 16])

    y_out = nc.dram_tensor("y", [SEQ, D], F32, kind="ExternalOutput").ap()
    xsA = nc.dram_tensor("xsA", [SEQ, D], F32).ap()
    xsB = nc.dram_tensor("xsB", [SEQ, D], F32).ap()
    NBLK = n_tiles * 128 * 4 // 512 + NE
    hd_dram = nc.dram_tensor("hd_dram", [SEQ, D], BF16).ap()
    rowsbuf = nc.dram_tensor("rowsbuf", [NBLK * 512, D], BF16).ap()
    orows = nc.dram_tensor("orows", [NBLK * 512, D], F32).ap()

    st = ExitStack()
    with st:
        p = Prog(nc)
        ARENA = 206 * 1024
        arena = st.enter_context(nc.sbuf_tensor("arena", [128, ARENA], U8))
        psum = st.enter_context(nc.psum_tensor("psum", [128, 4096], F32))
        ESZ = {F32: 4, BF16: 2, I32: 4}

        class Alloc:
            def __init__(self, base, limit):
                self.off = base
                self.limit = limit

            def __call__(self, name, shape, dt=F32, parts=128):
                n = int(np.prod(shape))
                nbytes = n * ESZ[dt]
                off = (self.off + 31) // 32 * 32
                self.off = off + nbytes
                assert self.off <= self.limit, (name, self.off, self.limit)
                v = arena[0:parts, off:off + nbytes].bitcast(dt)
                if len(shape) == 2:
                    v = v.rearrange("p (a b) -> p a b", a=shape[0])
                elif len(shape) == 3:
                    v = v.rearrange("p (a b c) -> p a b c", a=shape[0], b=shape[1])
                return v

        bank = lambda b: psum[:, b * 512:(b + 1) * 512]
        bankbf = lambda b: psum[:, b * 512:(b + 1) * 512].bitcast(BF16)
        B = lambda b: ("ps", b)
        rr = [0]

        def nextbank(lo=0, hi=3):
            b = lo + rr[0] % (hi - lo)
            rr[0] += 1
            return b

        def mm(out, lhsT, rhs, start, stop, R, W):
            return p.op("pe", lambda e: e.matmul(out, lhsT=lhsT, rhs=rhs, start=start, stop=stop), reads=R, writes=W)

        def tr(out, in_, idn, R, W):
            return p.op("pe", lambda e: e.transpose(out, in_, idn), reads=R, writes=W)

        def act(out, in_, func, R, W, **kw):
            return p.op("act", lambda e: e.activation(out, in_, func, **kw), reads=R, writes=W)

        def tt(eng, out, in0, in1, op, R, W):
            return p.op(eng, lambda e: e.tensor_tensor(out, in0, in1, op), reads=R, writes=W)

        def ts(eng, out, in0, s1, s2, op0, op1, R, W):
            if s2 is None:
                return p.op(eng, lambda e: e.tensor_scalar(out, in0, s1, None, op0), reads=R, writes=W)
            return p.op(eng, lambda e: e.tensor_scalar(out, in0, s1, s2, op0, op1), reads=R, writes=W)

        def stt(out, in0, scalar, in1, op0, op1, R, W, **kw):
            return p.op("dve", lambda e: e.scalar_tensor_tensor(out, in0, scalar, in1, op0, op1, **kw), reads=R, writes=W)

        def cp(eng, out, in_, R, W):
            if eng == "act":
                return p.op("act", lambda e: e.copy(out, in_), reads=R, writes=W)
            return p.op(eng, lambda e: e.tensor_copy(out, in_), reads=R, writes=W)

        A = Alloc(0, ARENA)
        ident32 = A("ident32", [128]); ident16 = A("ident16", [128], BF16)
        maskT = A("maskT", [128]); triS = A("triS", [128]); revS = A("revS", [128])
        allS = A("allS", [1]); sel127 = A("sel127", [128]); tri16 = A("tri16", [128], BF16)
        retE = A("retE", [3, 192]); retdec = A("retdec", [4]); kcol = A("kcol", [2])
        nh = A("nh", [4]); ones16 = A("ones16", [128], BF16); ones32 = A("ones32", [128])
        stri = A("stri", [128]); idxc = A("idxc", [73])
        condT = A("condT", [KC])
        modr = A("modr", [3 * D])
        gnwbc = A("gnwbc", [768]); dbc = A("dbc", [256])
        PERS_END = A.off

        for dst, src, nm in [(ident32, c_ident, "ident32"), (maskT, c_maskT, "maskT"), (triS, c_triS, "triS"),
                             (revS, c_revS, "revS"), (allS, c_allS, "allS"), (sel127, c_sel127, "sel127"),
                             (retE, c_retE, "retE"), (kcol, c_kcol, "kcol")]:
            p.dma("sp", dst, src, writes=[nm])
        p.dma("sp", retdec[0:48, :], c_retdec, writes=["retdec"])
        p.dma("sp", stri, c_stri, writes=["stri"])
        p.dma("sp", idxc, c_idxc, writes=["idxc"])
        p.dma("pool", ident16, c_ident, writes=["ident16"])
        p.dma("pool", tri16, c_maskT, writes=["tri16"])
        p.op("pool", lambda e: e.memset(nh, -0.5), writes=["nh"])
        p.op("pool", lambda e: e.memset(ones16, 1.0), writes=["ones16"])
        p.op("pool", lambda e: e.memset(ones32, 1.0), writes=["ones32"])
        ctmp = A("ctmp", [KC]); cth = A("cth", [KC])
        p.dma("sp", ctmp, c_in, writes=["ctmp"])
        act(cth, ctmp, AF.Tanh, ["ctmp"], ["cth"], scale=0.5)
        ts("dve", cth, cth, 1.0, 0.5, ALU.add, ALU.mult, ["cth"], ["cth"])
        tt("dve", condT, cth, ctmp, ALU.mult, ["cth", "ctmp"], ["condT"])
        PERS_END = A.off

        def compute_mod(l, which, Aa):
            condbc = Aa("condbc", [KC, 128])
            wbuf = [Aa("wmodbuf0", [KC, 256]), Aa("wmodbuf1", [KC, 256])]
            brow = [Aa("brow0", [256], parts=1), Aa("brow1", [256], parts=1)]
            nwbc = Aa("nwbc", [D])
            for k in range(KC):
                cp("dve", condbc[:, k, :], condT[:, k:k + 1].to_broadcast([128, 128]), ["condT"], [("condbc", k)])
            p.dma("sp", nwbc, nw[l, :, which * D:(which + 1) * D].partition_broadcast(128), writes=["nwbc"])
            for n in range(12):
                c0 = which * 3 * D + n * 256
                wb = wbuf[n % 2]
                wn = ("wmodbuf", n % 2)
                p.dma("sp", brow[n % 2][0:1, :], b_mod[l, :, c0:c0 + 256], writes=[("brow", n % 2)])
                for k in range(KC):
                    p.dma("sp" if k % 2 == 0 else "act", wb[:, k, :], w_mod[l, k * 128:(k + 1) * 128, c0:c0 + 256], writes=[(wn, k)])
                b = nextbank()
                for k in range(KC):
                    mm(bank(b)[:, 0:256], condbc[:, k, :], wb[:, k, :], k == 0, False, [("condbc", k), (wn, k)], [B(b)])
                mm(bank(b)[:, 0:256], ones32[0:1, :], brow[n % 2][0:1, :], False, True, ["ones32", ("brow", n % 2)], [B(b)])
                seg = n // 4
                q4 = n % 4
                sl = slice(q4 * 256, (q4 + 1) * 256)
                if seg == 0:
                    cp("act", modr[:, D + q4 * 256:D + (q4 + 1) * 256], bank(b)[:, 0:256], [B(b)], [("modr", 1)])
                elif seg == 1:
                    stt(modr[:, sl], bank(b)[:, 0:256], 1.0, nwbc[:, sl], ALU.add, ALU.mult, [B(b), "nwbc"], [("modr", 0)])
                else:
                    cp("act", modr[:, 2 * D + q4 * 256:2 * D + (q4 + 1) * 256], bank(b)[:, 0:256], [B(b)], [("modr", 2)])
        MODR = lambda s: [("modr", s)]

        def norm_mod(xt, xt_res, hdn_out, hdn_res, scr, scr_res, ss, rstd, ss_res="ss", rstd_res="rstd"):
            act(scr, xt, AF.Square, [xt_res], [scr_res, ss_res], accum_out=ss)
            ts("pool", rstd, ss, 1.0 / D, EPS, ALU.mult, ALU.add, [ss_res], [rstd_res])
            tt("pool", rstd, rstd, nh[:, 0:1], ALU.pow, [rstd_res, "nh"], [rstd_res])
            stt(scr, xt, rstd, modr[:, 0:D], ALU.mult, ALU.mult, [xt_res, rstd_res] + MODR(0), [scr_res])
            tt("pool", hdn_out, scr, modr[:, D:2 * D], ALU.add, [scr_res] + MODR(1), [hdn_res])

        def mixer_phase(l, src, dst):
            Aa = Alloc(PERS_END, ARENA)
            win16 = Aa("win16", [KC, NCOL], BF16)
            wout16 = Aa("wout16", [KC, D], BF16)
            bblk16 = Aa("bblk16", [2, 2048], BF16)
            cst16 = Aa("cst16", [16, 16], BF16)
            wglu16 = Aa("wglu16", [2, 256], BF16)
            bglu16 = Aa("bglu16", [256], BF16, parts=1)
            wa2b_sb = Aa("wa2b_sb", [192], parts=17)
            Tn_c = Aa("Tn_c", [1024]); Tn_s = Aa("Tn_s", [1024]); Tp_c = Aa("Tp_c", [1024]); Tp_s = Aa("Tp_s", [1024])
            MIX_W_END = Aa.off
            for k in range(KC):
                p.dma("pool", win16[:, k, :], w_in_r[l, k * 128:(k + 1) * 128, :], writes=[("win16", k)])
            for k in range(KC):
                p.dma("pool", wout16[:, k, :], w_out[l, k * 128:(k + 1) * 128, :], writes=[("wout16", k)])
            for k in range(2):
                p.dma("pool", bblk16[:, k, :], bblk[l, k * 128:(k + 1) * 128, :], writes=["bblk16"])
                p.dma("pool", wglu16[:, k, :], wglu[l, k * 128:(k + 1) * 128, :], writes=["wglu16"])
            p.dma("pool", bglu16[0:1, :], bglu[l], writes=["bglu16"])
            p.dma("sp", wa2b_sb[0:17, :], wa2b[l], writes=["wa2b"])
            p.dma("sp", gnwbc, gnw[l].partition_broadcast(128), writes=["gnwbc"])
            p.dma("sp", dbc, s5d[l].partition_broadcast(128), writes=["dbc"])
            ts("pool", gnwbc, gnwbc, 0.5 * math.sqrt(96.0), None, ALU.mult, None, ["gnwbc"], ["gnwbc"])

            At = Alloc(MIX_W_END, ARENA)
            cst32 = At("cst32", [16, 16])
            p.dma("sp", cst32, cst[l], writes=["cst32"])
            cp("dve", cst16[0:64], cst32[0:64], ["cst32"], ["cst16a"])
            ts("dve", cst16[64:128], cst32[64:128], -1.0, None, ALU.mult, None, ["cst32"], ["cst16b"])
            compute_mod(l, 0, At)
            At = Alloc(At.off, ARENA)
            rows = At("s5rows_sb", [3072])
            p.dma("sp", rows, s5rows[l].partition_broadcast(128), writes=["rows"])
            are = rows[:, 0:1024]; aim = rows[:, 1024:2048]; ldt = rows[:, 2048:3072]
            wre = At("wre", [1024]); wim = At("wim", [1024]); t0 = At("t0", [1024]); t1 = At("t1", [1024])
            t2 = At("t2", [1024]); t3 = At("t3", [1024]); ti = At("ti", [1024], I32)
            cr = At("cr", [1024]); ci = At("ci", [1024])
            act(t0, ldt, AF.Exp, ["rows"], ["t0"])
            tt("dve", wre, are, t0, ALU.mult, ["rows", "t0"], ["wre"])
            tt("dve", wim, aim, t0, ALU.mult, ["rows", "t0"], ["wim"])

            def sincos(ang_ap, ang_res, s_out, s_res, c_out, c_res):
                ts("dve", ti, ang_ap, 1.0 / (2 * math.pi), None, ALU.mult, None, [ang_res], ["ti"])
                cp("dve", t3, ti, ["ti"], ["t3"])
                stt(t3, t3, -2 * math.pi, ang_ap, ALU.mult, ALU.add, ["t3", ang_res], ["t3"])
                ts("dve", t3, t3, math.pi, -math.pi, ALU.min, ALU.max, ["t3"], ["t3"])
                act(s_out, t3, AF.Sin, ["t3"], [s_res])
                ts("dve", t3, t3, math.pi / 2, None, ALU.add, None, ["t3"], ["t3"])
                ts("dve", t2, t3, math.pi, 2 * math.pi, ALU.is_gt, ALU.mult, ["t3"], ["t2"])
                tt("dve", t3, t3, t2, ALU.subtract, ["t3", "t2"], ["t3"])
                ts("dve", t3, t3, math.pi, -math.pi, ALU.min, ALU.max, ["t3"], ["t3"])
                act(c_out, t3, AF.Sin, ["t3"], [c_res])

            m1 = At("m1", [1024]); c1 = At("c1", [1024]); s1 = At("s1", [1024])
            act(m1, wre, AF.Exp, ["wre"], ["m1"])
            sincos(wim, "wim", s1, "s1", c1, "c1")
            tt("dve", c1, c1, m1, ALU.mult, ["c1", "m1"], ["c1"])
            ts("dve", c1, c1, -1.0, None, ALU.add, None, ["c1"], ["c1"])
            tt("dve", s1, s1, m1, ALU.mult, ["s1", "m1"], ["s1"])
            tt("dve", t0, are, are, ALU.mult, ["rows"], ["t0"])
            tt("dve", t1, aim, aim, ALU.mult, ["rows"], ["t1"])
            tt("dve", t0, t0, t1, ALU.add, ["t0", "t1"], ["t0"])
            p.op("dve", lambda e: e.reciprocal(t0, t0), reads=["t0"], writes=["t0"])
            tt("dve", cr, c1, are, ALU.mult, ["c1", "rows"], ["cr"])
            tt("dve", t1, s1, aim, ALU.mult, ["s1", "rows"], ["t1"])
            tt("dve", cr, cr, t1, ALU.add, ["cr", "t1"], ["cr"])
            tt("dve", cr, cr, t0, ALU.mult, ["cr", "t0"], ["cr"])
            tt("dve", ci, s1, are, ALU.mult, ["s1", "rows"], ["ci"])
            tt("dve", t1, c1, aim, ALU.mult, ["c1", "rows"], ["t1"])
            tt("dve", ci, ci, t1, ALU.subtract, ["ci", "t1"], ["ci"])
            tt("dve", ci, ci, t0, ALU.mult, ["ci", "t0"], ["ci"])
            ang = m1; sn = s1; cs = c1; mp = At("mp", [1024]); mn = At("mn", [1024])
            act(mp, wre, AF.Exp, ["wre", "kcol"], ["mp"], scale=kcol[:, 0:1])
            act(mn, wre, AF.Exp, ["wre", "kcol"], ["mn"], scale=kcol[:, 1:2])
            ts("dve", ang, wim, kcol[:, 0:1], None, ALU.mult, None, ["wim", "kcol"], ["m1"])
            sincos(ang, "m1", sn, "s1", cs, "c1")
            tt("dve", Tp_c, mp, cs, ALU.mult, ["mp", "c1"], ["Tp_c"])
            tt("dve", Tp_s, mp, sn, ALU.mult, ["mp", "s1"], ["Tp_s"])
            tt("dve", t0, mn, cs, ALU.mult, ["mn", "c1"], ["t0"])
            tt("dve", t1, mn, sn, ALU.mult, ["mn", "s1"], ["t1"])
            tt("dve", Tn_c, t0, cr, ALU.mult, ["t0", "cr"], ["Tn_c"])
            tt("dve", t2, t1, ci, ALU.mult, ["t1", "ci"], ["t2"])
            tt("dve", Tn_c, Tn_c, t2, ALU.add, ["Tn_c", "t2"], ["Tn_c"])
            tt("dve", Tn_s, t0, ci, ALU.mult, ["t0", "ci"], ["Tn_s"])
            tt("dve", t2, t1, cr, ALU.mult, ["t1", "cr"], ["t2"])
            tt("dve", Tn_s, Tn_s, t2, ALU.subtract, ["Tn_s", "t2"], ["Tn_s"])
            p.barrier()

            Ab = Alloc(MIX_W_END, ARENA)
            xt = [Ab("xt0", [D]), Ab("xt1", [D])]
            F1 = Ab("F1", [D])
            hdn16 = Ab("hdn16", [D], BF16)
            hT16 = Ab("hT16", [KC, 128], BF16)
            mT16 = Ab("mT16", [KC, 128], BF16)
            mixed16 = Ab("mixed16", [D], BF16)
            ss = Ab("ss", [1]); rstd = Ab("rstd", [1])
            rope_sb2 = [Ab("rope_sb0", [2, 24]), Ab("rope_sb1", [2, 24])]
            gqk2 = [Ab("gqk0", [400]), Ab("gqk1", [400])]; rqk2 = [Ab("rqk0", [384]), Ab("rqk1", [384])]
            v162 = [[Ab("gv16_0", [384], BF16), Ab("rv16_0", [384], BF16)], [Ab("gv16_1", [384], BF16), Ab("rv16_1", [384], BF16)]]
            sgm2 = [[Ab("gsg0", [384]), Ab("rsg0", [384])], [Ab("gsg1", [384]), Ab("rsg1", [384])]]
            th = Ab("th", [384]); sq = [th, th]
            gaT = Ab("gaT", [128], parts=17)
            e1 = Ab("e1", [192]); sp_ = Ab("sp", [192])
            Eq = Ab("Eq", [192]); Ek = Ab("Ek", [192]); Eend = Ab("Eend", [192])
            dec = Ab("dec", [4], parts=48)
            qd16 = [Ab("qd16g", [192], BF16), Ab("qd16r", [192], BF16)]
            ki16 = [Ab("ki16g", [192], BF16), Ab("ki16r", [192], BF16)]
            ke16 = [Ab("ke16g", [192], BF16), Ab("ke16r", [192], BF16)]
            qkT16 = [Ab("qkT16g", [8, 128], BF16, parts=48), Ab("qkT16r", [8, 128], BF16, parts=48)]
            sc16 = [Ab("sc16g", [4, 128], BF16), Ab("sc16r", [4, 128], BF16)]
            S32 = [Ab("S32g", [4, 96], parts=48), Ab("S32r", [4, 96], parts=48)]
            S16 = [Ab("S16g", [4, 96], BF16, parts=48), Ab("S16r", [4, 96], BF16, parts=48)]
            o_sb = [Ab("o_sbg", [384]), Ab("o_sbr", [384])]
            st4 = [Ab("st4g", [4]), Ab("st4r", [4])]
            st4b = Ab("st4b", [4])
            rot = Ab("rot", [384]); ra = Ab("ra", [192]); rb = Ab("rb", [192])
            u_sb2 = [Ab("u_sb0", [256]), Ab("u_sb1", [256])]; u162 = [Ab("u16_0", [256], BF16), Ab("u16_1", [256], BF16)]
            uT16 = Ab("uT16", [2, 128], BF16)
            print("mixer SBUF end", Ab.off, ARENA)
            c1t2 = [Ab("c1t0", [512]), Ab("c1t1", [512])]; c2t2 = [Ab("c2t0", [512]), Ab("c2t1", [512])]
            z162 = [Ab("z16_0", [1024], BF16), Ab("z16_1", [1024], BF16)]
            s32 = [Ab("s32a", [2048]), Ab("s32b", [2048])]
            sT16 = Ab("sT16", [16, 128], BF16)
            ya = Ab("ya", [256]); yb = Ab("yb", [256]); yc = Ab("yc", [256]); gy16 = Ab("gy16", [256], BF16)
            gyT16 = Ab("gyT16", [2, 128], BF16)

            for i in range(2):
                p.op("pool", lambda e, i=i: e.memset(S32[i][0:48], 0.0), writes=[("S32", i)])
                p.op("pool", lambda e, i=i: e.memset(S16[i][0:48], 0.0), writes=[("S16", i)])
            p.op("pool", lambda e: e.memset(gaT[0:17, :], 1.0), writes=["gaT"])

            def mkrot(banks):
                st_ = [0]

                def nxt():
                    b_ = banks[st_[0] % len(banks)]
                    st_[0] += 1
                    return b_
                return nxt
            rotG = mkrot([0]); rotR = mkrot([2]); rotP = mkrot([1, 0])
            TB3 = 3

            def attn_core(mi, rotm, dec_ap, dec_res, nw_off, mix_off, centered, pp):
                QT = qkT16[mi]; QR = ("qkT16", mi)
                vv = v162[pp][mi]; VR = ("v16", pp, mi)
                bs = rotm()
                for h in range(4):
                    mm(bank(bs)[:, h * 128:(h + 1) * 128], QT[0:48, 4 + h, :], QT[0:48, h, :], True, True, [QR], [B(bs)])
                tt("dve", sc16[mi], bank(bs).rearrange("p (h i) -> p h i", h=4), maskT.unsqueeze(1).to_broadcast([128, 4, 128]),
                   ALU.mult, [B(bs), "maskT"], [("sc16", mi)])
                yield
                bo = rotm()
                for h in range(4):
                    mm(bank(bo)[:, h * 96:(h + 1) * 96], sc16[mi][:, h, :], vv[:, h * 96:(h + 1) * 96], True, False, [("sc16", mi), VR], [B(bo)])
                    mm(bank(bo)[:, h * 96:(h + 1) * 96], QT[0:48, h, :], S16[mi][0:48, h, :], False, True, [QR, ("S16", mi)], [B(bo)])
                O = o_sb[mi]; OR_ = ("o_sb", mi)
                cp("act", O, bank(bo)[:, 0:384], [B(bo)], [OR_])
                yield
                bd = rotm()
                for h in range(4):
                    mm(bank(bd)[0:48, h * 96:(h + 1) * 96], ke16[mi][:, h * 48:(h + 1) * 48], vv[:, h * 96:(h + 1) * 96], True, True, [("ke16", mi), VR], [B(bd)])
                tt("pool", S32[mi][0:48], S32[mi][0:48], dec_ap[0:48].unsqueeze(2).to_broadcast([48, 4, 96]), ALU.mult,
                   [("S32", mi), dec_res], [("S32", mi)])
                tt("dve", S32[mi][0:48], S32[mi][0:48], bank(bd)[0:48, 0:384].rearrange("p (h v) -> p h v", h=4), ALU.add,
                   [("S32", mi), B(bd)], [("S32", mi)])
                cp("act", S16[mi][0:48], S32[mi][0:48], [("S32", mi)], [("S16", mi)])
                yield
                o3 = O.rearrange("p (h v) -> p h v", h=4)
                if centered:
                    p.op("dve", lambda e: e.tensor_reduce(st4b, o3, AX.X, ALU.add), reads=[OR_], writes=["st4b"])
                    ts("pool", st4b, st4b, -1.0 / 96.0, None, ALU.mult, None, ["st4b"], ["st4b"])
                    tt("dve", o3, o3, st4b.unsqueeze(2).to_broadcast([128, 4, 96]), ALU.add, [OR_, "st4b"], [OR_])
                    yield
                SQ = sq[mi]; S4 = st4[mi]
                act(SQ, O, AF.Square, [OR_], ["th"])
                p.op("dve", lambda e: e.tensor_reduce(S4, SQ.rearrange("p (h v) -> p h v", h=4), AX.X, ALU.add), reads=["th"], writes=[("st4", mi)])
                ts("pool", S4, S4, 96.0 * EPS, None, ALU.add, None, [("st4", mi)], [("st4", mi)])
                tt("pool", S4, S4, nh, ALU.pow, [("st4", mi), "nh"], [("st4", mi)])
                yield
                tt("dve", o3, o3, S4.unsqueeze(2).to_broadcast([128, 4, 96]), ALU.mult, [OR_, ("st4", mi)], [OR_])
                tt("pool", mixed16[:, mix_off:mix_off + 384], O, sgm2[pp][mi], ALU.mult, [OR_, ("sg", pp, mi)], [("mixed16", mix_off)])

            def gate_prep(gbank, mi, nw_off, pp):
                act(th, bank(gbank)[:, 0:384], AF.Tanh, [B(gbank)], ["th"], scale=0.5)
                stt(sgm2[pp][mi], th, 1.0, bank(gbank)[:, 0:384], ALU.add, ALU.mult, ["th", B(gbank)], [("sg", pp, mi)])
                tt("pool", sgm2[pp][mi], sgm2[pp][mi], gnwbc[:, nw_off:nw_off + 384], ALU.mult, [("sg", pp, mi), "gnwbc"], [("sg", pp, mi)])

            def transposes_qk(mi):
                bt = TB3
                for h in range(4):
                    tr(bankbf(bt)[0:48, h * 128:(h + 1) * 128], qd16[mi][:, h * 48:(h + 1) * 48], ident16, [("qd16", mi), "ident16"], [B(bt)])
                    tr(bankbf(bt)[0:48, (4 + h) * 128:(5 + h) * 128], ki16[mi][:, h * 48:(h + 1) * 48], ident16, [("ki16", mi), "ident16"], [B(bt)])
                cp("act", qkT16[mi][0:48].rearrange("p a b -> p (a b)"), bankbf(bt)[0:48, :], [B(bt)], [("qkT16", mi)])

            def chain_gla(t):
                pp = t % 2
                gqk = gqk2[pp]; GQ = ("gqk", pp)
                bz = rotG()
                tr(bank(bz)[0:16, 0:128], gqk[:, 384:400], ident32, [GQ, "ident32"], [B(bz)])
                cp("act", gaT[0:16, :], bank(bz)[0:16, 0:128], [B(bz)], ["gaT"])
                yield
                bz2 = rotG()
                mm(bank(bz2)[:, 0:192], gaT[0:17, :], wa2b_sb[0:17, :], True, True, ["gaT", "wa2b"], [B(bz2)])
                act(e1, bank(bz2)[:, 0:192], AF.Exp, [B(bz2)], ["e1"], scale=-1.0)
                act(sp_, e1, AF.Ln, ["e1"], ["sp"], bias=1.0, scale=1.0)
                yield
                bc_ = rotG()
                mm(bank(bc_)[:, 0:192], triS, sp_, True, True, ["triS", "sp"], [B(bc_)])
                mm(bank(bc_)[:, 192:384], revS, sp_, True, True, ["revS", "sp"], [B(bc_)])
                for h in range(4):
                    mm(bank(bc_)[0:48, 384 + h:385 + h], sp_[:, h * 48:(h + 1) * 48], allS, True, True, ["sp", "allS"], [B(bc_)])
                act(Eq, bank(bc_)[:, 0:192], AF.Exp, [B(bc_)], ["Eq"])
                act(Ek, bank(bc_)[:, 0:192], AF.Exp, [B(bc_)], ["Ek"], scale=-1.0)
                act(Eend, bank(bc_)[:, 192:384], AF.Exp, [B(bc_)], ["Eend"])
                act(dec[0:48], bank(bc_)[0:48, 384:388], AF.Exp, [B(bc_)], ["dec"])
                yield
                stt(qd16[0], gqk[:, 0:192], 48.0 ** -0.5, Eq, ALU.mult, ALU.mult, [GQ, "Eq"], [("qd16", 0)])
                tt("pool", ki16[0], gqk[:, 192:384], Ek, ALU.mult, [GQ, "Ek"], [("ki16", 0)])
                tt("pool", ke16[0], gqk[:, 192:384], Eend, ALU.mult, [GQ, "Eend"], [("ke16", 0)])
                yield
                transposes_qk(0)
                yield
                yield from attn_core(0, rotG, dec, "dec", 0, 0, False, pp)

            def chain_ret(t):
                pp = t % 2
                rqk = rqk2[pp]; rope_sb = rope_sb2[pp]
                x4 = rqk.rearrange("p (a c d) -> p a c d", a=8, c=2)
                r4 = rot.rearrange("p (a c d) -> p a c d", a=8, c=2)
                cosb = rope_sb[:, 0, :].unsqueeze(1).to_broadcast([128, 8, 24])
                sinb = rope_sb[:, 1, :].unsqueeze(1).to_broadcast([128, 8, 24])
                ra3 = ra.rearrange("p (a d) -> p a d", a=8)
                rb3 = rb.rearrange("p (a d) -> p a d", a=8)
                tt("dve", ra3, x4[:, :, 0, :], cosb, ALU.mult, [("rqk", pp), ("rope_sb", pp)], ["ra"])
                tt("pool", rb3, x4[:, :, 1, :], sinb, ALU.mult, [("rqk", pp), ("rope_sb", pp)], ["rb"])
                tt("dve", r4[:, :, 0, :], ra3, rb3, ALU.subtract, ["ra", "rb"], ["rot"])
                yield
                tt("dve", ra3, x4[:, :, 0, :], sinb, ALU.mult, [("rqk", pp), ("rope_sb", pp)], ["ra"])
                tt("pool", rb3, x4[:, :, 1, :], cosb, ALU.mult, [("rqk", pp), ("rope_sb", pp)], ["rb"])
                tt("dve", r4[:, :, 1, :], ra3, rb3, ALU.add, ["ra", "rb"], ["rot"])
                yield
                tt("dve", qd16[1], rot[:, 0:192], retE[:, 0, :], ALU.mult, ["rot", "retE"], [("qd16", 1)])
                tt("pool", ki16[1], rot[:, 192:384], retE[:, 1, :], ALU.mult, ["rot", "retE"], [("ki16", 1)])
                tt("pool", ke16[1], rot[:, 192:384], retE[:, 2, :], ALU.mult, ["rot", "retE"], [("ke16", 1)])
                yield
                transposes_qk(1)
                yield
                yield from attn_core(1, rotR, retdec, "retdec", 384, 384, True, pp)

            Tnc = Tn_c.rearrange("p (g s) -> p g s", g=16); Tns = Tn_s.rearrange("p (g s) -> p g s", g=16)
            Tpc = Tp_c.rearrange("p (g s) -> p g s", g=16); Tps = Tp_s.rearrange("p (g s) -> p g s", g=16)
            def cmul(hf, src4, src_res, dst4, dst_res, Tc, Ts, Tres):
                c1v = c1t2[hf].rearrange("p (g s) -> p g s", g=8)
                c2v = c2t2[hf].rearrange("p (g s) -> p g s", g=8)
                C1 = ("c1t", hf); C2 = ("c2t", hf)
                tt("dve", c1v, src4[:, :, 0, :], Tc, ALU.mult, src_res + Tres, [C1])
                tt("dve", c2v, src4[:, :, 1, :], Ts, ALU.mult, src_res + Tres, [C2])
                tt("pool", dst4[:, :, 0, :], c1v, c2v, ALU.subtract, [C1, C2], [dst_res])
                tt("dve", c1v, src4[:, :, 0, :], Ts, ALU.mult, src_res + Tres, [C1])
                tt("dve", c2v, src4[:, :, 1, :], Tc, ALU.mult, src_res + Tres, [C2])
                tt("pool", dst4[:, :, 1, :], c1v, c2v, ALU.add, [C1, C2], [dst_res])

            def s5_half(t, hf):
                scur = s32[t % 2]
                sprev = s32[(t + 1) % 2]
                b0 = 6 if hf == 0 else 4
                BIG = [B(b0), B(b0 + 1)]
                big = psum[:, b0 * 512:(b0 + 2) * 512].rearrange("p (g c s) -> p g c s", g=8, c=2)
                z16 = z162[hf]; ZR = ("z16", hf)
                z4 = z16.rearrange("p (g c s) -> p g c s", g=8, c=2)
                c0 = hf * 1024
                SR = ("s32", t % 2, hf)
                for n in range(2):
                    mm(bank(b0 + n), uT16[:, hf, :], bblk16[:, hf, c0 + n * 512:c0 + (n + 1) * 512], True, True, ["uT16", "bblk16"], [B(b0 + n)])
                cmul(hf, big, BIG, z4, ZR, Tnc[:, hf * 8:(hf + 1) * 8, :], Tns[:, hf * 8:(hf + 1) * 8, :], ["Tn_c", "Tn_s"])
                yield
                for n in range(2):
                    mm(bank(b0 + n), tri16, z16[:, n * 512:(n + 1) * 512], True, t == 0, ["tri16", ZR], [B(b0 + n)])
                    if t > 0:
                        mm(bank(b0 + n), sel127, sprev[:, c0 + n * 512:c0 + (n + 1) * 512], False, True, ["sel127", ("s32", (t + 1) % 2, hf)], [B(b0 + n)])
                cmul(hf, big, BIG, scur[:, c0:c0 + 1024].rearrange("p (g c s) -> p g c s", g=8, c=2), SR,
                     Tpc[:, hf * 8:(hf + 1) * 8, :], Tps[:, hf * 8:(hf + 1) * 8, :], ["Tp_c", "Tp_s"])
                yield
                for g in range(8):
                    tr(bank(b0 + g // 4)[:, (g % 4) * 128:(g % 4 + 1) * 128], scur[:, c0 + g * 128:c0 + (g + 1) * 128], ident32,
                       [SR, "ident32"], [B(b0 + g // 4)])
                sTf = sT16.rearrange("p a b -> p (a b)")
                cp("act", sTf[:, c0:c0 + 512], bank(b0), [B(b0)], [("sT16", hf)])
                cp("dve", sTf[:, c0 + 512:c0 + 1024], bank(b0 + 1), [B(b0 + 1)], [("sT16", hf)])

            def chain_s5(t):
                pp = t % 2
                u_sb = u_sb2[pp]; u16 = u162[pp]
                bt = TB3
                for k in range(2):
                    tr(bankbf(bt)[:, k * 128:(k + 1) * 128], u16[:, k * 128:(k + 1) * 128], ident16, [("u16", pp), "ident16"], [B(bt)])
                cp("act", uT16.rearrange("p a b -> p (a b)"), bankbf(bt)[:, 0:256], [B(bt)], ["uT16"])
                yield
                halves = [s5_half(t, 0), s5_half(t, 1)]
                while halves:
                    for h_ in list(halves):
                        try:
                            next(h_)
                        except StopIteration:
                            halves.remove(h_)
                    yield
                by = 6
                for g in range(16):
                    mm(bank(by)[:, g * 16:(g + 1) * 16], sT16[:, g, :], cst16[:, g, :], True, True,
                       [("sT16", g // 8), "cst16a", "cst16b"], [B(by)])
                tt("pool", ya, u_sb, dbc, ALU.mult, [("u_sb", pp), "dbc"], ["ya"])
                tt("dve", ya, ya, bank(by)[:, 0:256], ALU.add, ["ya", B(by)], ["ya"])
                yield
                tt("pool", yb, ya, ya, ALU.mult, ["ya"], ["yb"])
                ts("pool", yb, yb, 0.044715, 1.0, ALU.mult, ALU.add, ["yb"], ["yb"])
                tt("dve", yb, yb, ya, ALU.mult, ["yb", "ya"], ["yb"])
                act(yc, yb, AF.Tanh, ["yb"], ["yc"], scale=math.sqrt(2.0 / math.pi))
                yield
                ts("pool", yc, yc, 1.0, 0.5, ALU.add, ALU.mult, ["yc"], ["yc"])
                tt("dve", ya, ya, yc, ALU.mult, ["ya", "yc"], ["ya"])
                cp("dve", gy16, ya, ["ya"], ["gy16"])
                yield
                for k in range(2):
                    tr(bankbf(bt)[:, k * 128:(k + 1) * 128], gy16[:, k * 128:(k + 1) * 128], ident16, ["gy16", "ident16"], [B(bt)])
                cp("act", gyT16.rearrange("p a b -> p (a b)"), bankbf(bt)[:, 0:256], [B(bt)], ["gyT16"])
                yield
                bgl = 7
                for k in range(2):
                    mm(bank(bgl)[:, 0:256], gyT16[:, k, :], wglu16[:, k, :], k == 0, False, ["gyT16", "wglu16"], [B(bgl)])
                mm(bank(bgl)[:, 0:256], ones16[0:1, :], bglu16[0:1, :], False, True, ["ones16", "bglu16"], [B(bgl)])
                act(yc, bank(bgl)[:, 0:256], AF.Tanh, [B(bgl)], ["yc"], scale=0.5)
                ts("pool", yc, yc, 1.0, 0.5, ALU.add, ALU.mult, ["yc"], ["yc"])
                tt("dve", mixed16[:, 768:1024], ya, yc, ALU.mult, ["ya", "yc"], [("mixed16", 768)])

            rotF = mkrot([1])

            def front(t):
                pp = t % 2
                X = xt[pp]; XR = ("xt", pp)
                p.dma("sp", X, src[t * 128:(t + 1) * 128, :], writes=[XR])
                p.dma("sp", rope_sb2[pp], c_rope[:, t], writes=[("rope_sb", pp)])
                norm_mod(X, XR, hdn16, "hdn16", F1, "F1", ss, rstd)
                yield
                bt = TB3
                for k in range(KC):
                    tr(bankbf(bt)[:, k * 128:(k + 1) * 128], hdn16[:, k * 128:(k + 1) * 128], ident16, ["hdn16", "ident16"], [B(bt)])
                cp("act", hT16.rearrange("p a b -> p (a b)"), bankbf(bt), [B(bt)], ["hT16"])
                yield

                def proj(cols):
                    b_ = rotF()
                    n = cols[1] - cols[0]
                    for k in range(KC):
                        mm(bank(b_)[:, 0:n], hT16[:, k, :], win16[:, k, cols[0]:cols[1]], k == 0, k == KC - 1, ["hT16", ("win16", k)], [B(b_)])
                    return b_

                b1 = proj(G1)
                cp("act", gqk2[pp], bank(b1)[:, 0:400], [B(b1)], [("gqk", pp)])
                yield
                b1 = proj(R1)
                cp("act", rqk2[pp], bank(b1)[:, 0:384], [B(b1)], [("rqk", pp)])
                yield
                b1 = proj(S1)
                cp("act", u_sb2[pp], bank(b1)[:, 0:256], [B(b1)], [("u_sb", pp)])
                cp("dve", u162[pp], bank(b1)[:, 0:256], [B(b1)], [("u16", pp)])
                yield
                b2 = proj(G2)
                cp("dve", v162[pp][0], bank(b2)[:, 0:384], [B(b2)], [("v16", pp, 0)])
                yield
                b2 = proj(R2)
                cp("dve", v162[pp][1], bank(b2)[:, 0:384], [B(b2)], [("v16", pp, 1)])
                yield
                bg = proj(G3)
                gate_prep(bg, 0, 0, pp)
                yield
                bg = proj(R3)
                gate_prep(bg, 1, 384, pp)

            for _ in front(0):
                pass
            for t in range(n_tiles):
                X = xt[t % 2]
                XR = ("xt", t % 2)
                bt = TB3
                gens = [chain_gla(t), chain_ret(t), chain_s5(t)]
                if t + 1 < n_tiles:
                    gens.append(front(t + 1))
                while gens:
                    for g_ in list(gens):
                        try:
                            next(g_)
                        except StopIteration:
                            gens.remove(g_)

                MX = [("mixed16", 0), ("mixed16", 384), ("mixed16", 768)]
                for k in range(KC):
                    tr(bankbf(bt)[:, k * 128:(k + 1) * 128], mixed16[:, k * 128:(k + 1) * 128], ident16, MX + ["ident16"], [B(bt)])
                cp("act", mT16.rearrange("p a b -> p (a b)"), bankbf(bt), [B(bt)], ["mT16"])
                for half in range(2):
                    b_ = rotP()
                    for k in range(KC):
                        mm(bank(b_), mT16[:, k, :], wout16[:, k, half * 512:(half + 1) * 512], k == 0, k == KC - 1, ["mT16", ("wout16", k)], [B(b_)])
                    tt("dve", F1[:, half * 512:(half + 1) * 512], bank(b_), modr[:, 2 * D + half * 512:2 * D + (half + 1) * 512], ALU.mult,
                       [B(b_), ("modr", 2)], ["F1"])
                tt("pool", F1, F1, X, ALU.add, ["F1", XR], ["F1"])
                tk = p.dma("sp", dst[t * 128:(t + 1) * 128, :], F1, reads=["F1"], writes=[("dst", t)])
                if stop == "mix":
                    p.out_toks.append(tk)
            p.barrier()

        def moe_phase(l, src, dst, final):
            Aa = Alloc(PERS_END, ARENA)
            wup = [Aa("wup0", [KC, 2 * D], BF16), Aa("wup1", [KC, 2 * D], BF16)]
            wdn = Aa("wdn", [KC, D], BF16)
            hT16 = Aa("mhT16", [KC, TB], BF16)
            acc = Aa("acc", [NTB, D])
            actT = Aa("actT", [KC, 512], BF16)
            rw32 = Aa("rw32", [KC, NE])
            rb32 = Aa("rb32", [NE], parts=1)
            bup = Aa("bup", [NE, 16])
            bup1 = Aa("bup1", [NE, 8])
            bdn = Aa("bdn", [D], parts=32)
            gates = Aa("gates", [NTB, NE])
            gT = Aa("gT", [128], parts=32)
            MOE_W_END = Aa.off
            for k in range(KC):
                p.dma("sp", rw32[:, k, :], router_w[l, k * 128:(k + 1) * 128, :], writes=["rw32"])
            p.dma("sp", rb32[0:1, :], router_b[l], writes=["rb32"])
            p.dma("sp", bup, b_upT[l], writes=["bup"])
            p.dma("sp", bdn[0:32, :], b_down[l], writes=["bdn"])
            ts("pool", bup1, bup[:, :, 8:16], 1.0, None, ALU.add, None, ["bup"], ["bup1"])
            At = Alloc(MOE_W_END, ARENA)
            if final:
                fnwbc = At("fnwbc", [D])
                p.dma("sp", fnwbc, fnw.partition_broadcast(128), writes=["fnwbc"])
            At2 = Alloc(At.off, ARENA)
            compute_mod(l, 1, At)
            p.barrier()
            Ab = At2
            xt = [Ab("mxt0", [D]), Ab("mxt1", [D])]
            F1 = Ab("mF1", [D]); F2 = Ab("mF2", [D])
            hT32 = Ab("hT32", [KC, 128])
            ss = Ab("mss", [1]); rstd = Ab("mrstd", [1])
            lg = Ab("lg", [NE]); mx8 = Ab("mx8", [8]); nm0 = Ab("nm0", [1]); ex = Ab("ex", [NE]); ssum = Ab("ssum", [1])
            gsb = Ab("gsb", [512]); sgb = Ab("sgb", [512]); lsb = Ab("lsb", [512])

            def load_expert(e, buf):
                for k in range(KC):
                    p.dma("pool", wup[buf][:, k, :], w_up[l, e, k * 128:(k + 1) * 128, :], writes=[("wup", buf, k)])

            def load_down(e):
                for k in range(KC):
                    p.dma("pool", wdn[:, k, :], w_down[l, e, k * 128:(k + 1) * 128, :], writes=[("wdn", k)])

            ntb = max(1, n_tiles // NTB)
            tiles_per_blk = min(NTB, n_tiles)
            for tb in range(ntb):
                load_expert(0, 0)
                load_down(0)
                for tl in range(tiles_per_blk):
                    t = tb * NTB + tl
                    X = xt[tl % 2]; XR = ("mxt", tl % 2)
                    p.dma("sp", X, src[t * 128:(t + 1) * 128, :], reads=[("dst", t)], writes=[XR])
                    if stage < 1:
                        continue
                    norm_mod(X, XR, F2, "mF2", F1, "mF1", ss, rstd)
                    if stage < 2:
                        continue
                    for hh in range(2):
                        b = nextbank()
                        for k4 in range(4):
                            k = hh * 4 + k4
                            tr(bank(b)[:, k4 * 128:(k4 + 1) * 128], F2[:, k * 128:(k + 1) * 128], ident32, ["mF2", "ident32"], [B(b)])
                        cp("act", hT32[:, hh * 4:(hh + 1) * 4, :].rearrange("p a b -> p (a b)"), bank(b), [B(b)], [("hT32", hh)])
                        for k4 in range(4):
                            k = hh * 4 + k4
                            cp("dve", hT16[:, k, tl * 128:(tl + 1) * 128], bank(b)[:, k4 * 128:(k4 + 1) * 128], [B(b)], [("mhT16", tl)])
                    if stage < 3:
                        continue
                    b = nextbank()
                    for k in range(KC):
                        mm(bank(b)[:, 0:NE], hT32[:, k, :], rw32[:, k, :], k == 0, False, [("hT32", k // 4), "rw32"], [B(b)])
                    mm(bank(b)[:, 0:NE], ones32[0:1, :], rb32[0:1, :], False, True, ["ones32", "rb32"], [B(b)])
                    cp("act", lg, bank(b)[:, 0:NE], [B(b)], ["lg"])
                    if stage < 4:
                        continue
                    p.op("dve", lambda e: e.max(out=mx8, in_=lg), reads=["lg"], writes=["mx8"])
                    ts("dve", nm0, mx8[:, 0:1], -1.0, None, ALU.mult, None, ["mx8"], ["nm0"])
                    act(ex, lg, AF.Exp, ["lg", "nm0"], ["ex"], bias=nm0, scale=1.0)
                    stt(ex, lg, mx8[:, 3:4], ex, ALU.is_ge, ALU.mult, ["lg", "mx8", "ex"], ["ex", "ssum"], accum_out=ssum)
                    p.op("dve", lambda e: e.reciprocal(ssum, ssum), reads=["ssum"], writes=["ssum"])
                    ts("dve", gates[:, tl, :], ex, ssum, None, ALU.mult, None, ["ex", "ssum"], [("gates", tl)])
                    if stage < 5:
                        continue
                    b = nextbank()
                    tr(bank(b)[0:32, 0:128], gates[:, tl, :], ident32, [("gates", tl), "ident32"], [B(b)])
                    cp("act", gT[0:32, :], bank(b)[0:32, 0:128], [B(b)], ["gT"])
                    for half in range(2):
                        b = nextbank()
                        mm(bank(b), gT[0:32, :], bdn[0:32, half * 512:(half + 1) * 512], True, True, ["gT", "bdn"], [B(b)])
                        cp("act", acc[:, tl, half * 512:(half + 1) * 512], bank(b), [B(b)], [("acc", tl, half)])
                nhalf = max(1, tiles_per_blk * 128 // 512)
                for e in range(n_exp):
                    buf = e % 2
                    if e + 1 < n_exp:
                        load_expert(e + 1, 1 - buf)
                    for hh in range(nhalf):
                        tok0 = hh * 512
                        ntok = min(512, tiles_per_blk * 128)
                        for cch in range(KC):
                            bg_ = nextbank(0, 8); bl_ = nextbank(0, 8)
                            for k in range(KC):
                                mm(bank(bg_)[:, 0:ntok], wup[buf][:, k, cch * 128:(cch + 1) * 128], hT16[:, k, tok0:tok0 + ntok], k == 0, k == KC - 1,
                                   [("wup", buf, k)] + [("mhT16", tok0 // 128 + i) for i in range(ntok // 128)], [B(bg_)])
                            for k in range(KC):
                                mm(bank(bl_)[:, 0:ntok], wup[buf][:, k, D + cch * 128:D + (cch + 1) * 128], hT16[:, k, tok0:tok0 + ntok], k == 0, k == KC - 1,
                                   [("wup", buf, k)] + [("mhT16", tok0 // 128 + i) for i in range(ntok // 128)], [B(bl_)])
                            ts("dve", gsb[:, 0:ntok], bank(bg_)[:, 0:ntok], bup[:, e, cch:cch + 1], 7.0, ALU.add, ALU.min, [B(bg_), "bup"], ["gsb"])
                            act(sgb[:, 0:ntok], gsb[:, 0:ntok], AF.Sigmoid, ["gsb"], ["sgb"], scale=1.702)
                            act(lsb[:, 0:ntok], bank(bl_)[:, 0:ntok], AF.Identity, [B(bl_), "bup1"], ["lsb"], bias=bup1[:, e, cch:cch + 1], scale=1.0)
                            ts("pool", lsb[:, 0:ntok], lsb[:, 0:ntok], 8.0, -6.0, ALU.min, ALU.max, ["lsb"], ["lsb"])
                            tt("pool", gsb[:, 0:ntok], gsb[:, 0:ntok], lsb[:, 0:ntok], ALU.mult, ["gsb", "lsb"], ["gsb"])
                            tt("dve", actT[:, cch, 0:ntok], sgb[:, 0:ntok], gsb[:, 0:ntok], ALU.mult, ["sgb", "gsb"], [("actT", cch)])
                        if hh == nhalf - 1 and e + 1 < n_exp:
                            pass
                        for tl4 in range(ntok // 128):
                            tl = hh * 4 + tl4
                            for half in range(2):
                                b = nextbank(0, 8)
                                for k in range(KC):
                                    mm(bank(b), actT[:, k, tl4 * 128:(tl4 + 1) * 128], wdn[:, k, half * 512:(half + 1) * 512], k == 0, k == KC - 1,
                                       [("actT", k), ("wdn", k)], [B(b)])
                                stt(acc[:, tl, half * 512:(half + 1) * 512], bank(b), gates[:, tl, e:e + 1], acc[:, tl, half * 512:(half + 1) * 512],
                                    ALU.mult, ALU.add, [B(b), ("gates", tl), ("acc", tl, half)], [("acc", tl, half)])
                    if e + 1 < n_exp:
                        load_down(e + 1)
                for tl in range(tiles_per_blk):
                    t = tb * NTB + tl
                    X = xt[tl % 2]; XR = ("mxt", tl % 2)
                    p.dma("sp", X, src[t * 128:(t + 1) * 128, :], reads=[("dst", t)], writes=[XR])
                    tt("dve", F1, acc[:, tl, :], modr[:, 2 * D:3 * D], ALU.mult, [("acc", tl, 0), ("acc", tl, 1)] + MODR(2), ["mF1"])
                    tt("pool", F1, F1, X, ALU.add, ["mF1", XR], ["mF1"])
                    if final:
                        act(F2, F1, AF.Square, ["mF1"], ["mF2", "ss"], accum_out=ss)
                        ts("pool", rstd, ss, 1.0 / D, EPS, ALU.mult, ALU.add, ["ss"], ["rstd"])
                        tt("pool", rstd, rstd, nh[:, 0:1], ALU.pow, ["rstd", "nh"], ["rstd"])
                        stt(F2, F1, rstd, fnwbc, ALU.mult, ALU.mult, ["mF1", "rstd", "fnwbc"], ["mF2"])
                        tk = p.dma("sp", dst[t * 128:(t + 1) * 128, :], F2, reads=["mF2"], writes=[("dst2", t)])
                    else:
                        tk = p.dma("sp", dst[t * 128:(t + 1) * 128, :], F1, reads=["mF1"], writes=[("dst2", t)])
                    if final or stop == "moe":
                        p.out_toks.append(tk)
            p.barrier()


        def moe_sparse(l, src, dst, final):
            nt = n_tiles
            Aa = Alloc(PERS_END, ARENA)
            wup = [Aa("wup0", [KC, 2 * D], BF16), Aa("wup1", [KC, 2 * D], BF16)]
            wdn = [Aa("wdn0", [KC, D], BF16), Aa("wdn1", [KC, D], BF16)]
            bupg = [Aa("bupg0", [16]), Aa("bupg1", [16])]
            bupl = [Aa("bupl0", [8]), Aa("bupl1", [8])]
            rw32 = Aa("rw32", [KC, NE])
            rb32 = Aa("rb32", [NE], parts=1)
            bdn = Aa("bdn", [D], parts=32)
            onehot = Aa("onehot", [nt, 4, NE], BF16)
            rank = Aa("rank", [nt, NE])
            g4 = Aa("g4", [nt, 4])
            desti = Aa("desti", [nt, 4], I32)
            widx = Aa("widx", [NBLK, 8], I32)
            bidx = Aa("bidx", [NBLK], I32)
            prevsum = Aa("prevsum", [NE])
            gT = Aa("gT", [128], parts=32)
            MOE_W_END = Aa.off
            for k in range(KC):
                p.dma("sp", rw32[:, k, :], router_w[l, k * 128:(k + 1) * 128, :], writes=["rw32"])
            p.dma("sp", rb32[0:1, :], router_b[l], writes=["rb32"])
            p.dma("sp", bdn[0:32, :], b_down[l], writes=["bdn"])
            At = Alloc(MOE_W_END, ARENA)
            if final:
                fnwbc = At("fnwbc", [D])
                p.dma("sp", fnwbc, fnw.partition_broadcast(128), writes=["fnwbc"])
            At2 = Alloc(At.off, ARENA)
            compute_mod(l, 1, At)
            p.barrier()
            Ab = At2
            M1_START = Ab.off
            W1 = 3
            hd16 = [Ab("hd16_%d" % i, [D], BF16) for i in range(W1)]
            HD_END = Ab.off
            SETS = []
            for i in range(W1):
                SETS.append(dict(
                    i=i, xt=Ab("mxt%d" % i, [D]), F1=Ab("mF1_%d" % i, [D]), F2=Ab("mF2_%d" % i, [D]), hT32=Ab("hT32_%d" % i, [KC, 128]),
                    ss=Ab("mss%d" % i, [1]), rstd=Ab("mrstd%d" % i, [1]), lg=Ab("lg%d" % i, [NE]), mx8=Ab("mx8_%d" % i, [8]),
                    nm0=Ab("nm0_%d" % i, [1]), ex4=Ab("ex4_%d" % i, [4]), ssum=Ab("ssum%d" % i, [1]), mask32=Ab("mask32_%d" % i, [NE])))
            xt = [SETS[0]["xt"], SETS[1]["xt"]]
            M1_END = Ab.off
            Ab = Alloc(M1_START, ARENA)
            gsb = Ab("gsb", [512]); sgb = Ab("sgb", [512]); lsb = Ab("lsb", [512])
            r16 = [Ab("r16a", [4, D], BF16), Ab("r16b", [4, D], BF16)]
            rT16 = [Ab("rT16a", [KC, 512], BF16), Ab("rT16b", [KC, 512], BF16)]
            actT = Ab("actT", [KC, 512], BF16)
            ysb = [Ab("ysb0", [D]), Ab("ysb1", [D])]

            def lockstep(gens):
                gens = list(gens)
                while gens:
                    for g_ in list(gens):
                        try:
                            next(g_)
                        except StopIteration:
                            gens.remove(g_)

            def m1_tile(t, S):
                i = S["i"]
                X = S["xt"]; XR = ("mxt", i)
                F1s = S["F1"]; F2s = S["F2"]; F1R = ("mF1", i); F2R = ("mF2", i)
                hT = S["hT32"]; lg = S["lg"]; mx8 = S["mx8"]; nm0 = S["nm0"]; ex4 = S["ex4"]; ssum = S["ssum"]; mask32 = S["mask32"]
                bk = [2 * i, 2 * i + 1]
                p.dma("sp", X, src[t * 128:(t + 1) * 128, :], writes=[XR])
                norm_mod(X, XR, F2s, F2R, F1s, F1R, S["ss"], S["rstd"], ("mss", i), ("mrstd", i))
                yield
                H = hd16[i]; HR = ("hd16", i)
                cp("pool", H, F2s, [F2R], [HR])
                p.dma("sp", hd_dram[t * 128:(t + 1) * 128, :], H, reads=[HR], writes=[("hd", t)])
                for hh in range(2):
                    b = bk[hh]
                    for k4 in range(4):
                        k = hh * 4 + k4
                        tr(bank(b)[:, k4 * 128:(k4 + 1) * 128], F2s[:, k * 128:(k + 1) * 128], ident32, [F2R, "ident32"], [B(b)])
                    cp("act", hT[:, hh * 4:(hh + 1) * 4, :].rearrange("p a b -> p (a b)"), bank(b), [B(b)], [("hT32", i, hh)])
                    yield
                b = bk[0]
                for k in range(KC):
                    mm(bank(b)[:, 0:NE], hT[:, k, :], rw32[:, k, :], k == 0, False, [("hT32", i, k // 4), "rw32"], [B(b)])
                mm(bank(b)[:, 0:NE], ones32[0:1, :], rb32[0:1, :], False, True, ["ones32", "rb32"], [B(b)])
                cp("act", lg, bank(b)[:, 0:NE], [B(b)], [("lg", i)])
                yield
                p.op("dve", lambda e: e.max(out=mx8, in_=lg), reads=[("lg", i)], writes=[("mx8", i)])
                ts("dve", nm0, mx8[:, 0:1], -1.0, None, ALU.mult, None, [("mx8", i)], [("nm0", i)])
                yield
                act(ex4, mx8[:, 0:4], AF.Exp, [("mx8", i), ("nm0", i)], [("ex4", i), ("ssum", i)], bias=nm0, scale=1.0, accum_out=ssum)
                for k in range(4):
                    ts("dve", onehot[:, t, k, :], lg, mx8[:, k:k + 1], None, ALU.is_equal, None, [("lg", i), ("mx8", i)], [("onehot", t)])
                p.op("dve", lambda e: e.tensor_reduce(mask32, onehot[:, t].rearrange("p k e -> p e k"), AX.X, ALU.add),
                     reads=[("onehot", t)], writes=[("mask32", i)])
                yield
                p.op("dve", lambda e: e.reciprocal(ssum, ssum), reads=[("ssum", i)], writes=[("ssum", i)])
                ts("dve", g4[:, t, :], ex4, ssum, None, ALU.mult, None, [("ex4", i), ("ssum", i)], [("g4", t)])
                yield
                b = bk[1]
                mm(bank(b)[:, 0:NE], stri, mask32, True, t == 0, ["stri", ("mask32", i)], [B(b)])
                if t > 0:
                    mm(bank(b)[:, 0:NE], ones32, prevsum, False, True, ["ones32", "prevsum"], [B(b)])
                cp("act", rank[:, t, :], bank(b)[:, 0:NE], [B(b)], [("rank", t)])
                if t == 0:
                    cp("dve", prevsum, mask32, [("mask32", i)], ["prevsum"])
                else:
                    tt("dve", prevsum, prevsum, mask32, ALU.add, ["prevsum", ("mask32", i)], ["prevsum"])

            for t0 in range(0, nt, W1):
                lockstep([m1_tile(t, SETS[t - t0]) for t in range(t0, min(nt, t0 + W1))])
            p.barrier()
            ONEHOT = [("onehot", t) for t in range(nt)]
            RANK = [("rank", t) for t in range(nt)]
            G4 = [("g4", t) for t in range(nt)]

            Ac = Alloc(HD_END, ARENA)
            cnt = Ac("cnt", [NE]); nb = Ac("nb", [NE]); nbi = Ac("nbi", [NE], I32); incl = Ac("incl", [NE]); pst = Ac("pst", [NE])
            onesr = Ac("onesr", [NE]); dest = Ac("dest", [nt, NE]); prod = Ac("prod", [nt, 4, NE]); destf = Ac("destf", [nt, 4])
            cmp_ = Ac("cmp", [NBLK, NE]); blke = Ac("blke", [NBLK]); wf = Ac("wf", [NBLK, 8]); bf_ = Ac("bf", [NBLK])
            b = nextbank()
            mm(bank(b)[:, 0:NE], ones32, prevsum, True, True, ["ones32", "prevsum"], [B(b)])
            cp("act", cnt, bank(b)[:, 0:NE], [B(b)], ["cnt"])
            ts("dve", nb, cnt, 511.0, 1.0 / 512.0, ALU.add, ALU.mult, ["cnt"], ["nb"])
            ts("dve", nbi, nb, -0.5 + 1.0 / 1024.0, None, ALU.add, None, ["nb"], ["nbi"])
            cp("dve", nb, nbi, ["nbi"], ["nb"])
            p.op("pool", lambda e: e.memset(onesr, 1.0), writes=["onesr"])
            p.op("dve", lambda e: e.tensor_tensor_scan(incl, onesr, nb, 0.0, ALU.mult, ALU.add), reads=["onesr", "nb"], writes=["incl"])
            tt("dve", pst, incl, nb, ALU.subtract, ["incl", "nb"], ["pst"])
            ts("dve", pst, pst, 512.0, None, ALU.mult, None, ["pst"], ["pst"])
            tt("dve", dest, rank, pst.unsqueeze(1).to_broadcast([128, nt, NE]), ALU.add, RANK + ["pst"], ["dest"])
            tt("dve", prod, onehot, dest.unsqueeze(2).to_broadcast([128, nt, 4, NE]), ALU.mult, ONEHOT + ["dest"], ["prod"])
            p.op("dve", lambda e: e.tensor_reduce(destf, prod, AX.X, ALU.add), reads=["prod"], writes=["destf"])
            cp("dve", desti, destf, ["destf"], ["desti"])
            NB_ = NBLK
            tt("dve", cmp_, incl.unsqueeze(1).to_broadcast([128, NB_, NE]), idxc[:, 0:NB_].unsqueeze(2).to_broadcast([128, NB_, NE]),
               ALU.is_le, ["incl", "idxc"], ["cmp"])
            p.op("dve", lambda e: e.tensor_reduce(blke, cmp_, AX.X, ALU.add), reads=["cmp"], writes=["blke"])
            ts("dve", blke, blke, float(NE - 1), None, ALU.min, None, ["blke"], ["blke"])
            ts("dve", bf_, blke, 128.0, idxc[:, 72:73], ALU.mult, ALU.add, ["blke", "idxc"], ["bf"])
            if l > 0:
                ts("dve", bf_, bf_, float(l * NE * 128), None, ALU.add, None, ["bf"], ["bf"])
            cp("dve", bidx, bf_, ["bf"], ["bidx"])
            ts("dve", blke, blke, 1024.0, float(l * NE * 1024), ALU.mult, ALU.add, ["blke", "bf"], ["blke"])
            tt("dve", wf, blke.unsqueeze(2).to_broadcast([128, NB_, 8]), idxc[:, 64:72].unsqueeze(1).to_broadcast([128, NB_, 8]),
               ALU.add, ["blke", "idxc"], ["wf"])
            cp("dve", widx, wf, ["wf"], ["widx"])

            SC = []
            for t in range(nt):
                H = hd16[t % 2]; HR = ("hd16", t % 2)
                p.dma("sp", H, hd_dram[t * 128:(t + 1) * 128, :], reads=[("hd", t)], writes=[HR])
                for k in range(4):
                    nm = ("rows", t, k)
                    SC.append(nm)
                    p.idma(lambda e, H=H, t=t, k=k: e.indirect_dma_start(
                        out=rowsbuf, out_offset=bass.IndirectOffsetOnAxis(ap=desti[:, t, k:k + 1], axis=0), in_=H, in_offset=None),
                        reads=[HR, "desti"], writes=[nm])

            p.barrier()
            wup_d = w_up.rearrange("l e r n -> (l e r) n")
            wdn_d = w_down.rearrange("l e r n -> (l e r) n")
            bup_d = b_upT2.rearrange("l r c -> (l r) c")

            def load_block_w(j, buf):
                for k in range(KC):
                    p.idma(lambda e, j=j, k=k, buf=buf: e.indirect_dma_start(
                        out=wup[buf][:, k, :], out_offset=None, in_=wup_d, in_offset=bass.IndirectOffsetOnAxis(ap=widx[:, j, k:k + 1], axis=0)),
                        reads=["widx"], writes=[("wup", buf, k)])
                for k in range(KC):
                    p.idma(lambda e, j=j, k=k, buf=buf: e.indirect_dma_start(
                        out=wdn[buf][:, k, :], out_offset=None, in_=wdn_d, in_offset=bass.IndirectOffsetOnAxis(ap=widx[:, j, k:k + 1], axis=0)),
                        reads=["widx"], writes=[("wdn", buf, k)])
                p.idma(lambda e, j=j, buf=buf: e.indirect_dma_start(
                    out=bupg[buf], out_offset=None, in_=bup_d, in_offset=bass.IndirectOffsetOnAxis(ap=bidx[:, j:j + 1], axis=0)),
                    reads=["bidx"], writes=[("bupg", buf)])

            OR = []
            load_block_w(0, 0)

            def load_rows(j):
                rb = j % 2
                p.dma("sp", r16[rb], rowsbuf[j * 512:(j + 1) * 512, :].rearrange("(a p) n -> p a n", p=128), reads=SC, writes=[("r16", rb)])

            def transpose_rows(j):
                rb = j % 2
                for k2 in range(4):
                    bt = nextbank(0, 8)
                    for kk in range(2):
                        k = k2 * 2 + kk
                        for a in range(4):
                            tr(bankbf(bt)[:, kk * 512 + a * 128:kk * 512 + (a + 1) * 128], r16[rb][:, a, k * 128:(k + 1) * 128], ident16,
                               [("r16", rb), "ident16"], [B(bt)])
                    cp("act" if k2 % 2 == 0 else "dve", rT16[rb][:, k2 * 2:k2 * 2 + 2, :].rearrange("p a b -> p (a b)"), bankbf(bt), [B(bt)], [("rT16", rb, k2)])

            load_rows(0)
            transpose_rows(0)
            if NBLK > 1:
                load_rows(1)
            for j in range(NBLK):
                buf = j % 2
                rb = j % 2
                RT = rT16[rb]
                if j + 1 < NBLK:
                    load_block_w(j + 1, 1 - buf)
                ts("dve", bupl[buf], bupg[buf][:, 8:16], 1.0, None, ALU.add, None, [("bupg", buf)], [("bupl", buf)])
                for cch in range(KC):
                    bg_ = nextbank(0, 8); bl_ = nextbank(0, 8)
                    for k in range(KC):
                        mm(bank(bg_), wup[buf][:, k, cch * 128:(cch + 1) * 128], RT[:, k, :], k == 0, k == KC - 1, [("wup", buf, k), ("rT16", rb, k // 2)], [B(bg_)])
                    for k in range(KC):
                        mm(bank(bl_), wup[buf][:, k, D + cch * 128:D + (cch + 1) * 128], RT[:, k, :], k == 0, k == KC - 1, [("wup", buf, k), ("rT16", rb, k // 2)], [B(bl_)])
                    ts("dve", gsb, bank(bg_), bupg[buf][:, cch:cch + 1], 7.0, ALU.add, ALU.min, [B(bg_), ("bupg", buf)], ["gsb"])
                    act(sgb, gsb, AF.Sigmoid, ["gsb"], ["sgb"], scale=1.702)
                    act(lsb, bank(bl_), AF.Identity, [B(bl_), ("bupl", buf)], ["lsb"], bias=bupl[buf][:, cch:cch + 1], scale=1.0)
                    ts("dve", lsb, lsb, 8.0, -6.0, ALU.min, ALU.max, ["lsb"], ["lsb"])
                    tt("dve", gsb, gsb, lsb, ALU.mult, ["gsb", "lsb"], ["gsb"])
                    tt("dve", actT[:, cch, :], sgb, gsb, ALU.mult, ["sgb", "gsb"], [("actT", cch)])
                if j + 1 < NBLK:
                    transpose_rows(j + 1)
                if j + 2 < NBLK:
                    load_rows(j + 2)
                for a in range(4):
                    Y = ysb[a % 2]; YR = ("ysb", a % 2)
                    for half in range(2):
                        b = nextbank(0, 8)
                        for k in range(KC):
                            mm(bank(b), actT[:, k, a * 128:(a + 1) * 128], wdn[buf][:, k, half * 512:(half + 1) * 512], k == 0, k == KC - 1,
                               [("actT", k), ("wdn", buf, k)], [B(b)])
                        cp("act" if half == 0 else "dve", Y[:, half * 512:(half + 1) * 512], bank(b), [B(b)], [YR])
                    nm = ("orow", j, a)
                    OR.append(nm)
                    p.dma("sp", orows[j * 512 + a * 128:j * 512 + (a + 1) * 128, :], Y, reads=[YR], writes=[nm])

            p.barrier()
            Ad = Alloc(PERS_END, PERS_END + 64 * 1024)
            W5 = 3
            M5S = [dict(i=i, yk=Ad("yk%d" % i, [4, D]), gd=Ad("gd%d" % i, [NE]), accb=Ad("accb%d" % i, [D]), gT=Ad("gT5_%d" % i, [128], parts=32))
                   for i in range(W5)]

            def m5_tile(t, S5_):
                i = S5_["i"]
                yk = S5_["yk"]; gd = S5_["gd"]; accb = S5_["accb"]; gTs = S5_["gT"]
                S = SETS[i]
                X = S["xt"]; XR = ("mxt", i); F1s = S["F1"]; F2s = S["F2"]; F1R = ("mF1", i); F2R = ("mF2", i)
                ssx = S["ss"]; rs = S["rstd"]; SSR = ("mss", i); RSR = ("mrstd", i)
                bk = [2 * i, 2 * i + 1]
                for k in range(4):
                    p.idma(lambda e, t=t, k=k: e.indirect_dma_start(
                        out=yk[:, k, :], out_offset=None, in_=orows, in_offset=bass.IndirectOffsetOnAxis(ap=desti[:, t, k:k + 1], axis=0)),
                        reads=OR + ["desti"], writes=[("yk", i, k)])
                p.dma("sp", X, src[t * 128:(t + 1) * 128, :], writes=[XR])
                ts("dve", gd, onehot[:, t, 0, :], g4[:, t, 0:1], None, ALU.mult, None, [("onehot", t), ("g4", t)], [("gd", i)])
                for k in range(1, 4):
                    stt(gd, onehot[:, t, k, :], g4[:, t, k:k + 1], gd, ALU.mult, ALU.add, [("onehot", t), ("g4", t), ("gd", i)], [("gd", i)])
                yield
                b = bk[0]
                tr(bank(b)[0:32, 0:128], gd, ident32, [("gd", i), "ident32"], [B(b)])
                cp("act", gTs[0:32, :], bank(b)[0:32, 0:128], [B(b)], [("gT5", i)])
                yield
                for half in range(2):
                    b = bk[half]
                    mm(bank(b), gTs[0:32, :], bdn[0:32, half * 512:(half + 1) * 512], True, True, [("gT5", i), "bdn"], [B(b)])
                    cp("act", accb[:, half * 512:(half + 1) * 512], bank(b), [B(b)], [("accb", i)])
                yield
                for k in range(4):
                    stt(accb, yk[:, k, :], g4[:, t, k:k + 1], accb, ALU.mult, ALU.add, [("yk", i, k), ("g4", t), ("accb", i)], [("accb", i)])
                    if k % 2 == 1:
                        yield
                tt("dve", F1s, accb, modr[:, 2 * D:3 * D], ALU.mult, [("accb", i)] + MODR(2), [F1R])
                tt("pool", F1s, F1s, X, ALU.add, [F1R, XR], [F1R])
                yield
                if final:
                    act(F2s, F1s, AF.Square, [F1R], [F2R, SSR], accum_out=ssx)
                    ts("pool", rs, ssx, 1.0 / D, EPS, ALU.mult, ALU.add, [SSR], [RSR])
                    tt("pool", rs, rs, nh[:, 0:1], ALU.pow, [RSR, "nh"], [RSR])
                    yield
                    stt(F2s, F1s, rs, fnwbc, ALU.mult, ALU.mult, [F1R, RSR, "fnwbc"], [F2R])
                    tk = p.dma("sp", dst[t * 128:(t + 1) * 128, :], F2s, reads=[F2R], writes=[("dst2", t)])
                else:
                    tk = p.dma("sp", dst[t * 128:(t + 1) * 128, :], F1s, reads=[F1R], writes=[("dst2", t)])
                if final or stop == "moe":
                    p.out_toks.append(tk)

            for t0 in range(0, nt, W5):
                lockstep([m5_tile(t, M5S[t - t0]) for t in range(t0, min(nt, t0 + W5))])
            p.barrier()

        src_name = "x"
        cur = x_in
        for l in range(n_layers):
            last = (l == n_layers - 1)
            if stop == "mix" and last:
                mixer_phase(l, cur, y_out)
                break
            mixer_phase(l, cur, xsA)
            if last:
                moe_sparse(l, xsA, y_out, final=(stop is None))
            else:
                moe_sparse(l, xsA, xsB, final=False)
                cur = xsB
        p.wait_all("sp", p.out_toks)
        p.run(st)
    return nc


_CACHE = {}


def kernel(**inputs):
    inp = {k: np.asarray(v) for k, v in inputs.items()}
    consts = make_consts()
    w = prep_weights(inp)
    if "nc" not in _CACHE:
        _CACHE["nc"] = build()
    nc = _CACHE["nc"]
    in_maps = []
    for b in range(8):
        m = dict(w)
        m.update(consts)
        m["x"] = np.ascontiguousarray(inp["x"][b])
        m["c"] = np.ascontiguousarray(inp["c"][b].reshape(KC, 128).T)
        in_maps.append(m)
    res = run_bass_kernel_spmd(nc, in_maps, core_ids=list(range(8)))
    return np.stack([r["y"] for r in res.results], axis=0).astype(np.float32)
```

```python
import math
import numpy as np
from contextlib import ExitStack
import concourse.bass as bass
import concourse.mybir as mybir
from concourse.bass_utils import run_bass_kernel_spmd

F32 = mybir.dt.float32
BF16 = mybir.dt.bfloat16
I32 = mybir.dt.int32
U8 = mybir.dt.uint8
AF = mybir.ActivationFunctionType
ALU = mybir.AluOpType
AX = mybir.AxisListType

EPOCH = 16000
NDSEM = 8


class Prog:
    ENGS = ("pe", "act", "dve", "pool", "sp")

    def __init__(self, nc):
        self.nc = nc
        self.streams = {e: [] for e in self.ENGS}
        self.count = {e: 0 for e in self.ENGS}
        self.sems = {}
        self.dcount = {}
        self.dn = {e: 0 for e in self.ENGS}
        self.last_w = {}
        self.readers = {}
        self.known = {e: {} for e in self.ENGS}
        self._stack = None
        self.out_toks = []

    def _sem(self, key):
        d = self.sems
        if key not in d:
            d[key] = self._stack.enter_context(
                self.nc.semaphore("s_" + "_".join(str(x) for x in key)))
        return d[key]

    def _need(self, eng, tok):
        if tok is None:
            return None
        semkey, val = tok
        if semkey[0] == eng and eng == "pe" and semkey[1] != "d":
            return None
        k = self.known[eng]
        if k.get(semkey, 0) >= val:
            return None
        k[semkey] = val
        return (semkey, val)

    def _deps(self, eng, reads, writes):
        need = []
        for r in reads:
            t = self._need(eng, self.last_w.get(r))
            if t:
                need.append(t)
        for w in writes:
            t = self._need(eng, self.last_w.get(w))
            if t:
                need.append(t)
            for rt in self.readers.get(w, {}).items():
                t = self._need(eng, rt)
                if t:
                    need.append(t)
        best = {}
        for sk, v in need:
            best[sk] = max(best.get(sk, 0), v)
        return list(best.items())

    def _commit(self, tok, reads, writes):
        for r in reads:
            d = self.readers.setdefault(r, {})
            d[tok[0]] = max(d.get(tok[0], 0), tok[1])
        for w in writes:
            self.last_w[w] = tok
            self.readers[w] = {}

    def op(self, eng, fn, reads=(), writes=()):
        psr = [r for r in reads if isinstance(r, tuple) and r[0] == "ps"]
        if psr:
            reads = [r for r in reads if not (isinstance(r, tuple) and r[0] == "ps")]
            writes = list(writes) + [r for r in psr if r not in writes]
        deps = self._deps(eng, reads, writes)
        self.count[eng] += 1
        n = self.count[eng]
        ep = (n - 1) // EPOCH
        semkey = (eng, ep)
        tok = (semkey, n - ep * EPOCH)
        self._commit(tok, reads, writes)

        def emit(e, deps=deps, semkey=semkey, fn=fn):
            for sk, v in deps:
                e.wait_ge(self._sem(sk), v)
            fn(e).then_inc(self._sem(semkey), 1)
        self.streams[eng].append(emit)
        return tok

    def dma(self, q, out, in_, reads=(), writes=(), **kw):
        r = self.dn[q] % NDSEM
        self.dn[q] += 1
        semkey = (q, "d", r)
        prev = self.dcount.get(semkey, 0)
        deps = self._deps(q, reads, writes)
        if prev > 0:
            t = self._need(q, (semkey, 16 * prev))
            if t:
                deps.append(t)
        self.dcount[semkey] = prev + 1
        tok = (semkey, 16 * (prev + 1))
        self._commit(tok, reads, writes)

        def emit(e, deps=deps, semkey=semkey):
            for sk, v in deps:
                e.wait_ge(self._sem(sk), v)
            e.dma_start(out=out, in_=in_, **kw).then_inc(self._sem(semkey), 16)
        self.streams[q].append(emit)
        return tok

    def idma(self, fn, reads=(), writes=()):
        q = "pool"
        r = self.dn[q] % NDSEM
        self.dn[q] += 1
        semkey = (q, "d", r)
        prev = self.dcount.get(semkey, 0)
        deps = self._deps(q, reads, writes)
        if prev > 0:
            t = self._need(q, (semkey, 16 * prev))
            if t:
                deps.append(t)
        self.dcount[semkey] = prev + 1
        tok = (semkey, 16 * (prev + 1))
        self._commit(tok, reads, writes)

        def emit(e, deps=deps, semkey=semkey):
            for sk, v in deps:
                e.wait_ge(self._sem(sk), v)
            fn(e).then_inc(self._sem(semkey), 16)
        self.streams[q].append(emit)
        return tok

    def barrier(self):
        toks = []
        for e in self.ENGS:
            n = self.count[e]
            if n:
                ep = (n - 1) // EPOCH
                toks.append(((e, ep), n - ep * EPOCH))
        for k, v in self.dcount.items():
            toks.append((k, 16 * v))
        for e in self.ENGS:
            deps = []
            for t in toks:
                if t[0][0] == e and len(t[0]) == 2:
                    continue
                n = self._need(e, t)
                if n:
                    deps.append(n)

            def emit(eng, deps=deps):
                for sk, v in deps:
                    eng.wait_ge(self._sem(sk), v)
            self.streams[e].append(emit)

    def wait_all(self, eng, toks):
        deps = []
        for t in toks:
            n = self._need(eng, t)
            if n:
                deps.append(n)

        def emit(e, deps=deps):
            for sk, v in deps:
                e.wait_ge(self._sem(sk), v)
        self.streams[eng].append(emit)

    def run(self, stack):
        self._stack = stack
        nc = self.nc
        for e in self.ENGS:
            for ep in range((self.count[e] + EPOCH - 1) // EPOCH):
                self._sem((e, ep))
        for k in self.dcount:
            self._sem(k)
        block = stack.enter_context(nc.Block())
        S = self.streams

        @block.tensor
        def _(e):
            for f in S["pe"]:
                f(e)

        @block.scalar
        def _(e):
            for f in S["act"]:
                f(e)

        @block.vector
        def _(e):
            for f in S["dve"]:
                f(e)

        @block.gpsimd
        def _(e):
            for f in S["pool"]:
                f(e)

        @block.sync
        def _(e):
            for f in S["sp"]:
                f(e)


D = 1024
SEQ = 4096
NT = 32
KC = 8
NCOL = 2576
DEPTH = 2
NE = 32
EPS = 1e-5
G1 = (0, 400)
G2 = (400, 784)
G3 = (784, 1168)
R1 = (1168, 1552)
R2 = (1552, 1936)
R3 = (1936, 2320)
S1 = (2320, 2576)
TB = 1024
NTB = TB // 128


def make_consts():
    f = np.float32
    j = np.arange(128)
    c = {}
    maskT = (j[:, None] <= j[None, :]).astype(f)
    c["c_maskT"] = maskT
    c["c_triS"] = (maskT * (-1.0 / 16.0)).astype(f)
    c["c_revS"] = ((j[:, None] > j[None, :]).astype(f) * (-1.0 / 16.0)).astype(f)
    c["c_allS"] = np.full((128, 1), -1.0 / 16.0, f)
    sel = np.zeros((128, 128), f)
    sel[127, :] = 1.0
    c["c_sel127"] = sel
    c["c_ident"] = np.eye(128, dtype=f)
    pos = np.arange(SEQ, dtype=f)
    inv_freq = (10000.0 ** (-np.arange(0, 48, 2, dtype=f) / f(48))).astype(f)
    ang = (pos[:, None] * inv_freq[None, :]).astype(f).astype(np.float64)
    cos = np.cos(ang).astype(f).reshape(NT, 128, 24).transpose(1, 0, 2)
    sin = np.sin(ang).astype(f).reshape(NT, 128, 24).transpose(1, 0, 2)
    c["c_rope"] = np.ascontiguousarray(np.stack([cos, sin], axis=2))
    lg = np.log1p(-np.exp2(-5.0 - np.arange(4, dtype=np.float64)))
    cum = (j[:, None] + 1.0) * lg[None, :]
    tot = 128.0 * lg
    sc = 48.0 ** -0.5
    rep = lambda a: np.repeat(a, 48, axis=1).astype(f)
    c["c_retE"] = np.ascontiguousarray(np.stack(
        [rep(np.exp(cum)), rep(np.exp(-cum) * sc), rep(np.exp(tot[None, :] - cum) * sc)], axis=1))
    c["c_retdec"] = np.repeat(np.exp(tot)[None, :], 48, axis=0).astype(f)
    kc = np.stack([j + 1.0, -(j + 1.0)], axis=1).astype(f)
    c["c_kcol"] = kc
    c["c_stri"] = (j[:, None] < j[None, :]).astype(f)
    idxc = np.zeros((128, 73), f)
    idxc[:, 0:64] = np.arange(64)[None, :]
    idxc[:, 64:72] = np.arange(8)[None, :] * 128 + j[:, None]
    idxc[:, 72] = j
    c["c_idxc"] = idxc
    return c


def prep_weights(inp):
    f = np.float32
    w = {}
    win = inp["w_in"]
    w["w_in_r"] = np.ascontiguousarray(np.concatenate(
        [win[:, :, 0:384], win[:, :, 1152:1168], win[:, :, 384:1152], win[:, :, 1168:]], axis=2))
    w["w_out"] = inp["w_out"]
    w["w_mod"] = inp["w_mod"]
    w["b_mod"] = np.ascontiguousarray(inp["b_mod"].reshape(DEPTH, 1, 6 * D))
    w["nw"] = np.ascontiguousarray(np.stack([inp["norm1_w"], inp["norm2_w"]], axis=1).reshape(DEPTH, 1, 2 * D))
    w["fnw"] = np.ascontiguousarray(inp["final_norm_w"].reshape(1, D))
    w["wa2b"] = np.ascontiguousarray(np.concatenate([inp["gla_w_a2"], inp["gla_b_a"][:, None, :]], axis=1))
    w["gnw"] = np.ascontiguousarray(np.concatenate([inp["gla_norm_w"], inp["ret_norm_w"]], axis=1).reshape(DEPTH, 1, 768))
    ldt = np.repeat(inp["s5_log_dt"][:, :, None], 64, axis=2)
    w["s5rows"] = np.ascontiguousarray(np.concatenate(
        [inp["s5_a_re"].reshape(DEPTH, 1024), inp["s5_a_im"].reshape(DEPTH, 1024), ldt.reshape(DEPTH, 1024)],
        axis=1).reshape(DEPTH, 1, 3072))
    bblk = np.zeros((DEPTH, 256, 2048), f)
    cst = np.zeros((DEPTH, 128, 16, 16), f)
    for g in range(16):
        bblk[:, g * 16:(g + 1) * 16, g * 128:g * 128 + 64] = inp["s5_b_re"][:, g].transpose(0, 2, 1)
        bblk[:, g * 16:(g + 1) * 16, g * 128 + 64:g * 128 + 128] = inp["s5_b_im"][:, g].transpose(0, 2, 1)
        cst[:, 0:64, g, :] = inp["s5_c_re"][:, g].transpose(0, 2, 1)
        cst[:, 64:128, g, :] = inp["s5_c_im"][:, g].transpose(0, 2, 1)
    w["bblk"] = bblk
    w["cst"] = cst
    w["s5d"] = np.ascontiguousarray(inp["s5_d"].reshape(DEPTH, 1, 256))
    w["wglu"] = inp["s5_w_glu"]
    w["bglu"] = np.ascontiguousarray(inp["s5_b_glu"].reshape(DEPTH, 1, 256))
    w["router_w"] = inp["router_w"]
    w["router_b"] = np.ascontiguousarray(inp["router_b"].reshape(DEPTH, 1, NE))
    w["w_up"] = inp["w_up"]
    w["w_down"] = inp["w_down"]
    w["b_upT"] = np.ascontiguousarray(inp["b_up"].reshape(DEPTH, NE, 16, 128).transpose(0, 3, 1, 2))
    w["b_down"] = inp["b_down"]
    w["b_upT2"] = np.ascontiguousarray(inp["b_up"].reshape(DEPTH, NE, 16, 128).transpose(0, 1, 3, 2).reshape(DEPTH, NE * 128, 16))
    return w


def build(n_layers=DEPTH, n_tiles=NT, n_exp=NE, stop=None, stage=99):
    nc = bass.Bass("TRN2", target_bir_lowering=False)
    ins = {}

    def IN(name, shape):
        ins[name] = nc.dram_tensor(name, list(shape), F32, kind="ExternalInput").ap()
        return ins[name]

    x_in = IN("x", [SEQ, D])
    c_in = IN("c", [128, KC])
    w_in_r = IN("w_in_r", [DEPTH, D, NCOL])
    w_out = IN("w_out", [DEPTH, D, D])
    w_mod = IN("w_mod", [DEPTH, D, 6 * D])
    b_mod = IN("b_mod", [DEPTH, 1, 6 * D])
    nw = IN("nw", [DEPTH, 1, 2 * D])
    fnw = IN("fnw", [1, D])
    wa2b = IN("wa2b", [DEPTH, 17, 192])
    gnw = IN("gnw", [DEPTH, 1, 768])
    s5rows = IN("s5rows", [DEPTH, 1, 3072])
    bblk = IN("bblk", [DEPTH, 256, 2048])
    cst = IN("cst", [DEPTH, 128, 16, 16])
    s5d = IN("s5d", [DEPTH, 1, 256])
    wglu = IN("wglu", [DEPTH, 256, 256])
    bglu = IN("bglu", [DEPTH, 1, 256])
    router_w = IN("router_w", [DEPTH, D, NE])
    router_b = IN("router_b", [DEPTH, 1, NE])
    w_up = IN("w_up", [DEPTH, NE, D, 2 * D])
    w_down = IN("w_down", [DEPTH, NE, D, D])
    b_upT = IN("b_upT", [DEPTH, 128, NE, 16])
    b_down = IN("b_down", [DEPTH, NE, D])
    c_maskT = IN("c_maskT", [128, 128])
    c_triS = IN("c_triS", [128, 128])
    c_revS = IN("c_revS", [128, 128])
    c_allS = IN("c_allS", [128, 1])
    c_sel127 = IN("c_sel127", [128, 128])
    c_ident = IN("c_ident", [128, 128])
    c_rope = IN("c_rope", [128, NT, 2, 24])
    c_retE = IN("c_retE", [128, 3, 192])
    c_retdec = IN("c_retdec", [48, 4])
    c_kcol = IN("c_kcol", [128, 2])
    c_stri = IN("c_stri", [128, 128])
    c_idxc = IN("c_idxc", [128, 73])
    b_upT2 = IN("b_upT2", [DEPTH, NE * 128, 16])

    y_out = nc.dram_tensor("y", [SEQ, D], F32, kind="ExternalOutput").ap()
    xsA = nc.dram_tensor("xsA", [SEQ, D], F32).ap()
    xsB = nc.dram_tensor("xsB", [SEQ, D], F32).ap()
    NBLK = n_tiles * 128 * 4 // 512 + NE
    hd_dram = nc.dram_tensor("hd_dram", [SEQ, D], BF16).ap()
    rowsbuf = nc.dram_tensor("rowsbuf", [NBLK * 512, D], BF16).ap()
    orows = nc.dram_tensor("orows", [NBLK * 512, D], F32).ap()

    st = ExitStack()
    with st:
        p = Prog(nc)
        ARENA = 206 * 1024
        arena = st.enter_context(nc.sbuf_tensor("arena", [128, ARENA], U8))
        psum = st.enter_context(nc.psum_tensor("psum", [128, 4096], F32))
        ESZ = {F32: 4, BF16: 2, I32: 4}

        class Alloc:
            def __init__(self, base, limit):
                self.off = base
                self.limit = limit

            def __call__(self, name, shape, dt=F32, parts=128):
                n = int(np.prod(shape))
                nbytes = n * ESZ[dt]
                off = (self.off + 31) // 32 * 32
                self.off = off + nbytes
                assert self.off <= self.limit, (name, self.off, self.limit)
                v = arena[0:parts, off:off + nbytes].bitcast(dt)
                if len(shape) == 2:
                    v = v.rearrange("p (a b) -> p a b", a=shape[0])
                elif len(shape) == 3:
                    v = v.rearrange("p (a b c) -> p a b c", a=shape[0], b=shape[1])
                return v

        bank = lambda b: psum[:, b * 512:(b + 1) * 512]
        bankbf = lambda b: psum[:, b * 512:(b + 1) * 512].bitcast(BF16)
        B = lambda b: ("ps", b)
        rr = [0]

        def nextbank(lo=0, hi=3):
            b = lo + rr[0] % (hi - lo)
            rr[0] += 1
            return b

        def mm(out, lhsT, rhs, start, stop, R, W):
            return p.op("pe", lambda e: e.matmul(out, lhsT=lhsT, rhs=rhs, start=start, stop=stop), reads=R, writes=W)

        def tr(out, in_, idn, R, W):
            return p.op("pe", lambda e: e.transpose(out, in_, idn), reads=R, writes=W)

        def act(out, in_, func, R, W, **kw):
            return p.op("act", lambda e: e.activation(out, in_, func, **kw), reads=R, writes=W)

        def tt(eng, out, in0, in1, op, R, W):
            return p.op(eng, lambda e: e.tensor_tensor(out, in0, in1, op), reads=R, writes=W)

        def ts(eng, out, in0, s1, s2, op0, op1, R, W):
            if s2 is None:
                return p.op(eng, lambda e: e.tensor_scalar(out, in0, s1, None, op0), reads=R, writes=W)
            return p.op(eng, lambda e: e.tensor_scalar(out, in0, s1, s2, op0, op1), reads=R, writes=W)

        def stt(out, in0, scalar, in1, op0, op1, R, W, **kw):
            return p.op("dve", lambda e: e.scalar_tensor_tensor(out, in0, scalar, in1, op0, op1, **kw), reads=R, writes=W)

        def cp(eng, out, in_, R, W):
            if eng == "act":
                return p.op("act", lambda e: e.copy(out, in_), reads=R, writes=W)
            return p.op(eng, lambda e: e.tensor_copy(out, in_), reads=R, writes=W)

        A = Alloc(0, ARENA)
        ident32 = A("ident32", [128]); ident16 = A("ident16", [128], BF16)
        maskT = A("maskT", [128]); triS = A("triS", [128]); revS = A("revS", [128])
        allS = A("allS", [1]); sel127 = A("sel127", [128]); tri16 = A("tri16", [128], BF16)
        retE = A("retE", [3, 192]); retdec = A("retdec", [4]); kcol = A("kcol", [2])
        nh = A("nh", [4]); ones16 = A("ones16", [128], BF16); ones32 = A("ones32", [128])
        stri = A("stri", [128]); idxc = A("idxc", [73])
        condT = A("condT", [KC])
        modr = A("modr", [3 * D])
        gnwbc = A("gnwbc", [768]); dbc = A("dbc", [256])
        PERS_END = A.off

        for dst, src, nm in [(ident32, c_ident, "ident32"), (maskT, c_maskT, "maskT"), (triS, c_triS, "triS"),
                             (revS, c_revS, "revS"), (allS, c_allS, "allS"), (sel127, c_sel127, "sel127"),
                             (retE, c_retE, "retE"), (kcol, c_kcol, "kcol")]:
            p.dma("sp", dst, src, writes=[nm])
        p.dma("sp", retdec[0:48, :], c_retdec, writes=["retdec"])
        p.dma("sp", stri, c_stri, writes=["stri"])
        p.dma("sp", idxc, c_idxc, writes=["idxc"])
        p.dma("pool", ident16, c_ident, writes=["ident16"])
        p.dma("pool", tri16, c_maskT, writes=["tri16"])
        p.op("pool", lambda e: e.memset(nh, -0.5), writes=["nh"])
        p.op("pool", lambda e: e.memset(ones16, 1.0), writes=["ones16"])
        p.op("pool", lambda e: e.memset(ones32, 1.0), writes=["ones32"])
        ctmp = A("ctmp", [KC]); cth = A("cth", [KC])
        p.dma("sp", ctmp, c_in, writes=["ctmp"])
        act(cth, ctmp, AF.Tanh, ["ctmp"], ["cth"], scale=0.5)
        ts("dve", cth, cth, 1.0, 0.5, ALU.add, ALU.mult, ["cth"], ["cth"])
        tt("dve", condT, cth, ctmp, ALU.mult, ["cth", "ctmp"], ["condT"])
        PERS_END = A.off

        def compute_mod(l, which, Aa):
            condbc = Aa("condbc", [KC, 128])
            wbuf = [Aa("wmodbuf0", [KC, 256]), Aa("wmodbuf1", [KC, 256])]
            brow = [Aa("brow0", [256], parts=1), Aa("brow1", [256], parts=1)]
            nwbc = Aa("nwbc", [D])
            for k in range(KC):
                cp("dve", condbc[:, k, :], condT[:, k:k + 1].to_broadcast([128, 128]), ["condT"], [("condbc", k)])
            p.dma("sp", nwbc, nw[l, :, which * D:(which + 1) * D].partition_broadcast(128), writes=["nwbc"])
            for n in range(12):
                c0 = which * 3 * D + n * 256
                wb = wbuf[n % 2]
                wn = ("wmodbuf", n % 2)
                p.dma("sp", brow[n % 2][0:1, :], b_mod[l, :, c0:c0 + 256], writes=[("brow", n % 2)])
                for k in range(KC):
                    p.dma("sp" if k % 2 == 0 else "act", wb[:, k, :], w_mod[l, k * 128:(k + 1) * 128, c0:c0 + 256], writes=[(wn, k)])
                b = nextbank()
                for k in range(KC):
                    mm(bank(b)[:, 0:256], condbc[:, k, :], wb[:, k, :], k == 0, False, [("condbc", k), (wn, k)], [B(b)])
                mm(bank(b)[:, 0:256], ones32[0:1, :], brow[n % 2][0:1, :], False, True, ["ones32", ("brow", n % 2)], [B(b)])
                seg = n // 4
                q4 = n % 4
                sl = slice(q4 * 256, (q4 + 1) * 256)
                if seg == 0:
                    cp("act", modr[:, D + q4 * 256:D + (q4 + 1) * 256], bank(b)[:, 0:256], [B(b)], [("modr", 1)])
                elif seg == 1:
                    stt(modr[:, sl], bank(b)[:, 0:256], 1.0, nwbc[:, sl], ALU.add, ALU.mult, [B(b), "nwbc"], [("modr", 0)])
                else:
                    cp("act", modr[:, 2 * D + q4 * 256:2 * D + (q4 + 1) * 256], bank(b)[:, 0:256], [B(b)], [("modr", 2)])
        MODR = lambda s: [("modr", s)]

        def norm_mod(xt, xt_res, hdn_out, hdn_res, scr, scr_res, ss, rstd, ss_res="ss", rstd_res="rstd"):
            act(scr, xt, AF.Square, [xt_res], [scr_res, ss_res], accum_out=ss)
            ts("pool", rstd, ss, 1.0 / D, EPS, ALU.mult, ALU.add, [ss_res], [rstd_res])
            tt("pool", rstd, rstd, nh[:, 0:1], ALU.pow, [rstd_res, "nh"], [rstd_res])
            stt(scr, xt, rstd, modr[:, 0:D], ALU.mult, ALU.mult, [xt_res, rstd_res] + MODR(0), [scr_res])
            tt("pool", hdn_out, scr, modr[:, D:2 * D], ALU.add, [scr_res] + MODR(1), [hdn_res])

        def mixer_phase(l, src, dst):
            Aa = Alloc(PERS_END, ARENA)
            win16 = Aa("win16", [KC, NCOL], BF16)
            wout16 = Aa("wout16", [KC, D], BF16)
            bblk16 = Aa("bblk16", [2, 2048], BF16)
            cst16 = Aa("cst16", [16, 16], BF16)
            wglu16 = Aa("wglu16", [2, 256], BF16)
            bglu16 = Aa("bglu16", [256], BF16, parts=1)
            wa2b_sb = Aa("wa2b_sb", [192], parts=17)
            Tn_c = Aa("Tn_c", [1024]); Tn_s = Aa("Tn_s", [1024]); Tp_c = Aa("Tp_c", [1024]); Tp_s = Aa("Tp_s", [1024])
            MIX_W_END = Aa.off
            for k in range(KC):
                p.dma("pool", win16[:, k, :], w_in_r[l, k * 128:(k + 1) * 128, :], writes=[("win16", k)])
            for k in range(KC):
                p.dma("pool", wout16[:, k, :], w_out[l, k * 128:(k + 1) * 128, :], writes=[("wout16", k)])
            for k in range(2):
                p.dma("pool", bblk16[:, k, :], bblk[l, k * 128:(k + 1) * 128, :], writes=["bblk16"])
                p.dma("pool", wglu16[:, k, :], wglu[l, k * 128:(k + 1) * 128, :], writes=["wglu16"])
            p.dma("pool", bglu16[0:1, :], bglu[l], writes=["bglu16"])
            p.dma("sp", wa2b_sb[0:17, :], wa2b[l], writes=["wa2b"])
            p.dma("sp", gnwbc, gnw[l].partition_broadcast(128), writes=["gnwbc"])
            p.dma("sp", dbc, s5d[l].partition_broadcast(128), writes=["dbc"])
            ts("pool", gnwbc, gnwbc, 0.5 * math.sqrt(96.0), None, ALU.mult, None, ["gnwbc"], ["gnwbc"])

            At = Alloc(MIX_W_END, ARENA)
            cst32 = At("cst32", [16, 16])
            p.dma("sp", cst32, cst[l], writes=["cst32"])
            cp("dve", cst16[0:64], cst32[0:64], ["cst32"], ["cst16a"])
            ts("dve", cst16[64:128], cst32[64:128], -1.0, None, ALU.mult, None, ["cst32"], ["cst16b"])
            compute_mod(l, 0, At)
            At = Alloc(At.off, ARENA)
            rows = At("s5rows_sb", [3072])
            p.dma("sp", rows, s5rows[l].partition_broadcast(128), writes=["rows"])
            are = rows[:, 0:1024]; aim = rows[:, 1024:2048]; ldt = rows[:, 2048:3072]
            wre = At("wre", [1024]); wim = At("wim", [1024]); t0 = At("t0", [1024]); t1 = At("t1", [1024])
            t2 = At("t2", [1024]); t3 = At("t3", [1024]); ti = At("ti", [1024], I32)
            cr = At("cr", [1024]); ci = At("ci", [1024])
            act(t0, ldt, AF.Exp, ["rows"], ["t0"])
            tt("dve", wre, are, t0, ALU.mult, ["rows", "t0"], ["wre"])
            tt("dve", wim, aim, t0, ALU.mult, ["rows", "t0"], ["wim"])

            def sincos(ang_ap, ang_res, s_out, s_res, c_out, c_res):
                ts("dve", ti, ang_ap, 1.0 / (2 * math.pi), None, ALU.mult, None, [ang_res], ["ti"])
                cp("dve", t3, ti, ["ti"], ["t3"])
                stt(t3, t3, -2 * math.pi, ang_ap, ALU.mult, ALU.add, ["t3", ang_res], ["t3"])
                ts("dve", t3, t3, math.pi, -math.pi, ALU.min, ALU.max, ["t3"], ["t3"])
                act(s_out, t3, AF.Sin, ["t3"], [s_res])
                ts("dve", t3, t3, math.pi / 2, None, ALU.add, None, ["t3"], ["t3"])
                ts("dve", t2, t3, math.pi, 2 * math.pi, ALU.is_gt, ALU.mult, ["t3"], ["t2"])
                tt("dve", t3, t3, t2, ALU.subtract, ["t3", "t2"], ["t3"])
                ts("dve", t3, t3, math.pi, -math.pi, ALU.min, ALU.max, ["t3"], ["t3"])
                act(c_out, t3, AF.Sin, ["t3"], [c_res])

            m1 = At("m1", [1024]); c1 = At("c1", [1024]); s1 = At("s1", [1024])
            act(m1, wre, AF.Exp, ["wre"], ["m1"])
            sincos(wim, "wim", s1, "s1", c1, "c1")
            tt("dve", c1, c1, m1, ALU.mult, ["c1", "m1"], ["c1"])
            ts("dve", c1, c1, -1.0, None, ALU.add, None, ["c1"], ["c1"])
            tt("dve", s1, s1, m1, ALU.mult, ["s1", "m1"], ["s1"])
            tt("dve", t0, are, are, ALU.mult, ["rows"], ["t0"])
            tt("dve", t1, aim, aim, ALU.mult, ["rows"], ["t1"])
            tt("dve", t0, t0, t1, ALU.add, ["t0", "t1"], ["t0"])
            p.op("dve", lambda e: e.reciprocal(t0, t0), reads=["t0"], writes=["t0"])
            tt("dve", cr, c1, are, ALU.mult, ["c1", "rows"], ["cr"])
            tt("dve", t1, s1, aim, ALU.mult, ["s1", "rows"], ["t1"])
            tt("dve", cr, cr, t1, ALU.add, ["cr", "t1"], ["cr"])
            tt("dve", cr, cr, t0, ALU.mult, ["cr", "t0"], ["cr"])
            tt("dve", ci, s1, are, ALU.mult, ["s1", "rows"], ["ci"])
            tt("dve", t1, c1, aim, ALU.mult, ["c1", "rows"], ["t1"])
            tt("dve", ci, ci, t1, ALU.subtract, ["ci", "t1"], ["ci"])
            tt("dve", ci, ci, t0, ALU.mult, ["ci", "t0"], ["ci"])
            ang = m1; sn = s1; cs = c1; mp = At("mp", [1024]); mn = At("mn", [1024])
            act(mp, wre, AF.Exp, ["wre", "kcol"], ["mp"], scale=kcol[:, 0:1])
            act(mn, wre, AF.Exp, ["wre", "kcol"], ["mn"], scale=kcol[:, 1:2])
            ts("dve", ang, wim, kcol[:, 0:1], None, ALU.mult, None, ["wim", "kcol"], ["m1"])
            sincos(ang, "m1", sn, "s1", cs, "c1")
            tt("dve", Tp_c, mp, cs, ALU.mult, ["mp", "c1"], ["Tp_c"])
            tt("dve", Tp_s, mp, sn, ALU.mult, ["mp", "s1"], ["Tp_s"])
            tt("dve", t0, mn, cs, ALU.mult, ["mn", "c1"], ["t0"])
            tt("dve", t1, mn, sn, ALU.mult, ["mn", "s1"], ["t1"])
            tt("dve", Tn_c, t0, cr, ALU.mult, ["t0", "cr"], ["Tn_c"])
            tt("dve", t2, t1, ci, ALU.mult, ["t1", "ci"], ["t2"])
            tt("dve", Tn_c, Tn_c, t2, ALU.add, ["Tn_c", "t2"], ["Tn_c"])
            tt("dve", Tn_s, t0, ci, ALU.mult, ["t0", "ci"], ["Tn_s"])
            tt("dve", t2, t1, cr, ALU.mult, ["t1", "cr"], ["t2"])
            tt("dve", Tn_s, Tn_s, t2, ALU.subtract, ["Tn_s", "t2"], ["Tn_s"])
            p.barrier()

            Ab = Alloc(MIX_W_END, ARENA)
            xt = [Ab("xt0", [D]), Ab("xt1", [D])]
            F1 = Ab("F1", [D])
            hdn16 = Ab("hdn16", [D], BF16)
            hT16 = Ab("hT16", [KC, 128], BF16)
            mT16 = Ab("mT16", [KC, 128], BF16)
            mixed16 = Ab("mixed16", [D], BF16)
            ss = Ab("ss", [1]); rstd = Ab("rstd", [1])
            rope_sb2 = [Ab("rope_sb0", [2, 24]), Ab("rope_sb1", [2, 24])]
            gqk2 = [Ab("gqk0", [400]), Ab("gqk1", [400])]; rqk2 = [Ab("rqk0", [384]), Ab("rqk1", [384])]
            v162 = [[Ab("gv16_0", [384], BF16), Ab("rv16_0", [384], BF16)], [Ab("gv16_1", [384], BF16), Ab("rv16_1", [384], BF16)]]
            sgm2 = [[Ab("gsg0", [384]), Ab("rsg0", [384])], [Ab("gsg1", [384]), Ab("rsg1", [384])]]
            th = Ab("th", [384]); sq = [th, th]
            gaT = Ab("gaT", [128], parts=17)
            e1 = Ab("e1", [192]); sp_ = Ab("sp", [192])
            Eq = Ab("Eq", [192]); Ek = Ab("Ek", [192]); Eend = Ab("Eend", [192])
            dec = Ab("dec", [4], parts=48)
            qd16 = [Ab("qd16g", [192], BF16), Ab("qd16r", [192], BF16)]
            ki16 = [Ab("ki16g", [192], BF16), Ab("ki16r", [192], BF16)]
            ke16 = [Ab("ke16g", [192], BF16), Ab("ke16r", [192], BF16)]
            qkT16 = [Ab("qkT16g", [8, 128], BF16, parts=48), Ab("qkT16r", [8, 128], BF16, parts=48)]
            sc16 = [Ab("sc16g", [4, 128], BF16), Ab("sc16r", [4, 128], BF16)]
            S32 = [Ab("S32g", [4, 96], parts=48), Ab("S32r", [4, 96], parts=48)]
            S16 = [Ab("S16g", [4, 96], BF16, parts=48), Ab("S16r", [4, 96], BF16, parts=48)]
            o_sb = [Ab("o_sbg", [384]), Ab("o_sbr", [384])]
            st4 = [Ab("st4g", [4]), Ab("st4r", [4])]
            st4b = Ab("st4b", [4])
            rot = Ab("rot", [384]); ra = Ab("ra", [192]); rb = Ab("rb", [192])
            u_sb2 = [Ab("u_sb0", [256]), Ab("u_sb1", [256])]; u162 = [Ab("u16_0", [256], BF16), Ab("u16_1", [256], BF16)]
            uT16 = Ab("uT16", [2, 128], BF16)
            print("mixer SBUF end", Ab.off, ARENA)
            c1t2 = [Ab("c1t0", [512]), Ab("c1t1", [512])]; c2t2 = [Ab("c2t0", [512]), Ab("c2t1", [512])]
            z162 = [Ab("z16_0", [1024], BF16), Ab("z16_1", [1024], BF16)]
            s32 = [Ab("s32a", [2048]), Ab("s32b", [2048])]
            sT16 = Ab("sT16", [16, 128], BF16)
            ya = Ab("ya", [256]); yb = Ab("yb", [256]); yc = Ab("yc", [256]); gy16 = Ab("gy16", [256], BF16)
            gyT16 = Ab("gyT16", [2, 128], BF16)

            for i in range(2):
                p.op("pool", lambda e, i=i: e.memset(S32[i][0:48], 0.0), writes=[("S32", i)])
                p.op("pool", lambda e, i=i: e.memset(S16[i][0:48], 0.0), writes=[("S16", i)])
            p.op("pool", lambda e: e.memset(gaT[0:17, :], 1.0), writes=["gaT"])

            def mkrot(banks):
                st_ = [0]

                def nxt():
                    b_ = banks[st_[0] % len(banks)]
                    st_[0] += 1
                    return b_
                return nxt
            rotG = mkrot([0]); rotR = mkrot([2]); rotP = mkrot([1, 0])
            TB3 = 3

            def attn_core(mi, rotm, dec_ap, dec_res, nw_off, mix_off, centered, pp):
                QT = qkT16[mi]; QR = ("qkT16", mi)
                vv = v162[pp][mi]; VR = ("v16", pp, mi)
                bs = rotm()
                for h in range(4):
                    mm(bank(bs)[:, h * 128:(h + 1) * 128], QT[0:48, 4 + h, :], QT[0:48, h, :], True, True, [QR], [B(bs)])
                tt("dve", sc16[mi], bank(bs).rearrange("p (h i) -> p h i", h=4), maskT.unsqueeze(1).to_broadcast([128, 4, 128]),
                   ALU.mult, [B(bs), "maskT"], [("sc16", mi)])
                yield
                bo = rotm()
                for h in range(4):
                    mm(bank(bo)[:, h * 96:(h + 1) * 96], sc16[mi][:, h, :], vv[:, h * 96:(h + 1) * 96], True, False, [("sc16", mi), VR], [B(bo)])
                    mm(bank(bo)[:, h * 96:(h + 1) * 96], QT[0:48, h, :], S16[mi][0:48, h, :], False, True, [QR, ("S16", mi)], [B(bo)])
                O = o_sb[mi]; OR_ = ("o_sb", mi)
                cp("act", O, bank(bo)[:, 0:384], [B(bo)], [OR_])
                yield
                bd = rotm()
                for h in range(4):
                    mm(bank(bd)[0:48, h * 96:(h + 1) * 96], ke16[mi][:, h * 48:(h + 1) * 48], vv[:, h * 96:(h + 1) * 96], True, True, [("ke16", mi), VR], [B(bd)])
                tt("pool", S32[mi][0:48], S32[mi][0:48], dec_ap[0:48].unsqueeze(2).to_broadcast([48, 4, 96]), ALU.mult,
                   [("S32", mi), dec_res], [("S32", mi)])
                tt("dve", S32[mi][0:48], S32[mi][0:48], bank(bd)[0:48, 0:384].rearrange("p (h v) -> p h v", h=4), ALU.add,
                   [("S32", mi), B(bd)], [("S32", mi)])
                cp("act", S16[mi][0:48], S32[mi][0:48], [("S32", mi)], [("S16", mi)])
                yield
                o3 = O.rearrange("p (h v) -> p h v", h=4)
                if centered:
                    p.op("dve", lambda e: e.tensor_reduce(st4b, o3, AX.X, ALU.add), reads=[OR_], writes=["st4b"])
                    ts("pool", st4b, st4b, -1.0 / 96.0, None, ALU.mult, None, ["st4b"], ["st4b"])
                    tt("dve", o3, o3, st4b.unsqueeze(2).to_broadcast([128, 4, 96]), ALU.add, [OR_, "st4b"], [OR_])
                    yield
                SQ = sq[mi]; S4 = st4[mi]
                act(SQ, O, AF.Square, [OR_], ["th"])
                p.op("dve", lambda e: e.tensor_reduce(S4, SQ.rearrange("p (h v) -> p h v", h=4), AX.X, ALU.add), reads=["th"], writes=[("st4", mi)])
                ts("pool", S4, S4, 96.0 * EPS, None, ALU.add, None, [("st4", mi)], [("st4", mi)])
                tt("pool", S4, S4, nh, ALU.pow, [("st4", mi), "nh"], [("st4", mi)])
                yield
                tt("dve", o3, o3, S4.unsqueeze(2).to_broadcast([128, 4, 96]), ALU.mult, [OR_, ("st4", mi)], [OR_])
                tt("pool", mixed16[:, mix_off:mix_off + 384], O, sgm2[pp][mi], ALU.mult, [OR_, ("sg", pp, mi)], [("mixed16", mix_off)])

            def gate_prep(gbank, mi, nw_off, pp):
                act(th, bank(gbank)[:, 0:384], AF.Tanh, [B(gbank)], ["th"], scale=0.5)
                stt(sgm2[pp][mi], th, 1.0, bank(gbank)[:, 0:384], ALU.add, ALU.mult, ["th", B(gbank)], [("sg", pp, mi)])
                tt("pool", sgm2[pp][mi], sgm2[pp][mi], gnwbc[:, nw_off:nw_off + 384], ALU.mult, [("sg", pp, mi), "gnwbc"], [("sg", pp, mi)])

            def transposes_qk(mi):
                bt = TB3
                for h in range(4):
                    tr(bankbf(bt)[0:48, h * 128:(h + 1) * 128], qd16[mi][:, h * 48:(h + 1) * 48], ident16, [("qd16", mi), "ident16"], [B(bt)])
                    tr(bankbf(bt)[0:48, (4 + h) * 128:(5 + h) * 128], ki16[mi][:, h * 48:(h + 1) * 48], ident16, [("ki16", mi), "ident16"], [B(bt)])
                cp("act", qkT16[mi][0:48].rearrange("p a b -> p (a b)"), bankbf(bt)[0:48, :], [B(bt)], [("qkT16", mi)])

            def chain_gla(t):
                pp = t % 2
                gqk = gqk2[pp]; GQ = ("gqk", pp)
                bz = rotG()
                tr(bank(bz)[0:16, 0:128], gqk[:, 384:400], ident32, [GQ, "ident32"], [B(bz)])
                cp("act", gaT[0:16, :], bank(bz)[0:16, 0:128], [B(bz)], ["gaT"])
                yield
                bz2 = rotG()
                mm(bank(bz2)[:, 0:192], gaT[0:17, :], wa2b_sb[0:17, :], True, True, ["gaT", "wa2b"], [B(bz2)])
                act(e1, bank(bz2)[:, 0:192], AF.Exp, [B(bz2)], ["e1"], scale=-1.0)
                act(sp_, e1, AF.Ln, ["e1"], ["sp"], bias=1.0, scale=1.0)
                yield
                bc_ = rotG()
                mm(bank(bc_)[:, 0:192], triS, sp_, True, True, ["triS", "sp"], [B(bc_)])
                mm(bank(bc_)[:, 192:384], revS, sp_, True, True, ["revS", "sp"], [B(bc_)])
                for h in range(4):
                    mm(bank(bc_)[0:48, 384 + h:385 + h], sp_[:, h * 48:(h + 1) * 48], allS, True, True, ["sp", "allS"], [B(bc_)])
                act(Eq, bank(bc_)[:, 0:192], AF.Exp, [B(bc_)], ["Eq"])
                act(Ek, bank(bc_)[:, 0:192], AF.Exp, [B(bc_)], ["Ek"], scale=-1.0)
                act(Eend, bank(bc_)[:, 192:384], AF.Exp, [B(bc_)], ["Eend"])
                act(dec[0:48], bank(bc_)[0:48, 384:388], AF.Exp, [B(bc_)], ["dec"])
                yield
                stt(qd16[0], gqk[:, 0:192], 48.0 ** -0.5, Eq, ALU.mult, ALU.mult, [GQ, "Eq"], [("qd16", 0)])
                tt("pool", ki16[0], gqk[:, 192:384], Ek, ALU.mult, [GQ, "Ek"], [("ki16", 0)])
                tt("pool", ke16[0], gqk[:, 192:384], Eend, ALU.mult, [GQ, "Eend"], [("ke16", 0)])
                yield
                transposes_qk(0)
                yield
                yield from attn_core(0, rotG, dec, "dec", 0, 0, False, pp)

            def chain_ret(t):
                pp = t % 2
                rqk = rqk2[pp]; rope_sb = rope_sb2[pp]
                x4 = rqk.rearrange("p (a c d) -> p a c d", a=8, c=2)
                r4 = rot.rearrange("p (a c d) -> p a c d", a=8, c=2)
                cosb = rope_sb[:, 0, :].unsqueeze(1).to_broadcast([128, 8, 24])
                sinb = rope_sb[:, 1, :].unsqueeze(1).to_broadcast([128, 8, 24])
                ra3 = ra.rearrange("p (a d) -> p a d", a=8)
                rb3 = rb.rearrange("p (a d) -> p a d", a=8)
                tt("dve", ra3, x4[:, :, 0, :], cosb, ALU.mult, [("rqk", pp), ("rope_sb", pp)], ["ra"])
                tt("pool", rb3, x4[:, :, 1, :], sinb, ALU.mult, [("rqk", pp), ("rope_sb", pp)], ["rb"])
                tt("dve", r4[:, :, 0, :], ra3, rb3, ALU.subtract, ["ra", "rb"], ["rot"])
                yield
                tt("dve", ra3, x4[:, :, 0, :], sinb, ALU.mult, [("rqk", pp), ("rope_sb", pp)], ["ra"])
                tt("pool", rb3, x4[:, :, 1, :], cosb, ALU.mult, [("rqk", pp), ("rope_sb", pp)], ["rb"])
                tt("dve", r4[:, :, 1, :], ra3, rb3, ALU.add, ["ra", "rb"], ["rot"])
                yield
                tt("dve", qd16[1], rot[:, 0:192], retE[:, 0, :], ALU.mult, ["rot", "retE"], [("qd16", 1)])
                tt("pool", ki16[1], rot[:, 192:384], retE[:, 1, :], ALU.mult, ["rot", "retE"], [("ki16", 1)])
                tt("pool", ke16[1], rot[:, 192:384], retE[:, 2, :], ALU.mult, ["rot", "retE"], [("ke16", 1)])
                yield
                transposes_qk(1)
                yield
                yield from attn_core(1, rotR, retdec, "retdec", 384, 384, True, pp)

            Tnc = Tn_c.rearrange("p (g s) -> p g s", g=16); Tns = Tn_s.rearrange("p (g s) -> p g s", g=16)
            Tpc = Tp_c.rearrange("p (g s) -> p g s", g=16); Tps = Tp_s.rearrange("p (g s) -> p g s", g=16)
            def cmul(hf, src4, src_res, dst4, dst_res, Tc, Ts, Tres):
                c1v = c1t2[hf].rearrange("p (g s) -> p g s", g=8)
                c2v = c2t2[hf].rearrange("p (g s) -> p g s", g=8)
                C1 = ("c1t", hf); C2 = ("c2t", hf)
                tt("dve", c1v, src4[:, :, 0, :], Tc, ALU.mult, src_res + Tres, [C1])
                tt("dve", c2v, src4[:, :, 1, :], Ts, ALU.mult, src_res + Tres, [C2])
                tt("pool", dst4[:, :, 0, :], c1v, c2v, ALU.subtract, [C1, C2], [dst_res])
                tt("dve", c1v, src4[:, :, 0, :], Ts, ALU.mult, src_res + Tres, [C1])
                tt("dve", c2v, src4[:, :, 1, :], Tc, ALU.mult, src_res + Tres, [C2])
                tt("pool", dst4[:, :, 1, :], c1v, c2v, ALU.add, [C1, C2], [dst_res])

            def s5_half(t, hf):
                scur = s32[t % 2]
                sprev = s32[(t + 1) % 2]
                b0 = 6 if hf == 0 else 4
                BIG = [B(b0), B(b0 + 1)]
                big = psum[:, b0 * 512:(b0 + 2) * 512].rearrange("p (g c s) -> p g c s", g=8, c=2)
                z16 = z162[hf]; ZR = ("z16", hf)
                z4 = z16.rearrange("p (g c s) -> p g c s", g=8, c=2)
                c0 = hf * 1024
                SR = ("s32", t % 2, hf)
                for n in range(2):
                    mm(bank(b0 + n), uT16[:, hf, :], bblk16[:, hf, c0 + n * 512:c0 + (n + 1) * 512], True, True, ["uT16", "bblk16"], [B(b0 + n)])
                cmul(hf, big, BIG, z4, ZR, Tnc[:, hf * 8:(hf + 1) * 8, :], Tns[:, hf * 8:(hf + 1) * 8, :], ["Tn_c", "Tn_s"])
                yield
                for n in range(2):
                    mm(bank(b0 + n), tri16, z16[:, n * 512:(n + 1) * 512], True, t == 0, ["tri16", ZR], [B(b0 + n)])
                    if t > 0:
                        mm(bank(b0 + n), sel127, sprev[:, c0 + n * 512:c0 + (n + 1) * 512], False, True, ["sel127", ("s32", (t + 1) % 2, hf)], [B(b0 + n)])
                cmul(hf, big, BIG, scur[:, c0:c0 + 1024].rearrange("p (g c s) -> p g c s", g=8, c=2), SR,
                     Tpc[:, hf * 8:(hf + 1) * 8, :], Tps[:, hf * 8:(hf + 1) * 8, :], ["Tp_c", "Tp_s"])
                yield
                for g in range(8):
                    tr(bank(b0 + g // 4)[:, (g % 4) * 128:(g % 4 + 1) * 128], scur[:, c0 + g * 128:c0 + (g + 1) * 128], ident32,
                       [SR, "ident32"], [B(b0 + g // 4)])
                sTf = sT16.rearrange("p a b -> p (a b)")
                cp("act", sTf[:, c0:c0 + 512], bank(b0), [B(b0)], [("sT16", hf)])
                cp("dve", sTf[:, c0 + 512:c0 + 1024], bank(b0 + 1), [B(b0 + 1)], [("sT16", hf)])

            def chain_s5(t):
                pp = t % 2
                u_sb = u_sb2[pp]; u16 = u162[pp]
                bt = TB3
                for k in range(2):
                    tr(bankbf(bt)[:, k * 128:(k + 1) * 128], u16[:, k * 128:(k + 1) * 128], ident16, [("u16", pp), "ident16"], [B(bt)])
                cp("act", uT16.rearrange("p a b -> p (a b)"), bankbf(bt)[:, 0:256], [B(bt)], ["uT16"])
                yield
                halves = [s5_half(t, 0), s5_half(t, 1)]
                while halves:
                    for h_ in list(halves):
                        try:
                            next(h_)
                        except StopIteration:
                            halves.remove(h_)
                    yield
                by = 6
                for g in range(16):
                    mm(bank(by)[:, g * 16:(g + 1) * 16], sT16[:, g, :], cst16[:, g, :], True, True,
                       [("sT16", g // 8), "cst16a", "cst16b"], [B(by)])
                tt("pool", ya, u_sb, dbc, ALU.mult, [("u_sb", pp), "dbc"], ["ya"])
                tt("dve", ya, ya, bank(by)[:, 0:256], ALU.add, ["ya", B(by)], ["ya"])
                yield
                tt("pool", yb, ya, ya, ALU.mult, ["ya"], ["yb"])
                ts("pool", yb, yb, 0.044715, 1.0, ALU.mult, ALU.add, ["yb"], ["yb"])
                tt("dve", yb, yb, ya, ALU.mult, ["yb", "ya"], ["yb"])
                act(yc, yb, AF.Tanh, ["yb"], ["yc"], scale=math.sqrt(2.0 / math.pi))
                yield
                ts("pool", yc, yc, 1.0, 0.5, ALU.add, ALU.mult, ["yc"], ["yc"])
                tt("dve", ya, ya, yc, ALU.mult, ["ya", "yc"], ["ya"])
                cp("dve", gy16, ya, ["ya"], ["gy16"])
                yield
                for k in range(2):
                    tr(bankbf(bt)[:, k * 128:(k + 1) * 128], gy16[:, k * 128:(k + 1) * 128], ident16, ["gy16", "ident16"], [B(bt)])
                cp("act", gyT16.rearrange("p a b -> p (a b)"), bankbf(bt)[:, 0:256], [B(bt)], ["gyT16"])
                yield
                bgl = 7
                for k in range(2):
                    mm(bank(bgl)[:, 0:256], gyT16[:, k, :], wglu16[:, k, :], k == 0, False, ["gyT16", "wglu16"], [B(bgl)])
                mm(bank(bgl)[:, 0:256], ones16[0:1, :], bglu16[0:1, :], False, True, ["ones16", "bglu16"], [B(bgl)])
                act(yc, bank(bgl)[:, 0:256], AF.Tanh, [B(bgl)], ["yc"], scale=0.5)
                ts("pool", yc, yc, 1.0, 0.5, ALU.add, ALU.mult, ["yc"], ["yc"])
                tt("dve", mixed16[:, 768:1024], ya, yc, ALU.mult, ["ya", "yc"], [("mixed16", 768)])

            rotF = mkrot([1])

            def front(t):
                pp = t % 2
                X = xt[pp]; XR = ("xt", pp)
                p.dma("sp", X, src[t * 128:(t + 1) * 128, :], writes=[XR])
                p.dma("sp", rope_sb2[pp], c_rope[:, t], writes=[("rope_sb", pp)])
                norm_mod(X, XR, hdn16, "hdn16", F1, "F1", ss, rstd)
                yield
                bt = TB3
                for k in range(KC):
                    tr(bankbf(bt)[:, k * 128:(k + 1) * 128], hdn16[:, k * 128:(k + 1) * 128], ident16, ["hdn16", "ident16"], [B(bt)])
                cp("act", hT16.rearrange("p a b -> p (a b)"), bankbf(bt), [B(bt)], ["hT16"])
                yield

                def proj(cols):
                    b_ = rotF()
                    n = cols[1] - cols[0]
                    for k in range(KC):
                        mm(bank(b_)[:, 0:n], hT16[:, k, :], win16[:, k, cols[0]:cols[1]], k == 0, k == KC - 1, ["hT16", ("win16", k)], [B(b_)])
                    return b_

                b1 = proj(G1)
                cp("act", gqk2[pp], bank(b1)[:, 0:400], [B(b1)], [("gqk", pp)])
                yield
                b1 = proj(R1)
                cp("act", rqk2[pp], bank(b1)[:, 0:384], [B(b1)], [("rqk", pp)])
                yield
                b1 = proj(S1)
                cp("act", u_sb2[pp], bank(b1)[:, 0:256], [B(b1)], [("u_sb", pp)])
                cp("dve", u162[pp], bank(b1)[:, 0:256], [B(b1)], [("u16", pp)])
                yield
                b2 = proj(G2)
                cp("dve", v162[pp][0], bank(b2)[:, 0:384], [B(b2)], [("v16", pp, 0)])
                yield
                b2 = proj(R2)
                cp("dve", v162[pp][1], bank(b2)[:, 0:384], [B(b2)], [("v16", pp, 1)])
                yield
                bg = proj(G3)
                gate_prep(bg, 0, 0, pp)
                yield
                bg = proj(R3)
                gate_prep(bg, 1, 384, pp)

            for _ in front(0):
                pass
            for t in range(n_tiles):
                X = xt[t % 2]
                XR = ("xt", t % 2)
                bt = TB3
                gens = [chain_gla(t), chain_ret(t), chain_s5(t)]
                if t + 1 < n_tiles:
                    gens.append(front(t + 1))
                while gens:
                    for g_ in list(gens):
                        try:
                            next(g_)
                        except StopIteration:
                            gens.remove(g_)

                MX = [("mixed16", 0), ("mixed16", 384), ("mixed16", 768)]
                for k in range(KC):
                    tr(bankbf(bt)[:, k * 128:(k + 1) * 128], mixed16[:, k * 128:(k + 1) * 128], ident16, MX + ["ident16"], [B(bt)])
                cp("act", mT16.rearrange("p a b -> p (a b)"), bankbf(bt), [B(bt)], ["mT16"])
                for half in range(2):
                    b_ = rotP()
                    for k in range(KC):
                        mm(bank(b_), mT16[:, k, :], wout16[:, k, half * 512:(half + 1) * 512], k == 0, k == KC - 1, ["mT16", ("wout16", k)], [B(b_)])
                    tt("dve", F1[:, half * 512:(half + 1) * 512], bank(b_), modr[:, 2 * D + half * 512:2 * D + (half + 1) * 512], ALU.mult,
                       [B(b_), ("modr", 2)], ["F1"])
                tt("pool", F1, F1, X, ALU.add, ["F1", XR], ["F1"])
                tk = p.dma("sp", dst[t * 128:(t + 1) * 128, :], F1, reads=["F1"], writes=[("dst", t)])
                if stop == "mix":
                    p.out_toks.append(tk)
            p.barrier()

        def moe_phase(l, src, dst, final):
            Aa = Alloc(PERS_END, ARENA)
            wup = [Aa("wup0", [KC, 2 * D], BF16), Aa("wup1", [KC, 2 * D], BF16)]
            wdn = Aa("wdn", [KC, D], BF16)
            hT16 = Aa("mhT16", [KC, TB], BF16)
            acc = Aa("acc", [NTB, D])
            actT = Aa("actT", [KC, 512], BF16)
            rw32 = Aa("rw32", [KC, NE])
            rb32 = Aa("rb32", [NE], parts=1)
            bup = Aa("bup", [NE, 16])
            bup1 = Aa("bup1", [NE, 8])
            bdn = Aa("bdn", [D], parts=32)
            gates = Aa("gates", [NTB, NE])
            gT = Aa("gT", [128], parts=32)
            MOE_W_END = Aa.off
            for k in range(KC):
                p.dma("sp", rw32[:, k, :], router_w[l, k * 128:(k + 1) * 128, :], writes=["rw32"])
            p.dma("sp", rb32[0:1, :], router_b[l], writes=["rb32"])
            p.dma("sp", bup, b_upT[l], writes=["bup"])
            p.dma("sp", bdn[0:32, :], b_down[l], writes=["bdn"])
            ts("pool", bup1, bup[:, :, 8:16], 1.0, None, ALU.add, None, ["bup"], ["bup1"])
            At = Alloc(MOE_W_END, ARENA)
            if final:
                fnwbc = At("fnwbc", [D])
                p.dma("sp", fnwbc, fnw.partition_broadcast(128), writes=["fnwbc"])
            At2 = Alloc(At.off, ARENA)
            compute_mod(l, 1, At)
            p.barrier()
            Ab = At2
            xt = [Ab("mxt0", [D]), Ab("mxt1", [D])]
            F1 = Ab("mF1", [D]); F2 = Ab("mF2", [D])
            hT32 = Ab("hT32", [KC, 128])
            ss = Ab("mss", [1]); rstd = Ab("mrstd", [1])
            lg = Ab("lg", [NE]); mx8 = Ab("mx8", [8]); nm0 = Ab("nm0", [1]); ex = Ab("ex", [NE]); ssum = Ab("ssum", [1])
            gsb = Ab("gsb", [512]); sgb = Ab("sgb", [512]); lsb = Ab("lsb", [512])

            def load_expert(e, buf):
                for k in range(KC):
                    p.dma("pool", wup[buf][:, k, :], w_up[l, e, k * 128:(k + 1) * 128, :], writes=[("wup", buf, k)])

            def load_down(e):
                for k in range(KC):
                    p.dma("pool", wdn[:, k, :], w_down[l, e, k * 128:(k + 1) * 128, :], writes=[("wdn", k)])

            ntb = max(1, n_tiles // NTB)
            tiles_per_blk = min(NTB, n_tiles)
            for tb in range(ntb):
                load_expert(0, 0)
                load_down(0)
                for tl in range(tiles_per_blk):
                    t = tb * NTB + tl
                    X = xt[tl % 2]; XR = ("mxt", tl % 2)
                    p.dma("sp", X, src[t * 128:(t + 1) * 128, :], reads=[("dst", t)], writes=[XR])
                    if stage < 1:
                        continue
                    norm_mod(X, XR, F2, "mF2", F1, "mF1", ss, rstd)
                    if stage < 2:
                        continue
                    for hh in range(2):
                        b = nextbank()
                        for k4 in range(4):
                            k = hh * 4 + k4
                            tr(bank(b)[:, k4 * 128:(k4 + 1) * 128], F2[:, k * 128:(k + 1) * 128], ident32, ["mF2", "ident32"], [B(b)])
                        cp("act", hT32[:, hh * 4:(hh + 1) * 4, :].rearrange("p a b -> p (a b)"), bank(b), [B(b)], [("hT32", hh)])
                        for k4 in range(4):
                            k = hh * 4 + k4
                            cp("dve", hT16[:, k, tl * 128:(tl + 1) * 128], bank(b)[:, k4 * 128:(k4 + 1) * 128], [B(b)], [("mhT16", tl)])
                    if stage < 3:
                        continue
                    b = nextbank()
                    for k in range(KC):
                        mm(bank(b)[:, 0:NE], hT32[:, k, :], rw32[:, k, :], k == 0, False, [("hT32", k // 4), "rw32"], [B(b)])
                    mm(bank(b)[:, 0:NE], ones32[0:1, :], rb32[0:1, :], False, True, ["ones32", "rb32"], [B(b)])
                    cp("act", lg, bank(b)[:, 0:NE], [B(b)], ["lg"])
                    if stage < 4:
                        continue
                    p.op("dve", lambda e: e.max(out=mx8, in_=lg), reads=["lg"], writes=["mx8"])
                    ts("dve", nm0, mx8[:, 0:1], -1.0, None, ALU.mult, None, ["mx8"], ["nm0"])
                    act(ex, lg, AF.Exp, ["lg", "nm0"], ["ex"], bias=nm0, scale=1.0)
                    stt(ex, lg, mx8[:, 3:4], ex, ALU.is_ge, ALU.mult, ["lg", "mx8", "ex"], ["ex", "ssum"], accum_out=ssum)
                    p.op("dve", lambda e: e.reciprocal(ssum, ssum), reads=["ssum"], writes=["ssum"])
                    ts("dve", gates[:, tl, :], ex, ssum, None, ALU.mult, None, ["ex", "ssum"], [("gates", tl)])
                    if stage < 5:
                        continue
                    b = nextbank()
                    tr(bank(b)[0:32, 0:128], gates[:, tl, :], ident32, [("gates", tl), "ident32"], [B(b)])
                    cp("act", gT[0:32, :], bank(b)[0:32, 0:128], [B(b)], ["gT"])
                    for half in range(2):
                        b = nextbank()
                        mm(bank(b), gT[0:32, :], bdn[0:32, half * 512:(half + 1) * 512], True, True, ["gT", "bdn"], [B(b)])
                        cp("act", acc[:, tl, half * 512:(half + 1) * 512], bank(b), [B(b)], [("acc", tl, half)])
                nhalf = max(1, tiles_per_blk * 128 // 512)
                for e in range(n_exp):
                    buf = e % 2
                    if e + 1 < n_exp:
                        load_expert(e + 1, 1 - buf)
                    for hh in range(nhalf):
                        tok0 = hh * 512
                        ntok = min(512, tiles_per_blk * 128)
                        for cch in range(KC):
                            bg_ = nextbank(0, 8); bl_ = nextbank(0, 8)
                            for k in range(KC):
                                mm(bank(bg_)[:, 0:ntok], wup[buf][:, k, cch * 128:(cch + 1) * 128], hT16[:, k, tok0:tok0 + ntok], k == 0, k == KC - 1,
                                   [("wup", buf, k)] + [("mhT16", tok0 // 128 + i) for i in range(ntok // 128)], [B(bg_)])
                            for k in range(KC):
                                mm(bank(bl_)[:, 0:ntok], wup[buf][:, k, D + cch * 128:D + (cch + 1) * 128], hT16[:, k, tok0:tok0 + ntok], k == 0, k == KC - 1,
                                   [("wup", buf, k)] + [("mhT16", tok0 // 128 + i) for i in range(ntok // 128)], [B(bl_)])
                            ts("dve", gsb[:, 0:ntok], bank(bg_)[:, 0:ntok], bup[:, e, cch:cch + 1], 7.0, ALU.add, ALU.min, [B(bg_), "bup"], ["gsb"])
                            act(sgb[:, 0:ntok], gsb[:, 0:ntok], AF.Sigmoid, ["gsb"], ["sgb"], scale=1.702)
                            act(lsb[:, 0:ntok], bank(bl_)[:, 0:ntok], AF.Identity, [B(bl_), "bup1"], ["lsb"], bias=bup1[:, e, cch:cch + 1], scale=1.0)
                            ts("pool", lsb[:, 0:ntok], lsb[:, 0:ntok], 8.0, -6.0, ALU.min, ALU.max, ["lsb"], ["lsb"])
                            tt("pool", gsb[:, 0:ntok], gsb[:, 0:ntok], lsb[:, 0:ntok], ALU.mult, ["gsb", "lsb"], ["gsb"])
                            tt("dve", actT[:, cch, 0:ntok], sgb[:, 0:ntok], gsb[:, 0:ntok], ALU.mult, ["sgb", "gsb"], [("actT", cch)])
                        if hh == nhalf - 1 and e + 1 < n_exp:
                            pass
                        for tl4 in range(ntok // 128):
                            tl = hh * 4 + tl4
                            for half in range(2):
                                b = nextbank(0, 8)
                                for k in range(KC):
                                    mm(bank(b), actT[:, k, tl4 * 128:(tl4 + 1) * 128], wdn[:, k, half * 512:(half + 1) * 512], k == 0, k == KC - 1,
                                       [("actT", k), ("wdn", k)], [B(b)])
                                stt(acc[:, tl, half * 512:(half + 1) * 512], bank(b), gates[:, tl, e:e + 1], acc[:, tl, half * 512:(half + 1) * 512],
                                    ALU.mult, ALU.add, [B(b), ("gates", tl), ("acc", tl, half)], [("acc", tl, half)])
                    if e + 1 < n_exp:
                        load_down(e + 1)
                for tl in range(tiles_per_blk):
                    t = tb * NTB + tl
                    X = xt[tl % 2]; XR = ("mxt", tl % 2)
                    p.dma("sp", X, src[t * 128:(t + 1) * 128, :], reads=[("dst", t)], writes=[XR])
                    tt("dve", F1, acc[:, tl, :], modr[:, 2 * D:3 * D], ALU.mult, [("acc", tl, 0), ("acc", tl, 1)] + MODR(2), ["mF1"])
                    tt("pool", F1, F1, X, ALU.add, ["mF1", XR], ["mF1"])
                    if final:
                        act(F2, F1, AF.Square, ["mF1"], ["mF2", "ss"], accum_out=ss)
                        ts("pool", rstd, ss, 1.0 / D, EPS, ALU.mult, ALU.add, ["ss"], ["rstd"])
                        tt("pool", rstd, rstd, nh[:, 0:1], ALU.pow, ["rstd", "nh"], ["rstd"])
                        stt(F2, F1, rstd, fnwbc, ALU.mult, ALU.mult, ["mF1", "rstd", "fnwbc"], ["mF2"])
                        tk = p.dma("sp", dst[t * 128:(t + 1) * 128, :], F2, reads=["mF2"], writes=[("dst2", t)])
                    else:
                        tk = p.dma("sp", dst[t * 128:(t + 1) * 128, :], F1, reads=["mF1"], writes=[("dst2", t)])
                    if final or stop == "moe":
                        p.out_toks.append(tk)
            p.barrier()


        def moe_sparse(l, src, dst, final):
            nt = n_tiles
            Aa = Alloc(PERS_END, ARENA)
            wup = [Aa("wup0", [KC, 2 * D], BF16), Aa("wup1", [KC, 2 * D], BF16)]
            wdn = [Aa("wdn0", [KC, D], BF16), Aa("wdn1", [KC, D], BF16)]
            bupg = [Aa("bupg0", [16]), Aa("bupg1", [16])]
            bupl = [Aa("bupl0", [8]), Aa("bupl1", [8])]
            rw32 = Aa("rw32", [KC, NE])
            rb32 = Aa("rb32", [NE], parts=1)
            bdn = Aa("bdn", [D], parts=32)
            onehot = Aa("onehot", [nt, 4, NE], BF16)
            rank = Aa("rank", [nt, NE])
            g4 = Aa("g4", [nt, 4])
            desti = Aa("desti", [nt, 4], I32)
            widx = Aa("widx", [NBLK, 8], I32)
            bidx = Aa("bidx", [NBLK], I32)
            prevsum = Aa("prevsum", [NE])
            gT = Aa("gT", [128], parts=32)
            MOE_W_END = Aa.off
            for k in range(KC):
                p.dma("sp", rw32[:, k, :], router_w[l, k * 128:(k + 1) * 128, :], writes=["rw32"])
            p.dma("sp", rb32[0:1, :], router_b[l], writes=["rb32"])
            p.dma("sp", bdn[0:32, :], b_down[l], writes=["bdn"])
            At = Alloc(MOE_W_END, ARENA)
            if final:
                fnwbc = At("fnwbc", [D])
                p.dma("sp", fnwbc, fnw.partition_broadcast(128), writes=["fnwbc"])
            At2 = Alloc(At.off, ARENA)
            compute_mod(l, 1, At)
            p.barrier()
            Ab = At2
            M1_START = Ab.off
            W1 = 3
            hd16 = [Ab("hd16_%d" % i, [D], BF16) for i in range(W1)]
            HD_END = Ab.off
            SETS = []
            for i in range(W1):
                SETS.append(dict(
                    i=i, xt=Ab("mxt%d" % i, [D]), F1=Ab("mF1_%d" % i, [D]), F2=Ab("mF2_%d" % i, [D]), hT32=Ab("hT32_%d" % i, [KC, 128]),
                    ss=Ab("mss%d" % i, [1]), rstd=Ab("mrstd%d" % i, [1]), lg=Ab("lg%d" % i, [NE]), mx8=Ab("mx8_%d" % i, [8]),
                    nm0=Ab("nm0_%d" % i, [1]), ex4=Ab("ex4_%d" % i, [4]), ssum=Ab("ssum%d" % i, [1]), mask32=Ab("mask32_%d" % i, [NE])))
            xt = [SETS[0]["xt"], SETS[1]["xt"]]
            M1_END = Ab.off
            Ab = Alloc(M1_START, ARENA)
            gsb = Ab("gsb", [512]); sgb = Ab("sgb", [512]); lsb = Ab("lsb", [512])
            r16 = [Ab("r16a", [4, D], BF16), Ab("r16b", [4, D], BF16)]
            rT16 = [Ab("rT16a", [KC, 512], BF16), Ab("rT16b", [KC, 512], BF16)]
            actT = Ab("actT", [KC, 512], BF16)
            ysb = [Ab("ysb0", [D]), Ab("ysb1", [D])]

            def lockstep(gens):
                gens = list(gens)
                while gens:
                    for g_ in list(gens):
                        try:
                            next(g_)
                        except StopIteration:
                            gens.remove(g_)

            def m1_tile(t, S):
                i = S["i"]
                X = S["xt"]; XR = ("mxt", i)
                F1s = S["F1"]; F2s = S["F2"]; F1R = ("mF1", i); F2R = ("mF2", i)
                hT = S["hT32"]; lg = S["lg"]; mx8 = S["mx8"]; nm0 = S["nm0"]; ex4 = S["ex4"]; ssum = S["ssum"]; mask32 = S["mask32"]
                bk = [2 * i, 2 * i + 1]
                p.dma("sp", X, src[t * 128:(t + 1) * 128, :], writes=[XR])
                norm_mod(X, XR, F2s, F2R, F1s, F1R, S["ss"], S["rstd"], ("mss", i), ("mrstd", i))
                yield
                H = hd16[i]; HR = ("hd16", i)
                cp("pool", H, F2s, [F2R], [HR])
                p.dma("sp", hd_dram[t * 128:(t + 1) * 128, :], H, reads=[HR], writes=[("hd", t)])
                for hh in range(2):
                    b = bk[hh]
                    for k4 in range(4):
                        k = hh * 4 + k4
                        tr(bank(b)[:, k4 * 128:(k4 + 1) * 128], F2s[:, k * 128:(k + 1) * 128], ident32, [F2R, "ident32"], [B(b)])
                    cp("act", hT[:, hh * 4:(hh + 1) * 4, :].rearrange("p a b -> p (a b)"), bank(b), [B(b)], [("hT32", i, hh)])
                    yield
                b = bk[0]
                for k in range(KC):
                    mm(bank(b)[:, 0:NE], hT[:, k, :], rw32[:, k, :], k == 0, False, [("hT32", i, k // 4), "rw32"], [B(b)])
                mm(bank(b)[:, 0:NE], ones32[0:1, :], rb32[0:1, :], False, True, ["ones32", "rb32"], [B(b)])
                cp("act", lg, bank(b)[:, 0:NE], [B(b)], [("lg", i)])
                yield
                p.op("dve", lambda e: e.max(out=mx8, in_=lg), reads=[("lg", i)], writes=[("mx8", i)])
                ts("dve", nm0, mx8[:, 0:1], -1.0, None, ALU.mult, None, [("mx8", i)], [("nm0", i)])
                yield
                act(ex4, mx8[:, 0:4], AF.Exp, [("mx8", i), ("nm0", i)], [("ex4", i), ("ssum", i)], bias=nm0, scale=1.0, accum_out=ssum)
                for k in range(4):
                    ts("dve", onehot[:, t, k, :], lg, mx8[:, k:k + 1], None, ALU.is_equal, None, [("lg", i), ("mx8", i)], [("onehot", t)])
                p.op("dve", lambda e: e.tensor_reduce(mask32, onehot[:, t].rearrange("p k e -> p e k"), AX.X, ALU.add),
                     reads=[("onehot", t)], writes=[("mask32", i)])
                yield
                p.op("dve", lambda e: e.reciprocal(ssum, ssum), reads=[("ssum", i)], writes=[("ssum", i)])
                ts("dve", g4[:, t, :], ex4, ssum, None, ALU.mult, None, [("ex4", i), ("ssum", i)], [("g4", t)])
                yield
                b = bk[1]
                mm(bank(b)[:, 0:NE], stri, mask32, True, t == 0, ["stri", ("mask32", i)], [B(b)])
                if t > 0:
                    mm(bank(b)[:, 0:NE], ones32, prevsum, False, True, ["ones32", "prevsum"], [B(b)])
                cp("act", rank[:, t, :], bank(b)[:, 0:NE], [B(b)], [("rank", t)])
                if t == 0:
                    cp("dve", prevsum, mask32, [("mask32", i)], ["prevsum"])
                else:
                    tt("dve", prevsum, prevsum, mask32, ALU.add, ["prevsum", ("mask32", i)], ["prevsum"])

            for t0 in range(0, nt, W1):
                lockstep([m1_tile(t, SETS[t - t0]) for t in range(t0, min(nt, t0 + W1))])
            p.barrier()
            ONEHOT = [("onehot", t) for t in range(nt)]
            RANK = [("rank", t) for t in range(nt)]
            G4 = [("g4", t) for t in range(nt)]

            Ac = Alloc(HD_END, ARENA)
            cnt = Ac("cnt", [NE]); nb = Ac("nb", [NE]); nbi = Ac("nbi", [NE], I32); incl = Ac("incl", [NE]); pst = Ac("pst", [NE])
            onesr = Ac("onesr", [NE]); dest = Ac("dest", [nt, NE]); prod = Ac("prod", [nt, 4, NE]); destf = Ac("destf", [nt, 4])
            cmp_ = Ac("cmp", [NBLK, NE]); blke = Ac("blke", [NBLK]); wf = Ac("wf", [NBLK, 8]); bf_ = Ac("bf", [NBLK])
            b = nextbank()
            mm(bank(b)[:, 0:NE], ones32, prevsum, True, True, ["ones32", "prevsum"], [B(b)])
            cp("act", cnt, bank(b)[:, 0:NE], [B(b)], ["cnt"])
            ts("dve", nb, cnt, 511.0, 1.0 / 512.0, ALU.add, ALU.mult, ["cnt"], ["nb"])
            ts("dve", nbi, nb, -0.5 + 1.0 / 1024.0, None, ALU.add, None, ["nb"], ["nbi"])
            cp("dve", nb, nbi, ["nbi"], ["nb"])
            p.op("pool", lambda e: e.memset(onesr, 1.0), writes=["onesr"])
            p.op("dve", lambda e: e.tensor_tensor_scan(incl, onesr, nb, 0.0, ALU.mult, ALU.add), reads=["onesr", "nb"], writes=["incl"])
            tt("dve", pst, incl, nb, ALU.subtract, ["incl", "nb"], ["pst"])
            ts("dve", pst, pst, 512.0, None, ALU.mult, None, ["pst"], ["pst"])
            tt("dve", dest, rank, pst.unsqueeze(1).to_broadcast([128, nt, NE]), ALU.add, RANK + ["pst"], ["dest"])
            tt("dve", prod, onehot, dest.unsqueeze(2).to_broadcast([128, nt, 4, NE]), ALU.mult, ONEHOT + ["dest"], ["prod"])
            p.op("dve", lambda e: e.tensor_reduce(destf, prod, AX.X, ALU.add), reads=["prod"], writes=["destf"])
            cp("dve", desti, destf, ["destf"], ["desti"])
            NB_ = NBLK
            tt("dve", cmp_, incl.unsqueeze(1).to_broadcast([128, NB_, NE]), idxc[:, 0:NB_].unsqueeze(2).to_broadcast([128, NB_, NE]),
               ALU.is_le, ["incl", "idxc"], ["cmp"])
            p.op("dve", lambda e: e.tensor_reduce(blke, cmp_, AX.X, ALU.add), reads=["cmp"], writes=["blke"])
            ts("dve", blke, blke, float(NE - 1), None, ALU.min, None, ["blke"], ["blke"])
            ts("dve", bf_, blke, 128.0, idxc[:, 72:73], ALU.mult, ALU.add, ["blke", "idxc"], ["bf"])
            if l > 0:
                ts("dve", bf_, bf_, float(l * NE * 128), None, ALU.add, None, ["bf"], ["bf"])
            cp("dve", bidx, bf_, ["bf"], ["bidx"])
            ts("dve", blke, blke, 1024.0, float(l * NE * 1024), ALU.mult, ALU.add, ["blke", "bf"], ["blke"])
            tt("dve", wf, blke.unsqueeze(2).to_broadcast([128, NB_, 8]), idxc[:, 64:72].unsqueeze(1).to_broadcast([128, NB_, 8]),
               ALU.add, ["blke", "idxc"], ["wf"])
            cp("dve", widx, wf, ["wf"], ["widx"])

            SC = []
            for t in range(nt):
                H = hd16[t % W1]; HR = ("hd16", t % W1)
                p.dma("sp", H, hd_dram[t * 128:(t + 1) * 128, :], reads=[("hd", t)], writes=[HR])
                for k in range(4):
                    nm = ("rows", t, k)
                    SC.append(nm)
                    p.idma(lambda e, H=H, t=t, k=k: e.indirect_dma_start(
                        out=rowsbuf, out_offset=bass.IndirectOffsetOnAxis(ap=desti[:, t, k:k + 1], axis=0), in_=H, in_offset=None),
                        reads=[HR, "desti"], writes=[nm])

            p.barrier()
            wup_d = w_up.rearrange("l e r n -> (l e r) n")
            wdn_d = w_down.rearrange("l e r n -> (l e r) n")
            bup_d = b_upT2.rearrange("l r c -> (l r) c")

            def load_block_w(j, buf):
                for k in range(KC):
                    p.idma(lambda e, j=j, k=k, buf=buf: e.indirect_dma_start(
                        out=wup[buf][:, k, :], out_offset=None, in_=wup_d, in_offset=bass.IndirectOffsetOnAxis(ap=widx[:, j, k:k + 1], axis=0)),
                        reads=["widx"], writes=[("wup", buf, k)])
                for k in range(KC):
                    p.idma(lambda e, j=j, k=k, buf=buf: e.indirect_dma_start(
                        out=wdn[buf][:, k, :], out_offset=None, in_=wdn_d, in_offset=bass.IndirectOffsetOnAxis(ap=widx[:, j, k:k + 1], axis=0)),
                        reads=["widx"], writes=[("wdn", buf, k)])
                p.idma(lambda e, j=j, buf=buf: e.indirect_dma_start(
                    out=bupg[buf], out_offset=None, in_=bup_d, in_offset=bass.IndirectOffsetOnAxis(ap=bidx[:, j:j + 1], axis=0)),
                    reads=["bidx"], writes=[("bupg", buf)])

            OR = []
            load_block_w(0, 0)

            def load_rows(j):
                rb = j % 2
                p.dma("sp", r16[rb], rowsbuf[j * 512:(j + 1) * 512, :].rearrange("(a p) n -> p a n", p=128), reads=SC, writes=[("r16", rb)])

            def transpose_rows(j):
                rb = j % 2
                for k2 in range(4):
                    bt = nextbank(0, 8)
                    for kk in range(2):
                        k = k2 * 2 + kk
                        for a in range(4):
                            tr(bankbf(bt)[:, kk * 512 + a * 128:kk * 512 + (a + 1) * 128], r16[rb][:, a, k * 128:(k + 1) * 128], ident16,
                               [("r16", rb), "ident16"], [B(bt)])
                    cp("act" if k2 % 2 == 0 else "dve", rT16[rb][:, k2 * 2:k2 * 2 + 2, :].rearrange("p a b -> p (a b)"), bankbf(bt), [B(bt)], [("rT16", rb, k2)])

            load_rows(0)
            transpose_rows(0)
            if NBLK > 1:
                load_rows(1)
            for j in range(NBLK):
                buf = j % 2
                rb = j % 2
                RT = rT16[rb]
                if j + 1 < NBLK:
                    load_block_w(j + 1, 1 - buf)
                ts("dve", bupl[buf], bupg[buf][:, 8:16], 1.0, None, ALU.add, None, [("bupg", buf)], [("bupl", buf)])
                for cch in range(KC):
                    bg_ = nextbank(0, 8); bl_ = nextbank(0, 8)
                    for k in range(KC):
                        mm(bank(bg_), wup[buf][:, k, cch * 128:(cch + 1) * 128], RT[:, k, :], k == 0, k == KC - 1, [("wup", buf, k), ("rT16", rb, k // 2)], [B(bg_)])
                    for k in range(KC):
                        mm(bank(bl_), wup[buf][:, k, D + cch * 128:D + (cch + 1) * 128], RT[:, k, :], k == 0, k == KC - 1, [("wup", buf, k), ("rT16", rb, k // 2)], [B(bl_)])
                    ts("dve", gsb, bank(bg_), bupg[buf][:, cch:cch + 1], 7.0, ALU.add, ALU.min, [B(bg_), ("bupg", buf)], ["gsb"])
                    act(sgb, gsb, AF.Sigmoid, ["gsb"], ["sgb"], scale=1.702)
                    act(lsb, bank(bl_), AF.Identity, [B(bl_), ("bupl", buf)], ["lsb"], bias=bupl[buf][:, cch:cch + 1], scale=1.0)
                    ts("dve", lsb, lsb, 8.0, -6.0, ALU.min, ALU.max, ["lsb"], ["lsb"])
                    tt("dve", gsb, gsb, lsb, ALU.mult, ["gsb", "lsb"], ["gsb"])
                    tt("dve", actT[:, cch, :], sgb, gsb, ALU.mult, ["sgb", "gsb"], [("actT", cch)])
                if j + 1 < NBLK:
                    transpose_rows(j + 1)
                if j + 2 < NBLK:
                    load_rows(j + 2)
                for a in range(4):
                    Y = ysb[a % 2]; YR = ("ysb", a % 2)
                    for half in range(2):
                        b = nextbank(0, 8)
                        for k in range(KC):
                            mm(bank(b), actT[:, k, a * 128:(a + 1) * 128], wdn[buf][:, k, half * 512:(half + 1) * 512], k == 0, k == KC - 1,
                               [("actT", k), ("wdn", buf, k)], [B(b)])
                        cp("act" if half == 0 else "dve", Y[:, half * 512:(half + 1) * 512], bank(b), [B(b)], [YR])
                    nm = ("orow", j, a)
                    OR.append(nm)
                    p.dma("sp", orows[j * 512 + a * 128:j * 512 + (a + 1) * 128, :], Y, reads=[YR], writes=[nm])

            p.barrier()
            Ad = Alloc(PERS_END, PERS_END + 64 * 1024)
            W5 = 3
            M5S = [dict(i=i, yk=Ad("yk%d" % i, [4, D]), gd=Ad("gd%d" % i, [NE]), accb=Ad("accb%d" % i, [D]), gT=Ad("gT5_%d" % i, [128], parts=32))
                   for i in range(W5)]

            def m5_tile(t, S5_):
                i = S5_["i"]
                yk = S5_["yk"]; gd = S5_["gd"]; accb = S5_["accb"]; gTs = S5_["gT"]
                S = SETS[i]
                X = S["xt"]; XR = ("mxt", i); F1s = S["F1"]; F2s = S["F2"]; F1R = ("mF1", i); F2R = ("mF2", i)
                ssx = S["ss"]; rs = S["rstd"]; SSR = ("mss", i); RSR = ("mrstd", i)
                bk = [2 * i, 2 * i + 1]
                for k in range(4):
                    p.idma(lambda e, t=t, k=k: e.indirect_dma_start(
                        out=yk[:, k, :], out_offset=None, in_=orows, in_offset=bass.IndirectOffsetOnAxis(ap=desti[:, t, k:k + 1], axis=0)),
                        reads=OR + ["desti"], writes=[("yk", i, k)])
                p.dma("sp", X, src[t * 128:(t + 1) * 128, :], writes=[XR])
                ts("dve", gd, onehot[:, t, 0, :], g4[:, t, 0:1], None, ALU.mult, None, [("onehot", t), ("g4", t)], [("gd", i)])
                for k in range(1, 4):
                    stt(gd, onehot[:, t, k, :], g4[:, t, k:k + 1], gd, ALU.mult, ALU.add, [("onehot", t), ("g4", t), ("gd", i)], [("gd", i)])
                yield
                b = bk[0]
                tr(bank(b)[0:32, 0:128], gd, ident32, [("gd", i), "ident32"], [B(b)])
                cp("act", gTs[0:32, :], bank(b)[0:32, 0:128], [B(b)], [("gT5", i)])
                yield
                for half in range(2):
                    b = bk[half]
                    mm(bank(b), gTs[0:32, :], bdn[0:32, half * 512:(half + 1) * 512], True, True, [("gT5", i), "bdn"], [B(b)])
                    cp("act", accb[:, half * 512:(half + 1) * 512], bank(b), [B(b)], [("accb", i)])
                yield
                for k in range(4):
                    stt(accb, yk[:, k, :], g4[:, t, k:k + 1], accb, ALU.mult, ALU.add, [("yk", i, k), ("g4", t), ("accb", i)], [("accb", i)])
                    if k % 2 == 1:
                        yield
                tt("dve", F1s, accb, modr[:, 2 * D:3 * D], ALU.mult, [("accb", i)] + MODR(2), [F1R])
                tt("pool", F1s, F1s, X, ALU.add, [F1R, XR], [F1R])
                yield
                if final:
                    act(F2s, F1s, AF.Square, [F1R], [F2R, SSR], accum_out=ssx)
                    ts("pool", rs, ssx, 1.0 / D, EPS, ALU.mult, ALU.add, [SSR], [RSR])
                    tt("pool", rs, rs, nh[:, 0:1], ALU.pow, [RSR, "nh"], [RSR])
                    yield
                    stt(F2s, F1s, rs, fnwbc, ALU.mult, ALU.mult, [F1R, RSR, "fnwbc"], [F2R])
                    tk = p.dma("sp", dst[t * 128:(t + 1) * 128, :], F2s, reads=[F2R], writes=[("dst2", t)])
                else:
                    tk = p.dma("sp", dst[t * 128:(t + 1) * 128, :], F1s, reads=[F1R], writes=[("dst2", t)])
                if final or stop == "moe":
                    p.out_toks.append(tk)

            for t0 in range(0, nt, W5):
                lockstep([m5_tile(t, M5S[t - t0]) for t in range(t0, min(nt, t0 + W5))])
            p.barrier()

        src_name = "x"
        cur = x_in
        for l in range(n_layers):
            last = (l == n_layers - 1)
            if stop == "mix" and last:
                mixer_phase(l, cur, y_out)
                break
            mixer_phase(l, cur, xsA)
            if last:
                moe_sparse(l, xsA, y_out, final=(stop is None))
            else:
                moe_sparse(l, xsA, xsB, final=False)
                cur = xsB
        p.wait_all("sp", p.out_toks)
        p.run(st)
    return nc


_CACHE = {}


def kernel(**inputs):
    inp = {k: np.asarray(v) for k, v in inputs.items()}
    consts = make_consts()
    w = prep_weights(inp)
    if "nc" not in _CACHE:
        _CACHE["nc"] = build()
    nc = _CACHE["nc"]
    in_maps = []
    for b in range(8):
        m = dict(w)
        m.update(consts)
        m["x"] = np.ascontiguousarray(inp["x"][b])
        m["c"] = np.ascontiguousarray(inp["c"][b].reshape(KC, 128).T)
        in_maps.append(m)
    res = run_bass_kernel_spmd(nc, in_maps, core_ids=list(range(8)))
    return np.stack([r["y"] for r in res.results], axis=0).astype(np.float32)
```
